# Optimizing a Trainium2 kernel written in Bass

```python
import math
import functools
import jax
import jax.numpy as jnp
from jax import lax
import numpy as np

D_MODEL = 2048
BATCH = 2
SEQ = 8192
DEPTH = 2

CTX_LEN = 256
GRID_W = 64
N_BRANCH = 4
BRANCH_W = D_MODEL // N_BRANCH
CONV_W = 4
LN_EPS = 1e-5

LRU_W = BRANCH_W
LRU_BLOCKS = 8
LRU_BW = LRU_W // LRU_BLOCKS
LRU_C = 8.0

NA_HEADS = 8
NA_HD = BRANCH_W // NA_HEADS
NA_KH = 8
NA_KW = 16

SSD_HEADS = 8
SSD_HD = BRANCH_W // SSD_HEADS
SSD_GROUPS = 2
SSD_STATE = 64
SSD_CHUNK = 128
SSD_GN = SSD_GROUPS * SSD_STATE

ML_HEADS = 4
ML_HD = BRANCH_W // ML_HEADS
ML_CHUNK = 128
ROPE_BASE = 10000.0

FFN_DENSE = 5632
N_EXPERTS = 8
TOP_K = 2
FFN_EXPERT = 2816
N_DENSE = (DEPTH + 1) // 2
N_MOE = DEPTH // 2

IN_SPLITS = (LRU_W, LRU_W,
             3 * BRANCH_W,
             BRANCH_W, BRANCH_W, SSD_GN, SSD_GN, 2 * SSD_HEADS,
             BRANCH_W, BRANCH_W, BRANCH_W, BRANCH_W, 4 * ML_HEADS,
             N_BRANCH * D_MODEL)
N_IN = sum(IN_SPLITS)

kernel_name = 'hybrid_lru_natten_ssd_mlstm_moe_dit'


def split_cols(p, sizes):
    return jnp.split(p, np.cumsum(sizes)[:-1].tolist(), axis=-1)


def flip_seq(t, direction):
    return t[:, ::-1] if direction == 1 else t


def layer_norm(x, g, b):
    xf = x.astype(jnp.float32)
    mu = xf.mean(-1, keepdims=True)
    var = jnp.square(xf - mu).mean(-1, keepdims=True)
    return ((xf - mu) * lax.rsqrt(var + LN_EPS)).astype(x.dtype) * g + b


def rms_norm(x, g):
    xf = x.astype(jnp.float32)
    return (xf * lax.rsqrt(jnp.square(xf).mean(-1, keepdims=True) + LN_EPS)).astype(x.dtype) * g


def dwconv_centred(x, w, b):
    ch = x.shape[-1]
    y = lax.conv_general_dilated(x, w[:, None, :].astype(x.dtype), window_strides=(1,),
                                 padding=[(CONV_W // 2, CONV_W - 1 - CONV_W // 2)],
                                 dimension_numbers=('NWC', 'WIO', 'NWC'), feature_group_count=ch)
    return y + b


def rope_2d_tables(seq, dtype):
    t = jnp.arange(seq, dtype=jnp.int32)
    pos = jnp.stack([t // GRID_W, t % GRID_W], axis=-1).astype(jnp.float32)
    nf = ML_HD // 4
    inv_freq = ROPE_BASE ** (-jnp.arange(nf, dtype=jnp.float32) / nf)
    ang = jnp.broadcast_to(pos[:, :, None, None] * inv_freq, (seq, 2, 2, nf)).reshape(seq, ML_HD)
    return jnp.cos(ang).astype(dtype), jnp.sin(ang).astype(dtype)


def apply_rope_2d(x, cos, sin):
    xs = x.reshape(x.shape[:-1] + (2, 2, ML_HD // 4))
    rot = jnp.stack([-xs[..., 1, :], xs[..., 0, :]], axis=-2).reshape(x.shape)
    return x * cos[:, None] + rot * sin[:, None]


def linear_scan(a, u, h0):
    def op(left, right):
        return (left[0] * right[0], right[0] * left[1] + right[1])
    a_cum, h = lax.associative_scan(op, (a, u), axis=1)
    return h + a_cum * h0[:, None]


def rglru_coeffs(x, wa, ba, wx, bx, lam):
    shp = x.shape
    xb = x.reshape(shp[:-1] + (LRU_BLOCKS, LRU_BW))
    r = jax.nn.sigmoid(jnp.einsum('blgi,gij->blgj', xb, wa).reshape(shp) + ba)
    i = jax.nn.sigmoid(jnp.einsum('blgi,gij->blgj', xb, wx).reshape(shp) + bx)
    log_a = (-LRU_C * r * jax.nn.softplus(-lam)).astype(jnp.float32)
    a = jnp.exp(log_a)
    u = jnp.sqrt(-jnp.expm1(2.0 * log_a)) * (i * x)
    return a.astype(x.dtype), u.astype(x.dtype)


def rglru_branch(xl, gl, xc, gc, conv_w, conv_b, wa, ba, wx, bx, lam, need_ctx_out):
    xl = dwconv_centred(xl, conv_w, conv_b)
    xc = dwconv_centred(xc, conv_w, conv_b)
    hl, hc = [], []
    for d in range(2):
        a, u = rglru_coeffs(flip_seq(xc, d), wa[d], ba[d], wx[d], bx[d], lam[d])
        h_c = linear_scan(a, u, jnp.zeros_like(u[:, 0]))
        a, u = rglru_coeffs(flip_seq(xl, d), wa[d], ba[d], wx[d], bx[d], lam[d])
        hl.append(flip_seq(linear_scan(a, u, h_c[:, -1]), d))
        if need_ctx_out:
            hc.append(flip_seq(h_c, d))
    y_l = (hl[0] + hl[1]) * jax.nn.gelu(gl)
    y_c = (hc[0] + hc[1]) * jax.nn.gelu(gc) if need_ctx_out else None
    return y_l, y_c


def na_branch(qkv_l, qkv_c, rpb, need_ctx_out):
    bsz, seq, _ = qkv_l.shape
    n_ctx = qkv_c.shape[1]
    rows = seq // GRID_W
    kh = min(NA_KH, rows)
    scale = NA_HD ** -0.5
    ql, kl, vl = [t.reshape(bsz, rows, GRID_W, NA_HEADS, NA_HD) for t in jnp.split(qkv_l, 3, axis=-1)]
    qc, kc, vc = [t.reshape(bsz, n_ctx, NA_HEADS, NA_HD) for t in jnp.split(qkv_c, 3, axis=-1)]
    r = jnp.arange(rows)
    row_start = jnp.clip(r - kh // 2, 0, rows - kh)
    key_rows = row_start[:, None] + jnp.arange(kh)[None, :]
    k_band = kl[:, key_rows]
    v_band = vl[:, key_rows]
    col = jnp.arange(GRID_W)
    col_start = jnp.clip(col - NA_KW // 2, 0, GRID_W - NA_KW)
    col_ok = (col[None, :] >= col_start[:, None]) & (col[None, :] < col_start[:, None] + NA_KW)
    dr = key_rows - r[:, None] + (NA_KH - 1)
    dc = jnp.clip(col[None, :] - col[:, None] + (NA_KW - 1), 0, 2 * NA_KW - 2)
    bias = rpb[:, dr[:, None, :, None], dc[None, :, None, :]]
    qs = ql * scale
    s_win = jnp.einsum('brwhd,brkxhd->bhrwkx', qs, k_band).astype(jnp.float32) + bias
    s_win = jnp.where(col_ok[:, None, :], s_win, -jnp.inf)
    s_ctx = jnp.einsum('brwhd,bchd->bhrwc', qs, kc).astype(jnp.float32)
    n_win = kh * GRID_W
    s = jnp.concatenate([s_win.reshape(bsz, NA_HEADS, rows, GRID_W, n_win), s_ctx], axis=-1)
    p = jax.nn.softmax(s, axis=-1).astype(vl.dtype)
    p_win = p[..., :n_win].reshape(bsz, NA_HEADS, rows, GRID_W, kh, GRID_W)
    out = (jnp.einsum('bhrwkx,brkxhd->brwhd', p_win, v_band)
           + jnp.einsum('bhrwc,bchd->brwhd', p[..., n_win:], vc))
    y_l = out.reshape(bsz, seq, BRANCH_W)
    y_c = None
    if need_ctx_out:
        p_c = jax.nn.softmax(jnp.einsum('bqhd,bkhd->bhqk', qc * scale, kc).astype(jnp.float32), axis=-1)
        y_c = jnp.einsum('bhqk,bkhd->bqhd', p_c.astype(vc.dtype), vc).reshape(bsz, n_ctx, BRANCH_W)
    return y_l, y_c


def ssd_scan(x, dt, a, bm, cm, h0, with_y):
    bsz, seq, nh, hp = x.shape
    ng, ns = bm.shape[-2:]
    hg = nh // ng
    q = SSD_CHUNK
    nc = seq // q
    xq = x.reshape(bsz, nc, q, ng, hg, hp)
    dtq = dt.reshape(bsz, nc, q, ng, hg).astype(jnp.float32)
    bq = bm.reshape(bsz, nc, q, ng, ns)
    cq = cm.reshape(bsz, nc, q, ng, ns)
    cum = jnp.cumsum(dtq * a.reshape(ng, hg), axis=2)
    last = cum[:, :, -1]
    w_end = jnp.exp(last[:, :, None] - cum) * dtq
    s_chunk = jnp.einsum('bcjgn,bcjgh,bcjghp->bcghpn', bq, w_end, xq)

    def step(h, inp):
        decay, s_c = inp
        return decay[..., None, None] * h + s_c, h

    h_fin, h_start = lax.scan(step, h0.reshape(bsz, ng, hg, hp, ns),
                              (jnp.moveaxis(jnp.exp(last), 1, 0), jnp.moveaxis(s_chunk, 1, 0)))
    h_fin = h_fin.reshape(bsz, nh, hp, ns)
    if not with_y:
        return None, h_fin
    h_start = jnp.moveaxis(h_start, 0, 1)
    y_inter = jnp.einsum('bcign,bcghpn,bcigh->bcighp', cq, h_start, jnp.exp(cum))
    cum_t = jnp.moveaxis(cum, 2, -1)
    lower = jnp.tril(jnp.ones((q, q), dtype=bool))
    decay = jnp.exp(jnp.where(lower, cum_t[..., :, None] - cum_t[..., None, :], -jnp.inf))
    cb = jnp.einsum('bcign,bcjgn->bcgij', cq, bq)
    m = cb[:, :, :, None] * decay * jnp.moveaxis(dtq, 2, -1)[..., None, :]
    y_intra = jnp.einsum('bcghij,bcjghp->bcighp', m, xq)
    return (y_intra + y_inter).reshape(bsz, seq, nh, hp), h_fin


def ssd_branch(parts_l, parts_c, conv_w, conv_b, dt_bias, a_log, d_skip, norm_g, need_ctx_out):
    def prep(parts):
        z, xs, bm, cm, dt_raw = parts
        bsz, seq = xs.shape[:2]
        xbc = jax.nn.silu(dwconv_centred(jnp.concatenate([xs, bm, cm], axis=-1), conv_w, conv_b))
        xs, bm, cm = split_cols(xbc, (BRANCH_W, SSD_GN, SSD_GN))
        return (z, xs.reshape(bsz, seq, SSD_HEADS, SSD_HD), bm.reshape(bsz, seq, SSD_GROUPS, SSD_STATE),
                cm.reshape(bsz, seq, SSD_GROUPS, SSD_STATE), dt_raw.reshape(bsz, seq, 2, SSD_HEADS))

    zl, xl, bl, cl, dtl = prep(parts_l)
    zc, xc, bc, cc, dtc = prep(parts_c)
    bsz = xl.shape[0]
    yl = xl * d_skip[:, None]
    yc = xc * d_skip[:, None] if need_ctx_out else None
    for d in range(2):
        a = -jnp.exp(a_log[d].astype(jnp.float32))
        dt_c = jax.nn.softplus(dtc[:, :, d] + dt_bias[d])
        dt_l = jax.nn.softplus(dtl[:, :, d] + dt_bias[d])
        h0 = jnp.zeros((bsz, SSD_HEADS, SSD_HD, SSD_STATE), jnp.float32)
        y_c, h_c = ssd_scan(flip_seq(xc, d), flip_seq(dt_c, d), a, flip_seq(bc, d), flip_seq(cc, d), h0, need_ctx_out)
        y_l, _ = ssd_scan(flip_seq(xl, d), flip_seq(dt_l, d), a, flip_seq(bl, d), flip_seq(cl, d), h_c, True)
        yl = yl + flip_seq(y_l, d)
        if need_ctx_out:
            yc = yc + flip_seq(y_c, d)

    def finish(y, z):
        return rms_norm(y.reshape(z.shape).astype(z.dtype) * jax.nn.silu(z), norm_g)

    return finish(yl, zl), (finish(yc, zc) if need_ctx_out else None)


def mlstm_scan(q, k, v, li, lf, state, with_y):
    bsz, seq = q.shape[:2]
    qn = ML_CHUNK
    nc = seq // qn
    lower = jnp.tril(jnp.ones((qn, qn), dtype=bool))

    def chunks(t):
        return jnp.moveaxis(t.reshape((bsz, nc, qn) + t.shape[2:]), 1, 0)

    def step(carry, inp):
        c_st, n_st, m_st = carry
        qc, kc, vc, ic, fc = inp
        b = jnp.cumsum(fc, axis=1)
        b_end = b[:, -1]
        end_log = b_end[:, None] - b + ic
        m_new = jnp.maximum(b_end + m_st, end_log.max(axis=1))
        w = jnp.exp(end_log - m_new[:, None])
        carry_scale = jnp.exp(b_end + m_st - m_new)
        c_new = carry_scale[..., None, None] * c_st + jnp.einsum('bjh,bjhv,bjhk->bhvk', w, vc, kc)
        n_new = carry_scale[..., None] * n_st + jnp.einsum('bjh,bjhk->bhk', w, kc)
        if not with_y:
            return (c_new, n_new, m_new), None
        dlog = jnp.where(lower[None, :, :, None], b[:, :, None, :] - b[:, None, :, :] + ic[:, None, :, :], -jnp.inf)
        m_inter = b + m_st[:, None]
        m_i = jnp.maximum(dlog.max(axis=2), m_inter)
        s = jnp.einsum('bihd,bjhd->bijh', qc, kc) * jnp.exp(dlog - m_i[:, :, None])
        w_in = jnp.exp(m_inter - m_i)
        num = jnp.einsum('bijh,bjhd->bihd', s, vc) + w_in[..., None] * jnp.einsum('bhvk,bihk->bihv', c_st, qc)
        den = s.sum(axis=2) + w_in * jnp.einsum('bhk,bihk->bih', n_st, qc)
        h = num / jnp.maximum(jnp.abs(den), jnp.exp(-m_i))[..., None]
        return (c_new, n_new, m_new), h

    state, hs = lax.scan(step, state, (chunks(q), chunks(k), chunks(v), chunks(li), chunks(lf)))
    if not with_y:
        return None, state
    return jnp.moveaxis(hs, 0, 1).reshape(q.shape), state


def mlstm_branch(parts_l, parts_c, cos, sin, conv_w, conv_b, i_bias, f_bias, need_ctx_out):
    def prep(parts, use_rope):
        qr, kr, v, o, g = parts
        bsz, seq = v.shape[:2]
        qk = jax.nn.silu(dwconv_centred(jnp.concatenate([qr, kr], axis=-1), conv_w, conv_b))
        q = qk[..., :BRANCH_W].reshape(bsz, seq, ML_HEADS, ML_HD)
        k = qk[..., BRANCH_W:].reshape(bsz, seq, ML_HEADS, ML_HD)
        if use_rope:
            q = apply_rope_2d(q, cos, sin)
            k = apply_rope_2d(k, cos, sin)
        return (q * ML_HD ** -0.5, k, v.reshape(bsz, seq, ML_HEADS, ML_HD), o,
                g.reshape(bsz, seq, 2, 2, ML_HEADS).astype(jnp.float32))

    ql, kl, vl, ol, gl = prep(parts_l, True)
    qc, kc, vc, oc, gc = prep(parts_c, False)
    bsz = ql.shape[0]
    hl, hc = [], []
    for d in range(2):
        state0 = (jnp.zeros((bsz, ML_HEADS, ML_HD, ML_HD), jnp.float32),
                  jnp.zeros((bsz, ML_HEADS, ML_HD), jnp.float32),
                  jnp.zeros((bsz, ML_HEADS), jnp.float32))
        li_c = gc[:, :, d, 0] + i_bias[d]
        lf_c = jax.nn.log_sigmoid(gc[:, :, d, 1] + f_bias[d])
        li_l = gl[:, :, d, 0] + i_bias[d]
        lf_l = jax.nn.log_sigmoid(gl[:, :, d, 1] + f_bias[d])
        h_c, st = mlstm_scan(flip_seq(qc, d), flip_seq(kc, d), flip_seq(vc, d), flip_seq(li_c, d),
                             flip_seq(lf_c, d), state0, need_ctx_out)
        h_l, _ = mlstm_scan(flip_seq(ql, d), flip_seq(kl, d), flip_seq(vl, d), flip_seq(li_l, d),
                            flip_seq(lf_l, d), st, True)
        hl.append(flip_seq(h_l, d))
        if need_ctx_out:
            hc.append(flip_seq(h_c, d))
    y_l = jax.nn.sigmoid(ol) * (hl[0] + hl[1]).reshape(ol.shape).astype(ol.dtype)
    y_c = jax.nn.sigmoid(oc) * (hc[0] + hc[1]).reshape(oc.shape).astype(oc.dtype) if need_ctx_out else None
    return y_l, y_c


def merge_branches(branches, gates_raw, w_branch, w_out):
    g = jax.nn.sigmoid(gates_raw)
    merged = None
    for n in range(N_BRANCH):
        term = g[..., n * D_MODEL:(n + 1) * D_MODEL] * (branches[n].astype(g.dtype) @ w_branch[n])
        merged = term if merged is None else merged + term
    return merged @ w_out


def hybrid_mixer(hl, hc, cos, sin, need_ctx_out, w_in, lru_conv_w, lru_conv_b, lru_wa, lru_ba, lru_wx,
                 lru_bx, lru_lambda, na_rpb, ssd_conv_w, ssd_conv_b, ssd_dt_bias, ssd_a_log, ssd_d,
                 ssd_norm_g, ml_conv_w, ml_conv_b, ml_i_bias, ml_f_bias, w_branch, w_out):
    pl = split_cols(hl @ w_in, IN_SPLITS)
    pc = split_cols(hc @ w_in, IN_SPLITS)
    ya_l, ya_c = rglru_branch(pl[0], pl[1], pc[0], pc[1], lru_conv_w, lru_conv_b, lru_wa, lru_ba,
                              lru_wx, lru_bx, lru_lambda, need_ctx_out)
    yb_l, yb_c = na_branch(pl[2], pc[2], na_rpb, need_ctx_out)
    yc_l, yc_c = ssd_branch(pl[3:8], pc[3:8], ssd_conv_w, ssd_conv_b, ssd_dt_bias, ssd_a_log, ssd_d,
                            ssd_norm_g, need_ctx_out)
    yd_l, yd_c = mlstm_branch(pl[8:13], pc[8:13], cos, sin, ml_conv_w, ml_conv_b, ml_i_bias, ml_f_bias,
                              need_ctx_out)
    y_l = merge_branches((ya_l, yb_l, yc_l, yd_l), pl[13], w_branch, w_out)
    y_c = merge_branches((ya_c, yb_c, yc_c, yd_c), pc[13], w_branch, w_out) if need_ctx_out else None
    return y_l, y_c


def swiglu(h, w_gate, w_up, w_down):
    return (jax.nn.silu(h @ w_gate) * (h @ w_up)) @ w_down


def moe_swiglu(h, w_router, b_router, w_gate, w_up, w_down):
    logits = (h @ w_router).astype(jnp.float32) + b_router
    top_val, top_idx = lax.top_k(logits, TOP_K)
    probs = jax.nn.softmax(top_val, axis=-1)
    combine = jnp.einsum('...k,...ke->...e', probs,
                         jax.nn.one_hot(top_idx, N_EXPERTS, dtype=probs.dtype)).astype(h.dtype)
    y = None
    for e in range(N_EXPERTS):
        term = combine[..., e:e + 1] * swiglu(h, w_gate[e], w_up[e], w_down[e])
        y = term if y is None else y + term
    return y


def setup_inputs(seed: int = 0) -> dict:
    key = jax.random.key(seed)
    keys = jax.random.split(key, 40)
    f32 = jnp.float32
    dm = D_MODEL
    beta = (8.0 * DEPTH) ** -0.25

    def nrm(i, shape, scale):
        return jax.random.normal(keys[i], shape, f32) * scale

    def uni(i, shape, lo, hi):
        return jax.random.uniform(keys[i], shape, f32, lo, hi)

    p_lru = uni(13, (DEPTH, 2, LRU_W), 0.9, 0.999) ** (1.0 / LRU_C)
    dt0 = jnp.exp(uni(17, (DEPTH, 2, SSD_HEADS), math.log(1e-3), math.log(1e-1)))
    return {
        'x': nrm(0, (BATCH, SEQ, dm), 1.0),
        'c': nrm(1, (BATCH, dm), 1.0),
        'ctx': nrm(2, (BATCH, CTX_LEN, dm), 1.0),
        'c_ctx': nrm(3, (dm,), 1.0),
        'w_ada': nrm(4, (DEPTH, dm, 6 * dm), 0.5 * dm ** -0.5),
        'b_ada': nrm(5, (DEPTH, 6 * dm), 0.02),
        'w_in': nrm(6, (DEPTH, dm, N_IN), dm ** -0.5),
        'lru_conv_w': nrm(7, (DEPTH, CONV_W, LRU_W), CONV_W ** -0.5),
        'lru_conv_b': nrm(8, (DEPTH, LRU_W), 0.02),
        'lru_wa': nrm(9, (DEPTH, 2, LRU_BLOCKS, LRU_BW, LRU_BW), LRU_BW ** -0.5),
        'lru_ba': nrm(10, (DEPTH, 2, LRU_W), 0.02),
        'lru_wx': nrm(11, (DEPTH, 2, LRU_BLOCKS, LRU_BW, LRU_BW), LRU_BW ** -0.5),
        'lru_bx': nrm(12, (DEPTH, 2, LRU_W), 0.02),
        'lru_lambda': jnp.log(p_lru) - jnp.log1p(-p_lru),
        'na_rpb': nrm(14, (DEPTH, NA_HEADS, 2 * NA_KH - 1, 2 * NA_KW - 1), 0.1),
        'ssd_conv_w': nrm(15, (DEPTH, CONV_W, BRANCH_W + 2 * SSD_GN), CONV_W ** -0.5),
        'ssd_conv_b': nrm(16, (DEPTH, BRANCH_W + 2 * SSD_GN), 0.02),
        'ssd_dt_bias': dt0 + jnp.log(-jnp.expm1(-dt0)),
        'ssd_a_log': jnp.log(uni(18, (DEPTH, 2, SSD_HEADS), 1.0, 16.0)),
        'ssd_d': 1.0 + nrm(19, (DEPTH, SSD_HEADS), 0.1),
        'ssd_norm_g': 1.0 + nrm(20, (DEPTH, BRANCH_W), 0.02),
        'ml_conv_w': nrm(21, (DEPTH, CONV_W, 2 * BRANCH_W), CONV_W ** -0.5),
        'ml_conv_b': nrm(22, (DEPTH, 2 * BRANCH_W), 0.02),
        'ml_i_bias': nrm(23, (DEPTH, 2, ML_HEADS), 0.1),
        'ml_f_bias': uni(24, (DEPTH, 2, ML_HEADS), 3.0, 6.0),
        'w_branch': nrm(25, (DEPTH, N_BRANCH, BRANCH_W, dm), BRANCH_W ** -0.5),
        'w_out': nrm(26, (DEPTH, dm, dm), beta * dm ** -0.5),
        'ln_g': 1.0 + nrm(27, (DEPTH, 2, dm), 0.02),
        'ln_b': nrm(28, (DEPTH, 2, dm), 0.02),
        'ffn_w_gate': nrm(29, (N_DENSE, dm, FFN_DENSE), dm ** -0.5),
        'ffn_w_up': nrm(30, (N_DENSE, dm, FFN_DENSE), dm ** -0.5),
        'ffn_w_down': nrm(31, (N_DENSE, FFN_DENSE, dm), beta * FFN_DENSE ** -0.5),
        'moe_w_router': nrm(32, (N_MOE, dm, N_EXPERTS), dm ** -0.5),
        'moe_b_router': nrm(33, (N_MOE, N_EXPERTS), 0.01),
        'moe_w_gate': nrm(34, (N_MOE, N_EXPERTS, dm, FFN_EXPERT), dm ** -0.5),
        'moe_w_up': nrm(35, (N_MOE, N_EXPERTS, dm, FFN_EXPERT), dm ** -0.5),
        'moe_w_down': nrm(36, (N_MOE, N_EXPERTS, FFN_EXPERT, dm), beta * FFN_EXPERT ** -0.5),
    }


def reference(x, c, ctx, c_ctx, w_ada, b_ada, w_in, lru_conv_w, lru_conv_b, lru_wa, lru_ba, lru_wx, lru_bx,
              lru_lambda, na_rpb, ssd_conv_w, ssd_conv_b, ssd_dt_bias, ssd_a_log, ssd_d, ssd_norm_g,
              ml_conv_w, ml_conv_b, ml_i_bias, ml_f_bias, w_branch, w_out, ln_g, ln_b,
              ffn_w_gate, ffn_w_up, ffn_w_down, moe_w_router, moe_b_router, moe_w_gate, moe_w_up, moe_w_down):
    alpha = (2.0 * DEPTH) ** 0.25
    cos, sin = rope_2d_tables(x.shape[1], x.dtype)
    c_act = jax.nn.silu(c)[:, None, :]
    cc_act = jax.nn.silu(c_ctx)[None, None, :]
    xl, xc = x, ctx
    for l in range(DEPTH):
        need_ctx_out = l < DEPTH - 1
        mod_l = jnp.split(c_act @ w_ada[l] + b_ada[l], 6, axis=-1)
        mod_c = jnp.split(cc_act @ w_ada[l] + b_ada[l], 6, axis=-1)
        yl, yc = hybrid_mixer(xl * (1.0 + mod_l[1]) + mod_l[0], xc * (1.0 + mod_c[1]) + mod_c[0], cos, sin,
                              need_ctx_out, w_in[l], lru_conv_w[l], lru_conv_b[l], lru_wa[l], lru_ba[l],
                              lru_wx[l], lru_bx[l], lru_lambda[l], na_rpb[l], ssd_conv_w[l], ssd_conv_b[l],
                              ssd_dt_bias[l], ssd_a_log[l], ssd_d[l], ssd_norm_g[l], ml_conv_w[l], ml_conv_b[l],
                              ml_i_bias[l], ml_f_bias[l], w_branch[l], w_out[l])
        xl = layer_norm(alpha * xl + mod_l[2] * yl, ln_g[l, 0], ln_b[l, 0])
        if need_ctx_out:
            xc = layer_norm(alpha * xc + mod_c[2] * yc, ln_g[l, 0], ln_b[l, 0])
        j = l // 2
        if l % 2 == 0:
            ffn = functools.partial(swiglu, w_gate=ffn_w_gate[j], w_up=ffn_w_up[j], w_down=ffn_w_down[j])
        else:
            ffn = functools.partial(moe_swiglu, w_router=moe_w_router[j], b_router=moe_b_router[j],
                                    w_gate=moe_w_gate[j], w_up=moe_w_up[j], w_down=moe_w_down[j])
        xl = layer_norm(alpha * xl + mod_l[5] * ffn(xl * (1.0 + mod_l[4]) + mod_l[3]), ln_g[l, 1], ln_b[l, 1])
        if need_ctx_out:
            xc = layer_norm(alpha * xc + mod_c[5] * ffn(xc * (1.0 + mod_c[4]) + mod_c[3]), ln_g[l, 1], ln_b[l, 1])
    return xl
```

```python
import numpy as np
from contextlib import ExitStack
import concourse.bass as bass
import concourse.mybir as mybir
from concourse.ap import AP

F32 = mybir.dt.float32
BF16 = mybir.dt.bfloat16
AF = mybir.ActivationFunctionType
ALU = mybir.AluOpType
AX = mybir.AxisListType

ENGS = ("pe", "act", "dve", "pool", "sp")
DMA_Q = ("sp", "act", "pool")
NSLOT = 8


class Tile:
    __slots__ = ("ap", "w", "r", "name")

    def __init__(self, ap, name=""):
        self.ap = ap
        self.w = {}
        self.r = {}
        self.name = name

    def __getitem__(self, k):
        return self.ap[k]


class _Rec:
    def __init__(self):
        self.call = None

    def __getattr__(self, name):
        def f(*a, **kw):
            self.call = (name, a, kw)
            return None
        return f


class Sched:
    def __init__(self, nc, es):
        self.nc = nc
        self.es = es
        self.ops = {e: [] for e in ENGS}
        self.sem = {e: es.enter_context(nc.semaphore("s_" + e)) for e in ENGS}
        self.cnt = {e: 0 for e in ENGS}
        self.slots = {q: [es.enter_context(nc.semaphore(f"d_{q}{i}")) for i in range(NSLOT)] for q in DMA_Q}
        self.slot_cnt = {q: [0] * NSLOT for q in DMA_Q}
        self.slot_i = {q: 0 for q in DMA_Q}
        self.bar = es.enter_context(nc.semaphore("bar"))
        self.bar_cnt = 0
        self.seen = {e: {} for e in ENGS}
        self.touched = set()
        self.n_ops = 0

    def _need(self, eng, deps):
        out = []
        seen = self.seen[eng]
        for s, v in deps.items():
            if seen.get(s, 0) < v:
                seen[s] = v
                out.append((s, v))
        return out

    def op(self, eng, fn, reads=(), writes=(), dma=False, acc=False):
        rec = _Rec()
        fn(rec)
        assert rec.call is not None
        name, a, kw = rec.call
        fn = (lambda h, name=name, a=a, kw=kw: getattr(h, name)(*a, **kw))
        deps = {}
        own = self.sem[eng]

        def add(d):
            for s, v in d.items():
                if deps.get(s, 0) < v:
                    deps[s] = v
        for t in reads:
            add(t.w)
        for t in writes:
            if acc and eng == "pe":
                add({s: v for s, v in t.w.items() if s is not own})
            else:
                add(t.w)
            add(t.r)
        if dma:
            q = eng
            i = self.slot_i[q]
            self.slot_i[q] = (i + 1) % NSLOT
            ssem = self.slots[q][i]
            add({ssem: self.slot_cnt[q][i]})
            self.slot_cnt[q][i] += 16
            ticket = (ssem, self.slot_cnt[q][i])
            inc = (ssem, 16)
        else:
            self.cnt[eng] += 1
            ticket = (own, self.cnt[eng])
            inc = (own, 1)
        waits = self._need(eng, deps)
        self.ops[eng].append((waits, fn, inc))
        self.n_ops += 1
        for t in reads:
            if t.r.get(ticket[0], 0) < ticket[1]:
                t.r[ticket[0]] = ticket[1]
            self.touched.add(t)
        for t in writes:
            if acc:
                t.w[ticket[0]] = ticket[1]
            else:
                t.w = {ticket[0]: ticket[1]}
                t.r = {}
            self.touched.add(t)
        return ticket

    def barrier(self):
        deps = {}
        for e in ENGS:
            if self.cnt[e] > 0:
                deps[self.sem[e]] = self.cnt[e]
        for q in DMA_Q:
            for i in range(NSLOT):
                if self.slot_cnt[q][i] > 0:
                    deps[self.slots[q][i]] = self.slot_cnt[q][i]
        waits = self._need("sp", deps)
        self.bar_cnt += 1
        bar, bc = self.bar, self.bar_cnt
        self.ops["sp"].append((waits, lambda e, bar=bar: e.sem_inc(bar, 1), None))
        for e in ENGS:
            if e != "sp":
                self.ops[e].append(([(bar, bc)], None, None))
            for s, v in deps.items():
                if self.seen[e].get(s, 0) < v:
                    self.seen[e][s] = v
        for t in self.touched:
            t.w = {}
            t.r = {}
        self.touched = set()

    def emit(self, block):
        nc = self.nc
        handles = {"pe": "tensor", "act": "scalar", "dve": "vector", "pool": "gpsimd", "sp": "sync"}

        def make(e):
            def body(h):
                for waits, fn, inc in self.ops[e]:
                    for s, v in waits:
                        h.wait_ge(s, v)
                    if fn is not None:
                        ins = fn(h)
                        if inc is not None:
                            ins.then_inc(inc[0], inc[1])
            return body
        for e in ENGS:
            getattr(block, handles[e])(make(e))


class Arena:
    def __init__(self, nc, es, name, cols, dtype):
        self.t = es.enter_context(nc.sbuf_tensor(name, [128, cols], dtype))
        self.cols = cols
        self.off = 0
        self.name = name

    def alloc(self, cols, name="", parts=128):
        assert self.off + cols <= self.cols, f"arena {self.name} overflow {self.off}+{cols}>{self.cols} ({name})"
        ap = self.t[0:parts, self.off:self.off + cols]
        self.off += cols
        return Tile(ap, name)

    def allocn(self, n, cols, name="", parts=128):
        return [self.alloc(cols, f"{name}{i}", parts) for i in range(n)]

    def reset(self, off=0):
        self.off = off


class Ring:
    def __init__(self, tiles):
        self.tiles = tiles
        self.i = 0

    def next(self):
        t = self.tiles[self.i]
        self.i = (self.i + 1) % len(self.tiles)
        return t


import numpy as np
from contextlib import ExitStack
import concourse.bass as bass
import concourse.mybir as mybir
from concourse.ap import AP
from concourse.bass_utils import run_bass_kernel_spmd

D = 2048
KC = 16
CTX = 256
GW = 64
N_IN = 14112
C_LRU_X, C_LRU_G, C_NA_Q, C_NA_K, C_NA_V = 0, 512, 1024, 1536, 2048
C_SSD_Z, C_SSD_X, C_SSD_B, C_SSD_C, C_SSD_DT = 2560, 3072, 3584, 3712, 3840
C_ML_Q, C_ML_K, C_ML_V, C_ML_O, C_ML_G, C_GATE = 3856, 4368, 4880, 5392, 5904, 5920


class Cfg:
    def __init__(self, SEQ, FD, FE, layers=(0, 1), dbg=()):
        self.SEQ, self.FD, self.FE, self.layers, self.dbg = SEQ, FD, FE, tuple(layers), tuple(dbg)
        self.NG = CTX + SEQ
        self.NT = self.NG // 128
        self.ROWS = SEQ // GW


def blocks(lo, hi, step):
    return [(a, min(step, hi - a)) for a in range(lo, hi, step)]


class K:
    def __init__(self, cfg):
        self.cfg = cfg
        self.es = ExitStack()
        nc = self.nc = bass.Bass("TRN2", target_bir_lowering=False)
        self.S = Sched(nc, self.es)
        self.ins = {}
        self.outs = {}
        self.scr = {}
        self.AF = Arena(nc, self.es, "af32", 24 * 1024, F32)
        self.AB = Arena(nc, self.es, "abf16", 55 * 1024, BF16)
        self.ps = [Tile(self.es.enter_context(nc.psum_tensor(f"ps{i}", [128, 512], F32))[:], f"ps{i}") for i in range(8)]
        self.psr = Ring(self.ps)

    def inp(self, name, shape, dt=F32):
        t = self.nc.dram_tensor(name, list(shape), dt, kind="ExternalInput").ap()
        self.ins[name] = t
        return t

    def out(self, name, shape, dt=F32):
        t = self.nc.dram_tensor(name, list(shape), dt, kind="ExternalOutput").ap()
        self.outs[name] = t
        return t

    def dram(self, name, shape, dt=F32):
        if name in self.cfg.dbg:
            return self.out(name, shape, dt)
        t = self.nc.dram_tensor(name, list(shape), dt, kind="Internal").ap()
        self.scr[name] = t
        return t

    def dma(self, q, out, in_, reads=(), writes=(), **kw):
        return self.S.op(q, lambda e: e.dma_start(out=out, in_=in_, **kw), reads=reads, writes=writes, dma=True)

    def phase(self):
        self.S.barrier()
        self.AF.reset(self.af_base)
        self.AB.reset(self.ab_base)

    def build(self):
        cfg = self.cfg
        S, nc = self.S, self.nc
        NG = cfg.NG
        x_in = self.inp("x", [cfg.SEQ, D])
        ctx_in = self.inp("ctx", [CTX, D])
        cT = self.inp("cT", [128, KC, 2])
        ident_in = self.inp("ident", [128, 128])
        w_ada = self.inp("w_ada", [2, D, 6 * D])
        b_ada = self.inp("b_ada", [2, 6 * D])
        w_in = self.inp("w_in", [2, D, N_IN])
        self.xmT = self.dram("xmT", [128, KC, NG], BF16)
        self.modD = self.dram("modD", [2, 2, 6 * D])
        self.ident = self.AF.alloc(128, "ident")
        self.identb = self.AB.alloc(128, "identb")
        self.af_base, self.ab_base = self.AF.off, self.AB.off
        self.dma("sp", self.ident.ap, ident_in, writes=[self.ident])
        S.op("dve", lambda e: e.tensor_copy(out=self.identb.ap, in_=self.ident.ap), reads=[self.ident], writes=[self.identb])
        V = self.V = {}
        V["lru_cw"] = self.inp("lru_cw", [2, 128, 4, 4]); V["lru_cb"] = self.inp("lru_cb", [2, 128, 4]); V["lru_vec"] = self.inp("lru_vec", [2, 128, 6, 4])
        V["lru_wa"] = self.inp("lru_wa", [2, 2, 8, 64, 64]); V["lru_wx"] = self.inp("lru_wx", [2, 2, 8, 64, 64])
        V["ssd_cw"] = self.inp("ssd_cw", [2, 128, 6, 4]); V["ssd_cb"] = self.inp("ssd_cb", [2, 128, 6]); V["ssd_hp"] = self.inp("ssd_hp", [2, 8, 4])
        V["ssd_dskip"] = self.inp("ssd_dskip", [2, 512]); V["ssd_gn"] = self.inp("ssd_gn", [2, 512]); V["tri"] = self.inp("tri", [2, 128, 128]); V["sel40"] = self.inp("sel40", [40, 8, 128])
        V["ml_cw"] = self.inp("ml_cw", [2, 128, 8, 4]); V["ml_cb"] = self.inp("ml_cb", [2, 128, 8]); V["ml_hp"] = self.inp("ml_hp", [2, 4, 4]); V["sel24"] = self.inp("sel24", [24, 4, 128])
        V["rope_cos"] = self.inp("rope_cos", [128, cfg.SEQ]); V["rope_sin"] = self.inp("rope_sin", [128, cfg.SEQ]); V["ropeP"] = self.inp("ropeP", [128, 128])
        V["na_bm"] = self.inp("na_bm", [2, len(na_rowinfo(cfg.ROWS)[1]), 8, 64, 640])
        V["moe_wr_p"] = self.inp("moe_wr_p", [1, 128, KC * 8]); V["moe_br_p"] = self.inp("moe_br_p", [1, 128, 8])
        V["moe_w_gate"] = self.inp("moe_w_gate", [1, 8, D, cfg.FE]); V["moe_w_up"] = self.inp("moe_w_up", [1, 8, D, cfg.FE]); V["moe_w_down"] = self.inp("moe_w_down", [1, 8, cfg.FE, D])
        V["w_branch"] = self.inp("w_branch", [2, 4, 512, D]); V["w_out"] = self.inp("w_out", [2, D, D])
        V["ln_g"] = self.inp("ln_g", [2, 2, D]); V["ln_b"] = self.inp("ln_b", [2, 2, D])
        V["ffn_w_gate"] = self.inp("ffn_w_gate", [1, D, cfg.FD]); V["ffn_w_up"] = self.inp("ffn_w_up", [1, D, cfg.FD]); V["ffn_w_down"] = self.inp("ffn_w_down", [1, cfg.FD, D])
        self.y_out = self.out("y_out", [cfg.SEQ, D])
        self.phase_mod(cT, w_ada, b_ada)
        for l in cfg.layers:
            self.layer(l, x_in, ctx_in, w_in)
        S.barrier()
        with nc.Block() as block:
            S.emit(block)
        return nc

    def phase_mod(self, cT, w_ada, b_ada):
        S = self.S
        self.phase()
        ct = self.AF.alloc(KC * 2, "ct")
        self.dma("sp", ct.ap, cT.rearrange("p k m -> p (k m)"), writes=[ct])
        ca = self.AF.alloc(KC * 2, "ca")
        S.op("act", lambda e: e.activation(out=ca.ap, in_=ct.ap, func=AF.Silu), reads=[ct], writes=[ca])
        cav = ca.ap.rearrange("p (k m) -> p k m", m=2)
        wr = Ring(self.AF.allocn(2, KC * 512, "wada"))
        orow = Ring(self.AF.allocn(2, 512, "orow", parts=2))
        brow = Ring(self.AF.allocn(2, 512, "brow", parts=2))
        for l in self.cfg.layers:
            for nb in range(6 * D // 512):
                wt = wr.next()
                wv = wt.ap.rearrange("p (k n) -> p k n", k=KC)
                self.dma("sp" if nb % 2 == 0 else "act", wv, w_ada[l, :, nb * 512:(nb + 1) * 512].rearrange("(k p) n -> p k n", p=128), writes=[wt])
                bt = brow.next()
                for m in range(2):
                    self.dma("pool", bt.ap[m:m + 1, :], b_ada[l:l + 1, nb * 512:(nb + 1) * 512], writes=[bt])
                ps = self.psr.next()
                for k in range(KC):
                    S.op("pe", lambda e, k=k, wv=wv, ps=ps: e.matmul(ps.ap[0:2, :], lhsT=cav[:, k, :], rhs=wv[:, k, :], start=(k == 0), stop=(k == KC - 1)),
                         reads=[ca, wt], writes=[ps], acc=k > 0)
                ot = orow.next()
                S.op("dve", lambda e, ot=ot, ps=ps, bt=bt: e.tensor_tensor(out=ot.ap, in0=ps.ap[0:2, :], in1=bt.ap, op=ALU.add), reads=[ps, bt], writes=[ot])
                self.dma("sp", self.modD[l, :, nb * 512:(nb + 1) * 512], ot.ap, reads=[ot])

    def bcast_row(self, q, tile, src_row_ap, n):
        self.dma(q, tile.ap[:, 0:n], src_row_ap.to_broadcast([128, n]) if hasattr(src_row_ap, "to_broadcast") else src_row_ap, writes=[tile])

    def phase_xt(self, l, x_src, ctx_src, shift_i, scale_i, router=None):
        S, cfg = self.S, self.cfg
        self.phase()
        rows = {}
        for kind in range(2):
            sc = self.AF.alloc(D, f"sc{kind}")
            sh = self.AF.alloc(D, f"sh{kind}")
            self.dma("sp", sc.ap, self.modD[l, kind:kind + 1, scale_i * D:(scale_i + 1) * D].to_broadcast([128, D]), writes=[sc])
            self.dma("act", sh.ap, self.modD[l, kind:kind + 1, shift_i * D:(shift_i + 1) * D].to_broadcast([128, D]), writes=[sh])
            S.op("dve", lambda e, sc=sc: e.tensor_scalar_add(out=sc.ap, in0=sc.ap, scalar1=1.0), reads=[sc], writes=[sc])
            rows[kind] = (sc, sh)
        xr = Ring(self.AF.allocn(2, D, "xtile"))
        xo = Ring(self.AB.allocn(2, KC * 128, "xmt"))
        if router is not None:
            wr_ap, br_ap = router
            if not hasattr(self, "combT"):
                self.combT = self.dram("combT", [8, cfg.NG])
            WR = self.AF.alloc(KC * 8, "WR")
            self.dma("sp", WR.ap, wr_ap, writes=[WR])
            BR = self.AF.alloc(8, "BR")
            self.dma("sp", BR.ap, br_ap, writes=[BR])
            hr = Ring(self.AF.allocn(2, D, "hT"))
            lr = Ring(self.AF.allocn(2, 40, "lg"))
            cr = Ring(self.AF.allocn(4, 128, "cT", parts=8))
        for ti in range(cfg.NT):
            kind = 1 if ti < CTX // 128 else 0
            src = ctx_src[ti * 128:(ti + 1) * 128, :] if kind == 1 else x_src[ti * 128 - CTX:(ti + 1) * 128 - CTX, :]
            xt = xr.next()
            self.dma("sp" if ti % 2 == 0 else "act", xt.ap, src, writes=[xt])
            sc, sh = rows[kind]
            S.op("dve", lambda e, xt=xt, sc=sc: e.tensor_tensor(out=xt.ap, in0=xt.ap, in1=sc.ap, op=ALU.mult), reads=[xt, sc], writes=[xt])
            S.op("pool", lambda e, xt=xt, sh=sh: e.tensor_tensor(out=xt.ap, in0=xt.ap, in1=sh.ap, op=ALU.add), reads=[xt, sh], writes=[xt])
            ot = xo.next()
            for g in range(4):
                ps = self.psr.next()
                for j in range(4):
                    k = g * 4 + j
                    S.op("pe", lambda e, ps=ps, xt=xt, j=j, k=k: e.transpose(ps.ap[:, j * 128:(j + 1) * 128], xt.ap[:, k * 128:(k + 1) * 128], self.ident.ap),
                         reads=[xt, self.ident], writes=[ps], acc=j > 0)
                if router is not None:
                    if g == 0:
                        HT = hr.next()
                    if g % 2 == 0:
                        S.op("act", lambda e, ps=ps, HT=HT, g=g: e.copy(out=HT.ap[:, g * 512:(g + 1) * 512], in_=ps.ap), reads=[ps], writes=[HT], acc=True)
                    else:
                        S.op("dve", lambda e, ps=ps, HT=HT, g=g: e.tensor_copy(out=HT.ap[:, g * 512:(g + 1) * 512], in_=ps.ap), reads=[ps], writes=[HT], acc=True)
                eng = "act" if g % 2 == 0 else "dve"
                if eng == "act":
                    S.op("act", lambda e, ps=ps, ot=ot, g=g: e.copy(out=ot.ap[:, g * 512:(g + 1) * 512], in_=ps.ap), reads=[ps], writes=[ot], acc=True)
                else:
                    S.op("dve", lambda e, ps=ps, ot=ot, g=g: e.tensor_copy(out=ot.ap[:, g * 512:(g + 1) * 512], in_=ps.ap), reads=[ps], writes=[ot], acc=True)
            self.dma("sp", self.xmT[:, :, ti * 128:(ti + 1) * 128], ot.ap.rearrange("p (k t) -> p k t", k=KC), reads=[ot])
            if router is not None:
                if "r0" in cfg.dbg: continue
                psL = self.psr.next()
                for k in range(KC):
                    S.op("pe", lambda e, psL=psL, HT=HT, k=k: e.matmul(psL.ap[0:8, 0:128], lhsT=WR.ap[:, k * 8:(k + 1) * 8], rhs=HT.ap[:, k * 128:(k + 1) * 128], start=(k == 0), stop=(k == KC - 1)), reads=[HT, WR], writes=[psL], acc=k > 0)
                if "r05" in cfg.dbg: continue
                LT = cr.next()
                S.op("act", lambda e: e.copy(out=LT.ap, in_=psL.ap[0:8, 0:128]), reads=[psL], writes=[LT])
                psL = self.psr.next()
                S.op("pe", lambda e: e.transpose(psL.ap[:, 0:8], LT.ap, self.ident.ap[0:8, 0:8]), reads=[LT, self.ident], writes=[psL])
                if "r1" in cfg.dbg: continue
                Lg = lr.next()
                L = Lg.ap[:, 0:8]; E1 = Lg.ap[:, 8:16]; L2 = Lg.ap[:, 16:24]; Pm = Lg.ap[:, 24:32]; sc_ = Lg.ap[:, 32:40]
                S.op("dve", lambda e: e.tensor_tensor(out=L, in0=psL.ap[:, 0:8], in1=BR.ap, op=ALU.add), reads=[psL, BR], writes=[Lg])
                S.op("dve", lambda e: e.reduce_max(out=sc_[:, 0:1], in_=L, axis=AX.X), reads=[Lg], writes=[Lg])
                S.op("dve", lambda e: e.tensor_scalar(out=E1, in0=L, scalar1=sc_[:, 0:1], scalar2=None, op0=ALU.is_equal), reads=[Lg], writes=[Lg])
                S.op("dve", lambda e: e.scalar_tensor_tensor(out=L2, in0=E1, scalar=-1e30, in1=L, op0=ALU.mult, op1=ALU.add), reads=[Lg], writes=[Lg])
                S.op("dve", lambda e: e.reduce_max(out=sc_[:, 1:2], in_=L2, axis=AX.X), reads=[Lg], writes=[Lg])
                S.op("dve", lambda e: e.tensor_scalar(out=E1, in0=L, scalar1=sc_[:, 1:2], scalar2=None, op0=ALU.is_ge), reads=[Lg], writes=[Lg])
                S.op("dve", lambda e: e.tensor_scalar_mul(out=sc_[:, 2:3], in0=sc_[:, 0:1], scalar1=-1.0), reads=[Lg], writes=[Lg])
                S.op("act", lambda e: e.activation(out=Pm, in_=L, func=AF.Exp, bias=sc_[:, 2:3]), reads=[Lg], writes=[Lg])
                S.op("dve", lambda e: e.tensor_tensor(out=Pm, in0=Pm, in1=E1, op=ALU.mult), reads=[Lg], writes=[Lg])
                S.op("dve", lambda e: e.reduce_sum(out=sc_[:, 3:4], in_=Pm, axis=AX.X), reads=[Lg], writes=[Lg])
                S.op("dve", lambda e: e.reciprocal(out=sc_[:, 4:5], in_=sc_[:, 3:4]), reads=[Lg], writes=[Lg])
                S.op("dve", lambda e: e.tensor_scalar_mul(out=Pm, in0=Pm, scalar1=sc_[:, 4:5]), reads=[Lg], writes=[Lg])
                if "r2" in cfg.dbg: continue
                psT = self.psr.next()
                S.op("pe", lambda e: e.transpose(psT.ap[0:8, 0:128], Pm, self.ident.ap), reads=[Lg, self.ident], writes=[psT])
                CT = cr.next()
                S.op("act", lambda e: e.copy(out=CT.ap, in_=psT.ap[0:8, 0:128]), reads=[psT], writes=[CT])
                self.dma("act", self.combT[:, ti * 128:(ti + 1) * 128], CT.ap, reads=[CT])

    def proj_fm(self, W, c0, ncols, tok_blocks, sink, wring, xring):
        S = self.S
        wt = wring.next()
        wv = wt.ap.rearrange("p (k n) -> p k n", k=KC)
        self.dma("pool", wv[:, :, 0:ncols], W[:, c0:c0 + ncols].rearrange("(k p) n -> p k n", p=128), writes=[wt])
        chunks = blocks(0, ncols, 128)
        for (g0, nb) in tok_blocks:
            xb = xring.next()
            xv = xb.ap.rearrange("p (k t) -> p k t", k=KC)
            self.dma("sp", xv[:, :, 0:nb], self.xmT[:, :, g0:g0 + nb], writes=[xb])
            for ci, (cc0, cn) in enumerate(chunks):
                ps = self.psr.next()
                for k in range(KC):
                    S.op("pe", lambda e, ps=ps, wv=wv, xv=xv, k=k, cc0=cc0, cn=cn, nb=nb: e.matmul(ps.ap[0:cn, 0:nb], lhsT=wv[:, k, cc0:cc0 + cn], rhs=xv[:, k, 0:nb], start=(k == 0), stop=(k == KC - 1)),
                         reads=[wt, xb], writes=[ps], acc=k > 0)
                sink(ci, c0 + cc0, cn, g0, nb, ps)

    def layer(self, l, x_in, ctx_in, w_in):
        cfg, S = self.cfg, self.S
        NG = cfg.NG
        if l == cfg.layers[0]:
            self.phase_xt(l, x_in, ctx_in, 0, 1)
        else:
            self.phase_xt(l, self.xres[1][CTX:NG, :], self.xres[1][0:CTX, :], 0, 1)
        self.phase()
        W = w_in[l]
        if not hasattr(self, "pfm"):
            self.pfm = self.dram("pfm", [N_IN - 8192, NG])
        wring = Ring(self.AB.allocn(2, KC * 512, "wt"))
        xring = Ring(self.AB.allocn(2, KC * 512, "xb"))
        oring = Ring(self.AF.allocn(4, 512, "ost"))
        cnt = [0]

        def sink(ci, col, cn, g0, nb, ps):
            ot = oring.next()
            if cnt[0] % 2 == 0:
                S.op("act", lambda e: e.copy(out=ot.ap[0:cn, 0:nb], in_=ps.ap[0:cn, 0:nb]), reads=[ps], writes=[ot])
            else:
                S.op("dve", lambda e: e.tensor_copy(out=ot.ap[0:cn, 0:nb], in_=ps.ap[0:cn, 0:nb]), reads=[ps], writes=[ot])
            cnt[0] += 1
            self.dma("act", self.pfm[col:col + cn, g0:g0 + nb], ot.ap[0:cn, 0:nb], reads=[ot])
        tb = blocks(0, NG, 512)
        for c0 in range(0, C_GATE, 512):
            self.proj_fm(W, c0, min(512, C_GATE - c0), tb, sink, wring, xring)
        V = self.V
        if "inj" in cfg.dbg:
            if not hasattr(self, "yT"):
                self.yT = self.inp("yT_in", [4, 512, NG], BF16)
        else:
            if "nolru" not in cfg.dbg:
                self.lru_phase(l, V)
            elif not hasattr(self, "yT"):
                self.yT = self.dram("yT", [4, 512, NG], BF16)
            if "nossd" not in cfg.dbg:
                self.ssd_phase(l, V)
            if "noml" not in cfg.dbg:
                self.mlstm_phase(l, V)
            zt = []
            if "nolru" in cfg.dbg: zt.append(self.yT[0])
            if "nossd" in cfg.dbg: zt.append(self.yT[2])
            if "noml" in cfg.dbg: zt.append(self.yT[3])
            if "nona" not in cfg.dbg:
                self.na_phase(l, V)
            else:
                zt.append(self.yT[1])
            if zt:
                self.zero_fill_phase(zt)
        if "stopmix" in cfg.dbg:
            return
        if not hasattr(self, "xres"):
            self.xres = [self.dram("xres0", [NG, D]), self.dram("xres1", [NG, D])]
        self.merge_phase(l, W, V["w_branch"][l], V["w_out"][l])
        xs, cs = (x_in, ctx_in) if l == cfg.layers[0] else (self.xres[1][CTX:NG, :], self.xres[1][0:CTX, :])
        self.epi_phase(l, xs, cs, 2, 0, self.xres[0], V["ln_g"], V["ln_b"])
        self.phase_xt(l, self.xres[0][CTX:NG, :], self.xres[0][0:CTX, :], 3, 4, router=(V["moe_wr_p"][l // 2], V["moe_br_p"][l // 2]) if (l % 2 == 1 and "norouter" not in cfg.dbg) else None)
        if "stopxt2" in cfg.dbg and l % 2 == 1:
            return
        if l % 2 == 0:
            self.ffn_dense_phase(l, V["ffn_w_gate"][l // 2], V["ffn_w_up"][l // 2], V["ffn_w_down"][l // 2], cfg.FD)
        else:
            self.moe_phase(l, V)
        if "stopxt2" in cfg.dbg and l % 2 == 1:
            return
        last = (l == cfg.layers[-1])
        self.epi_phase(l, self.xres[0][CTX:NG, :], self.xres[0][0:CTX, :], 5, 1, self.xres[1], V["ln_g"], V["ln_b"], out_ext=self.y_out if last else None)


def rev_ap(ap2d, n):
    return AP(ap2d.tensor, ap2d.offset + (n - 1) * ap2d.ap[-1][0], [list(ap2d.ap[0]), [-ap2d.ap[-1][0], n]])


def _segments(cfg):
    return [(0, CTX), (CTX, cfg.NG)]


def lru_phase(self, l, V):
    cfg, S = self.cfg, self.S
    NG = cfg.NG
    NB = 1024
    if not hasattr(self, "hD"):
        self.hD = self.dram("hD", [2, 512, NG])
        self.yT = self.dram("yT", [4, 512, NG], BF16)
    self.phase()
    AFa, ABa = self.AF, self.AB
    cw = AFa.alloc(16, "cw"); self.dma("sp", cw.ap, V["lru_cw"][l].rearrange("p c k -> p (c k)"), writes=[cw])
    cb = AFa.alloc(4, "cb"); self.dma("sp", cb.ap, V["lru_cb"][l], writes=[cb])
    pv = AFa.alloc(24, "pv"); self.dma("sp", pv.ap, V["lru_vec"][l].rearrange("p a c -> p (a c)"), writes=[pv])
    spc = AFa.alloc(8, "spc")
    S.op("act", lambda e: e.activation(out=spc.ap, in_=pv.ap[:, 16:24], func=AF.Exp, scale=-1.0), reads=[pv], writes=[spc])
    S.op("act", lambda e: e.activation(out=spc.ap, in_=spc.ap, func=AF.Ln, bias=1.0), reads=[spc], writes=[spc])
    S.op("dve", lambda e: e.tensor_scalar_mul(out=spc.ap, in0=spc.ap, scalar1=-8.0), reads=[spc], writes=[spc])
    wbd = {}
    for d in range(2):
        for wi, nm in enumerate(("lru_wa", "lru_wx")):
            for c in range(4):
                t = ABa.alloc(128, f"wbd{d}{wi}{c}")
                S.op("pool", lambda e, t=t: e.memset(t.ap, 0.0), writes=[t])
                for hblk in range(2):
                    self.dma("pool", t.ap[hblk * 64:(hblk + 1) * 64, hblk * 64:(hblk + 1) * 64], V[nm][l, d, 2 * c + hblk], writes=[t], reads=[t])
                wbd[(d, wi, c)] = t
    zero1 = AFa.alloc(1, "zero1")
    S.op("dve", lambda e: e.memset(zero1.ap, 0.0), writes=[zero1])
    names = ("XH", "XC", "R", "I", "A", "U", "H")
    rings = {n: Ring(AFa.allocn(2, NB + 4, n)) for n in names}
    xbr = Ring(ABa.allocn(2, NB, "XB"))
    carr = Ring(AFa.allocn(4, 1, "carry"))
    for c in range(4):
        for d in range(2):
            carry = zero1
            for (s0, s1) in _segments(cfg):
                blks = blocks(s0, s1, NB)
                if d == 1:
                    blks = blks[::-1]
                for (g0, nb) in blks:
                    XH, XC, R, I, A, U, H = [rings[n].next() for n in names]
                    XB = xbr.next()
                    lo = max(g0 - 2, s0); hi = min(g0 + nb + 1, s1)
                    S.op("pool", lambda e, XH=XH: e.memset(XH.ap[:, 0:NB + 4], 0.0), writes=[XH])
                    self.dma("sp", XH.ap[:, lo - (g0 - 2):hi - (g0 - 2)], self.pfm[c * 128:(c + 1) * 128, lo:hi], writes=[XH], reads=[XH])
                    S.op("dve", lambda e, XH=XH, XC=XC, nb=nb, c=c: e.tensor_scalar(out=XC.ap[:, 0:nb], in0=XH.ap[:, 0:nb], scalar1=cw.ap[:, c * 4:c * 4 + 1], scalar2=cb.ap[:, c:c + 1], op0=ALU.mult, op1=ALU.add), reads=[XH, cw, cb], writes=[XC])
                    for k in range(1, 4):
                        S.op("dve", lambda e, XH=XH, XC=XC, nb=nb, c=c, k=k: e.scalar_tensor_tensor(out=XC.ap[:, 0:nb], in0=XH.ap[:, k:k + nb], scalar=cw.ap[:, c * 4 + k:c * 4 + k + 1], in1=XC.ap[:, 0:nb], op0=ALU.mult, op1=ALU.add), reads=[XH, XC, cw], writes=[XC])
                    S.op("act", lambda e, XB=XB, XC=XC, nb=nb: e.copy(out=XB.ap[:, 0:nb], in_=XC.ap[:, 0:nb]), reads=[XC], writes=[XB])
                    for wi, (dst, bcol) in enumerate(((R, 0 + d), (I, 2 + d))):
                        for (o, n) in blocks(0, nb, 512):
                            ps = self.psr.next()
                            w = wbd[(d, wi, c)]
                            S.op("pe", lambda e, ps=ps, w=w, XB=XB, o=o, n=n: e.matmul(ps.ap[:, 0:n], lhsT=w.ap, rhs=XB.ap[:, o:o + n], start=True, stop=True), reads=[w, XB], writes=[ps])
                            S.op("act", lambda e, ps=ps, dst=dst, o=o, n=n, bcol=bcol, c=c: e.activation(out=dst.ap[:, o:o + n], in_=ps.ap[:, 0:n], func=AF.Sigmoid, bias=pv.ap[:, bcol * 4 + c:bcol * 4 + c + 1]), reads=[ps, pv], writes=[dst], acc=True)
                    S.op("act", lambda e, A=A, R=R, nb=nb, c=c, d=d: e.activation(out=A.ap[:, 0:nb], in_=R.ap[:, 0:nb], func=AF.Exp, scale=spc.ap[:, d * 4 + c:d * 4 + c + 1]), reads=[R, spc], writes=[A])
                    S.op("dve", lambda e, A=A, R=R, nb=nb: e.tensor_tensor(out=R.ap[:, 0:nb], in0=A.ap[:, 0:nb], in1=A.ap[:, 0:nb], op=ALU.mult), reads=[A], writes=[R])
                    S.op("act", lambda e, R=R, nb=nb: e.activation(out=R.ap[:, 0:nb], in_=R.ap[:, 0:nb], func=AF.Sqrt, scale=-1.0, bias=1.0), reads=[R], writes=[R])
                    S.op("dve", lambda e, U=U, R=R, I=I, nb=nb: e.tensor_tensor(out=U.ap[:, 0:nb], in0=R.ap[:, 0:nb], in1=I.ap[:, 0:nb], op=ALU.mult), reads=[R, I], writes=[U])
                    S.op("dve", lambda e, U=U, XC=XC, nb=nb: e.tensor_tensor(out=U.ap[:, 0:nb], in0=U.ap[:, 0:nb], in1=XC.ap[:, 0:nb], op=ALU.mult), reads=[U, XC], writes=[U])
                    f = (lambda a, nb=nb: a[:, 0:nb]) if d == 0 else (lambda a, nb=nb: rev_ap(a[:, 0:nb], nb))
                    S.op("dve", lambda e, H=H, A=A, U=U, f=f, carry=carry: e.tensor_tensor_scan(out=f(H.ap), data0=f(A.ap), data1=f(U.ap), initial=carry.ap[:, 0:1], op0=ALU.mult, op1=ALU.add), reads=[A, U, carry], writes=[H])
                    ncar = carr.next()
                    lastcol = nb - 1 if d == 0 else 0
                    S.op("act", lambda e, ncar=ncar, H=H, lastcol=lastcol: e.copy(out=ncar.ap, in_=H.ap[:, lastcol:lastcol + 1]), reads=[H], writes=[ncar])
                    carry = ncar
                    self.dma("act", self.hD[d, c * 128:(c + 1) * 128, g0:g0 + nb], H.ap[:, 0:nb], reads=[H])
    self.phase()
    rr = {n: Ring(self.AF.allocn(2, NB, n)) for n in ("H0", "H1", "G", "T")}
    yo = Ring(self.AB.allocn(2, NB, "Y"))
    for c in range(4):
        for (g0, nb) in blocks(0, NG, NB):
            H0, H1, G, T = [rr[n].next() for n in ("H0", "H1", "G", "T")]
            Y = yo.next()
            self.dma("sp", H0.ap[:, 0:nb], self.hD[0, c * 128:(c + 1) * 128, g0:g0 + nb], writes=[H0])
            self.dma("act", H1.ap[:, 0:nb], self.hD[1, c * 128:(c + 1) * 128, g0:g0 + nb], writes=[H1])
            self.dma("sp", G.ap[:, 0:nb], self.pfm[C_LRU_G + c * 128:C_LRU_G + (c + 1) * 128, g0:g0 + nb], writes=[G])
            S.op("pool", lambda e, H0=H0, H1=H1, nb=nb: e.tensor_tensor(out=H0.ap[:, 0:nb], in0=H0.ap[:, 0:nb], in1=H1.ap[:, 0:nb], op=ALU.add), reads=[H0, H1], writes=[H0])
            S.op("dve", lambda e, T=T, G=G, nb=nb: e.tensor_tensor(out=T.ap[:, 0:nb], in0=G.ap[:, 0:nb], in1=G.ap[:, 0:nb], op=ALU.mult), reads=[G], writes=[T])
            S.op("dve", lambda e, T=T, nb=nb: e.tensor_scalar(out=T.ap[:, 0:nb], in0=T.ap[:, 0:nb], scalar1=0.044715, scalar2=1.0, op0=ALU.mult, op1=ALU.add), reads=[T], writes=[T])
            S.op("dve", lambda e, T=T, G=G, nb=nb: e.tensor_tensor(out=T.ap[:, 0:nb], in0=T.ap[:, 0:nb], in1=G.ap[:, 0:nb], op=ALU.mult), reads=[T, G], writes=[T])
            S.op("act", lambda e, T=T, nb=nb: e.activation(out=T.ap[:, 0:nb], in_=T.ap[:, 0:nb], func=AF.Sigmoid, scale=1.5957691216057308), reads=[T], writes=[T])
            S.op("dve", lambda e, T=T, G=G, nb=nb: e.tensor_tensor(out=T.ap[:, 0:nb], in0=T.ap[:, 0:nb], in1=G.ap[:, 0:nb], op=ALU.mult), reads=[T, G], writes=[T])
            S.op("dve", lambda e, T=T, H0=H0, Y=Y, nb=nb: e.tensor_tensor(out=Y.ap[:, 0:nb], in0=T.ap[:, 0:nb], in1=H0.ap[:, 0:nb], op=ALU.mult), reads=[T, H0], writes=[Y])
            self.dma("sp", self.yT[0, c * 128:(c + 1) * 128, g0:g0 + nb], Y.ap[:, 0:nb], reads=[Y])


K.lru_phase = lru_phase


def conv_silu_phase(self, l, row0, nchunks, cwv, cbv, dst, post=None, out_f32=False):
    cfg, S = self.cfg, self.S
    NB = 1024
    self.phase()
    cw = self.AF.alloc(nchunks * 4, "cw"); self.dma("sp", cw.ap, cwv.rearrange("p c k -> p (c k)"), writes=[cw])
    cb = self.AF.alloc(nchunks, "cb"); self.dma("sp", cb.ap, cbv, writes=[cb])
    rXH = Ring(self.AF.allocn(2, NB + 4, "XH")); rXC = Ring(self.AF.allocn(2, NB, "XC")); rSG = Ring(self.AF.allocn(2, NB, "SG"))
    rO = Ring((self.AF if out_f32 else self.AB).allocn(2, NB, "O"))
    ctxp = dict(rings={})
    for c in range(nchunks):
        for (s0, s1) in _segments(cfg):
            for (g0, nb) in blocks(s0, s1, NB):
                XH, XC, SG, O = rXH.next(), rXC.next(), rSG.next(), rO.next()
                lo = max(g0 - 2, s0); hi = min(g0 + nb + 1, s1)
                S.op("pool", lambda e, XH=XH: e.memset(XH.ap[:, 0:NB + 4], 0.0), writes=[XH])
                self.dma("sp", XH.ap[:, lo - (g0 - 2):hi - (g0 - 2)], self.pfm[row0 + c * 128:row0 + (c + 1) * 128, lo:hi], writes=[XH], reads=[XH])
                S.op("dve", lambda e, XH=XH, XC=XC, nb=nb, c=c: e.tensor_scalar(out=XC.ap[:, 0:nb], in0=XH.ap[:, 0:nb], scalar1=cw.ap[:, c * 4:c * 4 + 1], scalar2=cb.ap[:, c:c + 1], op0=ALU.mult, op1=ALU.add), reads=[XH, cw, cb], writes=[XC])
                for k in range(1, 4):
                    S.op("dve", lambda e, XH=XH, XC=XC, nb=nb, c=c, k=k: e.scalar_tensor_tensor(out=XC.ap[:, 0:nb], in0=XH.ap[:, k:k + nb], scalar=cw.ap[:, c * 4 + k:c * 4 + k + 1], in1=XC.ap[:, 0:nb], op0=ALU.mult, op1=ALU.add), reads=[XH, XC, cw], writes=[XC])
                if post is None:
                    S.op("act", lambda e, O=O, XC=XC, nb=nb: e.activation(out=O.ap[:, 0:nb], in_=XC.ap[:, 0:nb], func=AF.Silu), reads=[XC], writes=[O])
                else:
                    S.op("act", lambda e, SG=SG, XC=XC, nb=nb: e.activation(out=SG.ap[:, 0:nb], in_=XC.ap[:, 0:nb], func=AF.Silu), reads=[XC], writes=[SG])
                    post(c, s0, g0, nb, SG, O, ctxp)
                self.dma("act", dst[c * 128:(c + 1) * 128, g0:g0 + nb], O.ap[:, 0:nb], reads=[O])


def softplus_ops(self, X, T, n, parts):
    S = self.S
    x = X.ap[0:parts, 0:n]; t = T.ap[0:parts, 0:n]
    S.op("act", lambda e: e.activation(out=t, in_=x, func=AF.Abs), reads=[X], writes=[T])
    S.op("act", lambda e: e.activation(out=t, in_=t, func=AF.Exp, scale=-1.0), reads=[T], writes=[T])
    S.op("act", lambda e: e.activation(out=t, in_=t, func=AF.Ln, bias=1.0), reads=[T], writes=[T])
    S.op("dve", lambda e: e.tensor_scalar_max(out=x, in0=x, scalar1=0.0), reads=[X], writes=[X])
    S.op("dve", lambda e: e.tensor_tensor(out=x, in0=x, in1=t, op=ALU.add), reads=[X, T], writes=[X])


def ssd_tab_phase(self, l, V):
    cfg, S = self.cfg, self.S
    NG = cfg.NG
    NB = 1024
    if not hasattr(self, "tabS"):
        self.tabS = self.dram("tabS", [2, 5, 8, NG])
    self.phase()
    pr = self.AF.alloc(4, "ssdp", parts=8)
    self.dma("sp", pr.ap, V["ssd_hp"][l], writes=[pr])
    Aneg = self.AF.alloc(2, "Aneg", parts=8)
    S.op("act", lambda e: e.activation(out=Aneg.ap, in_=pr.ap[:, 2:4], func=AF.Exp), reads=[pr], writes=[Aneg])
    S.op("dve", lambda e: e.tensor_scalar_mul(out=Aneg.ap, in0=Aneg.ap, scalar1=-1.0), reads=[Aneg], writes=[Aneg])
    msk = {}
    for d in range(2):
        m = self.AF.alloc(NB, f"rmask{d}", parts=8)
        S.op("dve", lambda e, m=m: e.memset(m.ap, 1.0), writes=[m])
        mv = m.ap.rearrange("p (c t) -> p c t", t=128)
        pos = 0 if d == 0 else 127
        S.op("dve", lambda e, mv=mv, pos=pos: e.memset(mv[:, :, pos:pos + 1], 0.0), writes=[m], reads=[m])
        msk[d] = m
    names = ("DT", "T", "DA", "CUM", "LAST", "W", "E", "DL")
    rr = {n: Ring(self.AF.allocn(2, NB, n, parts=8)) for n in names}
    for d in range(2):
        for (g0, nb) in blocks(0, NG, NB):
            DT, T, DA, CUM, LAST, W, E, DL = [rr[n].next() for n in names]
            sl = lambda X: X.ap[:, 0:nb]
            self.dma("sp", sl(DT), self.pfm[C_SSD_DT + d * 8:C_SSD_DT + d * 8 + 8, g0:g0 + nb], writes=[DT])
            S.op("dve", lambda e, DT=DT: e.tensor_scalar_add(out=DT.ap[:, 0:nb], in0=DT.ap[:, 0:nb], scalar1=pr.ap[:, d:d + 1]), reads=[DT, pr], writes=[DT])
            softplus_ops(self, DT, T, nb, 8)
            S.op("dve", lambda e, DA=DA, DT=DT: e.tensor_scalar_mul(out=DA.ap[:, 0:nb], in0=DT.ap[:, 0:nb], scalar1=Aneg.ap[:, d:d + 1]), reads=[DT, Aneg], writes=[DA])
            f = (lambda a: a) if d == 0 else (lambda a: rev_ap(a, nb))
            S.op("dve", lambda e, CUM=CUM, DA=DA, f=f, m=msk[d]: e.tensor_tensor_scan(out=f(CUM.ap[:, 0:nb]), data0=f(m.ap[:, 0:nb]), data1=f(DA.ap[:, 0:nb]), initial=0.0, op0=ALU.mult, op1=ALU.add), reads=[DA, msk[d]], writes=[CUM])
            endpos = 127 if d == 0 else 0
            cv = CUM.ap[:, 0:nb].rearrange("p (c t) -> p c t", t=128)
            lv = LAST.ap[:, 0:nb].rearrange("p (c t) -> p c t", t=128)
            S.op("dve", lambda e, cv=cv, lv=lv, CUM=CUM, LAST=LAST: e.tensor_copy(out=lv, in_=cv[:, :, endpos:endpos + 1].to_broadcast([8, nb // 128, 128])), reads=[CUM], writes=[LAST])
            S.op("dve", lambda e, W=W, LAST=LAST, CUM=CUM: e.tensor_tensor(out=W.ap[:, 0:nb], in0=LAST.ap[:, 0:nb], in1=CUM.ap[:, 0:nb], op=ALU.subtract), reads=[LAST, CUM], writes=[W])
            S.op("act", lambda e, W=W: e.activation(out=W.ap[:, 0:nb], in_=W.ap[:, 0:nb], func=AF.Exp), reads=[W], writes=[W])
            S.op("dve", lambda e, W=W, DT=DT: e.tensor_tensor(out=W.ap[:, 0:nb], in0=W.ap[:, 0:nb], in1=DT.ap[:, 0:nb], op=ALU.mult), reads=[W, DT], writes=[W])
            S.op("act", lambda e, E=E, CUM=CUM: e.activation(out=E.ap[:, 0:nb], in_=CUM.ap[:, 0:nb], func=AF.Exp), reads=[CUM], writes=[E])
            S.op("act", lambda e, DL=DL, LAST=LAST: e.activation(out=DL.ap[:, 0:nb], in_=LAST.ap[:, 0:nb], func=AF.Exp), reads=[LAST], writes=[DL])
            for qi, X in enumerate((CUM, DT, W, E, DL)):
                self.dma("sp" if qi % 2 else "act", self.tabS[d, qi, :, g0:g0 + nb], X.ap[:, 0:nb], reads=[X])


def chunk_order(cfg, d):
    nctx = CTX // 128
    a = list(range(0, nctx)); b = list(range(nctx, cfg.NT))
    return a + b if d == 0 else a[::-1] + b[::-1]


def ssd_main_phase(self, l, V):
    cfg, S = self.cfg, self.S
    NG = cfg.NG
    if not hasattr(self, "ytm"):
        self.ytm = self.dram("ytm", [NG, 512])
    for d in range(2):
        self.phase()
        AFa, ABa = self.AF, self.AB
        tri = AFa.alloc(128, "tri"); self.dma("sp", tri.ap, V["tri"][d], writes=[tri])
        sel = AFa.alloc(8 * 128, "sel", parts=40); self.dma("sp", sel.ap, V["sel40"].rearrange("r h p -> r (h p)"), writes=[sel])
        dsk = AFa.alloc(512, "dsk"); self.dma("sp", dsk.ap, V["ssd_dskip"][l:l + 1, :].to_broadcast([128, 512]), writes=[dsk])
        gn = AFa.alloc(512, "gn"); self.dma("sp", gn.ap, V["ssd_gn"][l:l + 1, :].to_broadcast([128, 512]), writes=[gn])
        H = AFa.alloc(512, "H"); S.op("dve", lambda e: e.memset(H.ap, 0.0), writes=[H])
        Hb = ABa.alloc(512, "Hb"); S.op("dve", lambda e: e.memset(Hb.ap, 0.0), writes=[Hb])
        rXB = Ring(ABa.allocn(2, 6 * 128, "xbc")); rTAB = Ring(AFa.allocn(2, 128, "tab", parts=40)); rTT = Ring(AFa.allocn(2, 40, "TT"))
        rXT = Ring(ABa.allocn(2, 512, "Xtm")); rBT = Ring(ABa.allocn(2, 128, "Btm")); rXW = Ring(ABa.allocn(2, 512, "XW"))
        rDm = Ring(AFa.allocn(3, 128, "Dm")); rMT = Ring(ABa.allocn(3, 128, "MT"))
        rY = Ring(AFa.allocn(2, 512, "Y")); rY2 = Ring(AFa.allocn(2, 512, "Y2")); rZ = Ring(AFa.allocn(2, 512, "Z")); rZs = Ring(AFa.allocn(2, 512, "Zs"))
        rS1 = Ring(AFa.allocn(4, 2, "s1")); rYo = Ring(ABa.allocn(2, 512, "Yo"))
        col = lambda q, h: q * 8 + h
        psq = Ring(self.ps[4:8])
        for c in chunk_order(cfg, d):
            g0 = c * 128
            XB = rXB.next(); TAB = rTAB.next(); TT = rTT.next(); XT = rXT.next(); BT = rBT.next(); XW = rXW.next()
            xv = XB.ap.rearrange("p (c t) -> p c t", c=6)
            self.dma("sp", xv, self.xbcT[:, g0:g0 + 128].rearrange("(c p) t -> p c t", p=128), writes=[XB])
            self.dma("act", TAB.ap, self.tabS[d, :, :, g0:g0 + 128].rearrange("q h t -> (q h) t"), writes=[TAB])
            if "m0" in cfg.dbg: continue
            ps = psq.next()
            S.op("pe", lambda e, ps=ps, TAB=TAB: e.transpose(ps.ap[:, 0:40], TAB.ap, self.ident.ap[0:40, 0:40]), reads=[TAB, self.ident], writes=[ps])
            S.op("act", lambda e, ps=ps, TT=TT: e.copy(out=TT.ap, in_=ps.ap[:, 0:40]), reads=[ps], writes=[TT])
            if "m05" in cfg.dbg: continue
            ps = psq.next()
            pb = ps.ap.bitcast(BF16)
            for j in range(5):
                S.op("pe", lambda e, pb=pb, xv=xv, j=j: e.transpose(pb[:, j * 128:(j + 1) * 128], xv[:, j, :], self.identb.ap), reads=[XB, self.identb], writes=[ps], acc=j > 0)
            S.op("act", lambda e, pb=pb, XT=XT: e.copy(out=XT.ap, in_=pb[:, 0:512]), reads=[ps], writes=[XT])
            S.op("act", lambda e, pb=pb, BT=BT: e.copy(out=BT.ap, in_=pb[:, 512:640]), reads=[ps], writes=[BT])
            if "m1" in cfg.dbg: continue
            xt3 = XT.ap.rearrange("p (h q) -> p h q", h=8)
            S.op("dve", lambda e, XW=XW, xt3=xt3, TT=TT: e.tensor_tensor(out=XW.ap.rearrange("p (h q) -> p h q", h=8), in0=xt3, in1=TT.ap[:, col(2, 0):col(2, 0) + 8].unsqueeze(2).to_broadcast([128, 8, 64]), op=ALU.mult), reads=[XT, TT], writes=[XW])
            if "m15" in cfg.dbg: continue
            psI = self.ps[0]
            S.op("pe", lambda e, psI=psI, xv=xv: e.matmul(psI.ap, lhsT=xv[:, 5, :], rhs=Hb.ap, start=True, stop=True), reads=[XB, Hb], writes=[psI])
            if "m17" in cfg.dbg: continue
            psCs = [self.ps[1], self.ps[2]]
            for g in range(2):
                S.op("pe", lambda e, psC=psCs[g], xv=xv, g=g: e.matmul(psC.ap[:, 0:128], lhsT=xv[g * 64:(g + 1) * 64, 4, :], rhs=xv[g * 64:(g + 1) * 64, 5, :], start=True, stop=True), reads=[XB], writes=[psCs[g]])
            if "m2" in cfg.dbg: continue
            psY = self.ps[3]
            for h in range(8):
                g = h // 4
                psR = psq.next()
                S.op("pe", lambda e, psR=psR, TAB=TAB, h=h: e.matmul(psR.ap[:, 0:128], lhsT=sel.ap[:, h * 128:(h + 1) * 128], rhs=TAB.ap, start=True, stop=True), reads=[sel, TAB], writes=[psR])
                Dm = rDm.next(); MT = rMT.next()
                S.op("dve", lambda e, Dm=Dm, psR=psR, TT=TT, h=h: e.tensor_scalar(out=Dm.ap, in0=psR.ap[:, 0:128], scalar1=TT.ap[:, col(0, h):col(0, h) + 1], scalar2=0.0, op0=ALU.subtract, op1=ALU.min), reads=[psR, TT], writes=[Dm])
                S.op("act", lambda e, Dm=Dm: e.activation(out=Dm.ap, in_=Dm.ap, func=AF.Exp), reads=[Dm], writes=[Dm])
                S.op("dve", lambda e, Dm=Dm, TT=TT, h=h: e.scalar_tensor_tensor(out=Dm.ap, in0=Dm.ap, scalar=TT.ap[:, col(1, h):col(1, h) + 1], in1=tri.ap, op0=ALU.mult, op1=ALU.mult), reads=[Dm, TT, tri], writes=[Dm])
                S.op("dve", lambda e, MT=MT, Dm=Dm, psC=psCs[g]: e.tensor_tensor(out=MT.ap, in0=Dm.ap, in1=psC.ap[:, 0:128], op=ALU.mult), reads=[Dm, psCs[g]], writes=[MT])
                S.op("pe", lambda e, psY=psY, MT=MT, XT=XT, h=h: e.matmul(psY.ap[:, h * 64:(h + 1) * 64], lhsT=MT.ap, rhs=XT.ap[:, h * 64:(h + 1) * 64], start=True, stop=True), reads=[MT, XT], writes=[psY], acc=h > 0)
            if "m3" in cfg.dbg: continue
            psS = psq.next()
            S.op("pe", lambda e, psS=psS, BT=BT, XW=XW: e.matmul(psS.ap, lhsT=BT.ap, rhs=XW.ap, start=True, stop=True), reads=[BT, XW], writes=[psS])
            for g in range(2):
                hb = H.ap[g * 64:(g + 1) * 64, g * 256:(g + 1) * 256]
                S.op("dve", lambda e, hb=hb, TT=TT, g=g: e.tensor_tensor(out=hb.rearrange("p (h q) -> p h q", h=4), in0=hb.rearrange("p (h q) -> p h q", h=4), in1=TT.ap[g * 64:(g + 1) * 64, col(4, 4 * g):col(4, 4 * g) + 4].unsqueeze(2).to_broadcast([64, 4, 64]), op=ALU.mult), reads=[H, TT, psI], writes=[H])
                S.op("dve", lambda e, hb=hb, psS=psS, g=g: e.tensor_tensor(out=hb, in0=hb, in1=psS.ap[g * 64:(g + 1) * 64, g * 256:(g + 1) * 256], op=ALU.add), reads=[H, psS], writes=[H])
            S.op("act", lambda e: e.copy(out=Hb.ap, in_=H.ap), reads=[H, psI], writes=[Hb])
            if "m4" in cfg.dbg: continue
            Y = rY.next(); Y2 = rY2.next()
            S.op("dve", lambda e, Y=Y, psI=psI, TT=TT: e.tensor_tensor(out=Y.ap.rearrange("p (h q) -> p h q", h=8), in0=psI.ap.rearrange("p (h q) -> p h q", h=8), in1=TT.ap[:, col(3, 0):col(3, 0) + 8].unsqueeze(2).to_broadcast([128, 8, 64]), op=ALU.mult), reads=[psI, TT], writes=[Y])
            S.op("dve", lambda e, Y=Y, psY=psY: e.tensor_tensor(out=Y.ap, in0=Y.ap, in1=psY.ap, op=ALU.add), reads=[Y, psY], writes=[Y])
            if d == 0:
                S.op("pool", lambda e, Y2=Y2, XT=XT: e.tensor_tensor(out=Y2.ap, in0=XT.ap, in1=dsk.ap, op=ALU.mult), reads=[XT, dsk], writes=[Y2])
                S.op("pool", lambda e, Y=Y, Y2=Y2: e.tensor_tensor(out=Y.ap, in0=Y.ap, in1=Y2.ap, op=ALU.add), reads=[Y, Y2], writes=[Y])
                self.dma("sp", self.ytm[g0:g0 + 128, :], Y.ap, reads=[Y])
            else:
                self.dma("sp", Y2.ap, self.ytm[g0:g0 + 128, :], writes=[Y2])
                S.op("pool", lambda e, Y=Y, Y2=Y2: e.tensor_tensor(out=Y.ap, in0=Y.ap, in1=Y2.ap, op=ALU.add), reads=[Y, Y2], writes=[Y])
                Z = rZ.next(); Zs = rZs.next()
                zv = Z.ap.rearrange("p (c t) -> p c t", c=4)
                self.dma("act", zv, self.pfm[C_SSD_Z:C_SSD_Z + 512, g0:g0 + 128].rearrange("(c p) t -> p c t", p=128), writes=[Z])
                psZ = psq.next()
                for j in range(4):
                    S.op("pe", lambda e, psZ=psZ, zv=zv, j=j: e.transpose(psZ.ap[:, j * 128:(j + 1) * 128], zv[:, j, :], self.ident.ap), reads=[Z, self.ident], writes=[psZ], acc=j > 0)
                S.op("act", lambda e, Zs=Zs, psZ=psZ: e.activation(out=Zs.ap, in_=psZ.ap, func=AF.Silu), reads=[psZ], writes=[Zs])
                S.op("dve", lambda e, Y=Y, Zs=Zs: e.tensor_tensor(out=Y.ap, in0=Y.ap, in1=Zs.ap, op=ALU.mult), reads=[Y, Zs], writes=[Y])
                s1 = rS1.next()
                S.op("dve", lambda e, Zs=Zs, Y=Y: e.tensor_tensor(out=Zs.ap, in0=Y.ap, in1=Y.ap, op=ALU.mult), reads=[Y], writes=[Zs])
                S.op("dve", lambda e, s1=s1, Zs=Zs: e.reduce_sum(out=s1.ap[:, 0:1], in_=Zs.ap, axis=AX.X), reads=[Zs], writes=[s1])
                S.op("dve", lambda e, s1=s1: e.tensor_scalar(out=s1.ap[:, 0:1], in0=s1.ap[:, 0:1], scalar1=1.0 / 512, scalar2=1e-5, op0=ALU.mult, op1=ALU.add), reads=[s1], writes=[s1])
                S.op("act", lambda e, s1=s1: e.activation(out=s1.ap[:, 0:1], in_=s1.ap[:, 0:1], func=AF.Sqrt), reads=[s1], writes=[s1])
                S.op("dve", lambda e, s1=s1: e.reciprocal(out=s1.ap[:, 1:2], in_=s1.ap[:, 0:1]), reads=[s1], writes=[s1])
                S.op("dve", lambda e, Y=Y, s1=s1: e.scalar_tensor_tensor(out=Y.ap, in0=Y.ap, scalar=s1.ap[:, 1:2], in1=gn.ap, op0=ALU.mult, op1=ALU.mult), reads=[Y, s1, gn], writes=[Y])
                self.store_tm_as_fm(Y, 2, g0, rYo, psq)


def store_tm_as_fm(self, Y, branch, g0, rYo, psq=None):
    S = self.S
    ps = (psq or self.psr).next()
    for j in range(4):
        S.op("pe", lambda e, ps=ps, j=j: e.transpose(ps.ap[:, j * 128:(j + 1) * 128], Y.ap[:, j * 128:(j + 1) * 128], self.ident.ap), reads=[Y, self.ident], writes=[ps], acc=j > 0)
    Yo = rYo.next()
    S.op("act", lambda e: e.copy(out=Yo.ap, in_=ps.ap), reads=[ps], writes=[Yo])
    self.dma("sp", self.yT[branch, :, g0:g0 + 128].rearrange("(c p) t -> p c t", p=128), Yo.ap.rearrange("p (c t) -> p c t", c=4), reads=[Yo])


def ssd_phase(self, l, V):
    if not hasattr(self, "xbcT"):
        self.xbcT = self.dram("xbcT", [768, self.cfg.NG], BF16)
    conv_silu_phase(self, l, C_SSD_X, 6, V["ssd_cw"][l], V["ssd_cb"][l], self.xbcT)
    if "stop1" in self.cfg.dbg: return
    ssd_tab_phase(self, l, V)
    if "stop2" in self.cfg.dbg: return
    ssd_main_phase(self, l, V)


K.ssd_phase = ssd_phase
K.store_tm_as_fm = store_tm_as_fm


def gemm_fm(self, ranges, src, kc, tok_blocks, sink, wring, xring, wcols, tbs=512):
    S = self.S
    wt = wring.next()
    wv = wt.ap[:, 0:kc * wcols].rearrange("p (k n) -> p k n", k=kc)
    off = 0
    offs = []
    for (W, c0, n) in ranges:
        self.dma("pool", wv[:, :, off:off + n], W[:, c0:c0 + n].rearrange("(k p) n -> p k n", p=128), writes=[wt], reads=[wt] if off else [])
        offs.append(off)
        off += n
    for (g0, nb) in tok_blocks:
        xb = xring.next()
        xv = xb.ap[:, 0:kc * tbs].rearrange("p (k t) -> p k t", k=kc)
        self.dma("sp", xv[:, :, 0:nb], src[:, 0:kc, g0:g0 + nb], writes=[xb])
        for ci, (W, c0, n) in enumerate(ranges):
            ps = self.psr.next()
            o = offs[ci]
            for k in range(kc):
                S.op("pe", lambda e: e.matmul(ps.ap[0:n, 0:nb], lhsT=wv[:, k, o:o + n], rhs=xv[:, k, 0:nb], start=(k == 0), stop=(k == kc - 1)),
                     reads=[wt, xb], writes=[ps], acc=k > 0)
            sink(ci, g0, nb, ps)


def merge_phase(self, l, W_in_l, wb, wo):
    cfg, S = self.cfg, self.S
    NG = cfg.NG
    if not hasattr(self, "PB"):
        self.PB = [self.dram(f"PB{n}", [D, NG]) for n in range(4)]
        self.mT = self.dram("mT", [128, KC, NG], BF16)
        self.yfm = self.dram("yfm", [D, NG])
    tb = blocks(0, NG, 512)
    self.phase()
    wring = Ring(self.AB.allocn(2, 4 * 512, "wt")); xring = Ring(self.AB.allocn(2, 4 * 512, "xb")); oring = Ring(self.AF.allocn(4, 512, "ost"))
    for n in range(4):
        src = self.yT[n].rearrange("(k p) t -> p k t", p=128)
        for c0 in range(0, D, 512):
            rngs = [(wb[n], c0 + j * 128, 128) for j in range(4)]

            def sink(ci, g0, nb, ps, n=n, c0=c0):
                ot = oring.next()
                S.op("act" if ci % 2 else "dve", (lambda e: e.copy(out=ot.ap[:, 0:nb], in_=ps.ap[:, 0:nb])) if ci % 2 else (lambda e: e.tensor_copy(out=ot.ap[:, 0:nb], in_=ps.ap[:, 0:nb])), reads=[ps], writes=[ot])
                self.dma("act", self.PB[n][c0 + ci * 128:c0 + (ci + 1) * 128, g0:g0 + nb], ot.ap[:, 0:nb], reads=[ot])
            gemm_fm(self, rngs, src, 4, tb, sink, wring, xring, 512)
    self.phase()
    wring = Ring(self.AB.allocn(1, KC * 512, "wt")); xring = Ring(self.AB.allocn(2, KC * 512, "xb"))
    sgr = Ring(self.AF.allocn(2, 512, "sg")); pbr = Ring(self.AF.allocn(2, 512, "pb")); accr = Ring(self.AF.allocn(2, 512, "acc")); mor = Ring(self.AB.allocn(2, 512, "mo"))
    for m in range(KC):
        rngs = [(W_in_l, C_GATE + n * D + m * 128, 128) for n in range(4)]
        st = {}

        def sink(ci, g0, nb, ps, m=m, st=st):
            sg = sgr.next(); pb = pbr.next()
            self.dma("act", pb.ap[:, 0:nb], self.PB[ci][m * 128:(m + 1) * 128, g0:g0 + nb], writes=[pb])
            S.op("act", lambda e: e.activation(out=sg.ap[:, 0:nb], in_=ps.ap[:, 0:nb], func=AF.Sigmoid), reads=[ps], writes=[sg])
            if ci == 0:
                st["acc"] = accr.next()
                acc = st["acc"]
                S.op("dve", lambda e: e.tensor_tensor(out=acc.ap[:, 0:nb], in0=sg.ap[:, 0:nb], in1=pb.ap[:, 0:nb], op=ALU.mult), reads=[sg, pb], writes=[acc])
            else:
                acc = st["acc"]
                S.op("dve", lambda e: e.tensor_tensor(out=sg.ap[:, 0:nb], in0=sg.ap[:, 0:nb], in1=pb.ap[:, 0:nb], op=ALU.mult), reads=[sg, pb], writes=[sg])
                S.op("pool", lambda e: e.tensor_tensor(out=acc.ap[:, 0:nb], in0=acc.ap[:, 0:nb], in1=sg.ap[:, 0:nb], op=ALU.add), reads=[acc, sg], writes=[acc])
            if ci == 3:
                mo = mor.next()
                S.op("act", lambda e: e.copy(out=mo.ap[:, 0:nb], in_=acc.ap[:, 0:nb]), reads=[acc], writes=[mo])
                self.dma("sp", self.mT[:, m, g0:g0 + nb], mo.ap[:, 0:nb], reads=[mo])
        gemm_fm(self, rngs, self.xmT, KC, tb, sink, wring, xring, 512)
    self.gemm_to_yfm(wo, self.mT, KC)


def gemm_to_yfm(self, W, src, kc, accumulate=False):
    cfg, S = self.cfg, self.S
    tb = blocks(0, cfg.NG, 256)
    self.phase()
    wring = Ring(self.AB.allocn(1, kc * 512, "wt")); xring = Ring(self.AB.allocn(2, kc * 256, "xb")); oring = Ring(self.AF.allocn(4, 512, "ost"))
    for c0 in range(0, D, 512):
        rngs = [(W, c0 + j * 128, 128) for j in range(4)]

        def sink(ci, g0, nb, ps, c0=c0):
            ot = oring.next()
            if accumulate:
                self.dma("sp", ot.ap[:, 0:nb], self.yfm[c0 + ci * 128:c0 + (ci + 1) * 128, g0:g0 + nb], writes=[ot])
                S.op("dve", lambda e: e.tensor_tensor(out=ot.ap[:, 0:nb], in0=ot.ap[:, 0:nb], in1=ps.ap[:, 0:nb], op=ALU.add), reads=[ps, ot], writes=[ot])
            else:
                S.op("act" if ci % 2 else "dve", (lambda e: e.copy(out=ot.ap[:, 0:nb], in_=ps.ap[:, 0:nb])) if ci % 2 else (lambda e: e.tensor_copy(out=ot.ap[:, 0:nb], in_=ps.ap[:, 0:nb])), reads=[ps], writes=[ot])
            self.dma("act", self.yfm[c0 + ci * 128:c0 + (ci + 1) * 128, g0:g0 + nb], ot.ap[:, 0:nb], reads=[ot])
        gemm_fm(self, rngs, src, kc, tb, sink, wring, xring, 512, tbs=256)


def epi_phase(self, l, x_src, ctx_src, gate_i, ln_i, dst, lng, lnb, out_ext=None):
    cfg, S = self.cfg, self.S
    alpha = float((2.0 * 2) ** 0.25)
    self.phase()
    gm = {}
    for kind in range(2):
        t = self.AF.alloc(D, f"gm{kind}")
        self.dma("sp", t.ap, self.modD[l, kind:kind + 1, gate_i * D:(gate_i + 1) * D].to_broadcast([128, D]), writes=[t])
        gm[kind] = t
    G = self.AF.alloc(D, "lng"); self.dma("sp", G.ap, lng[l, ln_i:ln_i + 1, :].to_broadcast([128, D]), writes=[G])
    Bt = self.AF.alloc(D, "lnb"); self.dma("sp", Bt.ap, lnb[l, ln_i:ln_i + 1, :].to_broadcast([128, D]), writes=[Bt])
    xr = Ring(self.AF.allocn(2, D, "x")); yr = Ring(self.AF.allocn(2, D, "y")); rr = Ring(self.AF.allocn(2, D, "r")); sr = Ring(self.AF.allocn(4, 4, "s"))
    for ti in range(cfg.NT):
        g0 = ti * 128
        kind = 1 if ti < CTX // 128 else 0
        src = ctx_src[g0:g0 + 128, :] if kind == 1 else x_src[g0 - CTX:g0 + 128 - CTX, :]
        X = xr.next(); Y = yr.next(); R = rr.next(); s = sr.next()
        self.dma("sp", X.ap, src, writes=[X])
        yv = Y.ap.rearrange("p (c t) -> p c t", c=KC)
        self.dma("act", yv, self.yfm[:, g0:g0 + 128].rearrange("(c p) t -> p c t", p=128), writes=[Y])
        for g in range(4):
            ps = self.psr.next()
            for j in range(4):
                S.op("pe", lambda e: e.transpose(ps.ap[:, j * 128:(j + 1) * 128], yv[:, g * 4 + j, :], self.ident.ap), reads=[Y, self.ident], writes=[ps], acc=j > 0)
            S.op("dve", lambda e: e.tensor_tensor(out=R.ap[:, g * 512:(g + 1) * 512], in0=ps.ap, in1=gm[kind].ap[:, g * 512:(g + 1) * 512], op=ALU.mult), reads=[ps, gm[kind]], writes=[R], acc=True)
        S.op("dve", lambda e: e.scalar_tensor_tensor(out=R.ap, in0=X.ap, scalar=alpha, in1=R.ap, op0=ALU.mult, op1=ALU.add), reads=[X, R], writes=[R])
        S.op("dve", lambda e: e.reduce_sum(out=s.ap[:, 0:1], in_=R.ap, axis=AX.X), reads=[R], writes=[s])
        S.op("dve", lambda e: e.tensor_scalar_mul(out=s.ap[:, 0:1], in0=s.ap[:, 0:1], scalar1=1.0 / D), reads=[s], writes=[s])
        S.op("dve", lambda e: e.tensor_scalar_sub(out=R.ap, in0=R.ap, scalar1=s.ap[:, 0:1]), reads=[R, s], writes=[R])
        S.op("pool", lambda e: e.tensor_tensor(out=X.ap, in0=R.ap, in1=R.ap, op=ALU.mult), reads=[R], writes=[X])
        S.op("dve", lambda e: e.reduce_sum(out=s.ap[:, 1:2], in_=X.ap, axis=AX.X), reads=[X], writes=[s])
        S.op("dve", lambda e: e.tensor_scalar(out=s.ap[:, 1:2], in0=s.ap[:, 1:2], scalar1=1.0 / D, scalar2=1e-5, op0=ALU.mult, op1=ALU.add), reads=[s], writes=[s])
        S.op("act", lambda e: e.activation(out=s.ap[:, 2:3], in_=s.ap[:, 1:2], func=AF.Sqrt), reads=[s], writes=[s])
        S.op("dve", lambda e: e.reciprocal(out=s.ap[:, 3:4], in_=s.ap[:, 2:3]), reads=[s], writes=[s])
        S.op("dve", lambda e: e.scalar_tensor_tensor(out=R.ap, in0=R.ap, scalar=s.ap[:, 3:4], in1=G.ap, op0=ALU.mult, op1=ALU.mult), reads=[R, s, G], writes=[R])
        S.op("pool", lambda e: e.tensor_tensor(out=R.ap, in0=R.ap, in1=Bt.ap, op=ALU.add), reads=[R, Bt], writes=[R])
        self.dma("sp", dst[g0:g0 + 128, :], R.ap, reads=[R])
        if out_ext is not None and kind == 0:
            self.dma("act", out_ext[g0 - CTX:g0 - CTX + 128, :], R.ap, reads=[R])


def ffn_dense_phase(self, l, wg, wu, wd, FD, comb_row=None, accumulate=False):
    cfg, S = self.cfg, self.S
    nf = FD // 128
    if not hasattr(self, "UT"):
        self.UT = self.dram("UT", [128, max(cfg.FD, cfg.FE) // 128, cfg.NG], BF16)
    self.phase()
    tb = blocks(0, cfg.NG, 512)
    wring = Ring(self.AB.allocn(1, KC * 512, "wt")); xring = Ring(self.AB.allocn(2, KC * 512, "xb"))
    sgr = Ring(self.AF.allocn(2, 512, "sg")); uor = Ring(self.AB.allocn(2, 512, "uo"))
    cbr = Ring(self.AF.allocn(2, 512, "cb")) if comb_row is not None else None
    for f0 in range(0, nf, 2):
        fcs = [f0, f0 + 1] if f0 + 1 < nf else [f0]
        rngs = []
        for fc in fcs:
            rngs += [(wg, fc * 128, 128), (wu, fc * 128, 128)]
        st = {}

        def sink(ci, g0, nb, ps, fcs=fcs, st=st):
            if ci == 0 and comb_row is not None:
                st["cb"] = cbr.next()
                self.dma("act", st["cb"].ap[:, 0:nb], comb_row[:, g0:g0 + nb].to_broadcast([128, nb]), writes=[st["cb"]])
            if ci % 2 == 0:
                st["sg"] = sgr.next()
                sg = st["sg"]
                S.op("act", lambda e: e.activation(out=sg.ap[:, 0:nb], in_=ps.ap[:, 0:nb], func=AF.Silu), reads=[ps], writes=[sg])
                if comb_row is not None:
                    cb = st["cb"]
                    S.op("pool", lambda e: e.tensor_tensor(out=sg.ap[:, 0:nb], in0=sg.ap[:, 0:nb], in1=cb.ap[:, 0:nb], op=ALU.mult), reads=[sg, cb], writes=[sg])
            else:
                sg = st["sg"]; uo = uor.next()
                S.op("dve", lambda e: e.tensor_tensor(out=uo.ap[:, 0:nb], in0=sg.ap[:, 0:nb], in1=ps.ap[:, 0:nb], op=ALU.mult), reads=[sg, ps], writes=[uo])
                self.dma("act", self.UT[:, fcs[ci // 2], g0:g0 + nb], uo.ap[:, 0:nb], reads=[uo])
        gemm_fm(self, rngs, self.xmT, KC, tb, sink, wring, xring, 512)
    self.gemm_to_yfm(wd, self.UT, nf, accumulate=accumulate)


K.merge_phase = merge_phase
K.gemm_to_yfm = gemm_to_yfm
K.epi_phase = epi_phase
K.ffn_dense_phase = ffn_dense_phase


def zero_fill_phase(self, targets):
    S = self.S
    self.phase()
    zf = self.AF.alloc(2048, "zf"); S.op("dve", lambda e: e.memset(zf.ap, 0.0), writes=[zf])
    zb = self.AB.alloc(2048, "zb"); S.op("dve", lambda e: e.memset(zb.ap, 0.0), writes=[zb])
    i = 0
    for t in targets:
        z = zb if t.dtype == BF16 else zf
        R, Cn = t.shape
        for r0 in range(0, R, 128):
            for (c0, n) in blocks(0, Cn, 2048):
                self.dma("sp" if i % 2 == 0 else "act", t[r0:r0 + 128, c0:c0 + n], z.ap[:, 0:n], reads=[z])
                i += 1


def moe_phase(self, l, V):
    j = l // 2
    for e in range(8):
        ffn_dense_phase(self, l, V["moe_w_gate"][j, e], V["moe_w_up"][j, e], V["moe_w_down"][j, e], self.cfg.FE, comb_row=self.combT[e:e + 1, :], accumulate=(e > 0))


K.zero_fill_phase = zero_fill_phase
K.moe_phase = moe_phase


def mlstm_tab_phase(self, l, V):
    cfg, S = self.cfg, self.S
    NG = cfg.NG
    NB = 1024
    if not hasattr(self, "tabM"):
        self.tabM = self.dram("tabM", [2, 6, 4, NG])
    self.phase()
    pr = self.AF.alloc(4, "mlp", parts=4)
    self.dma("sp", pr.ap, V["ml_hp"][l], writes=[pr])
    ones = self.AF.alloc(NB, "ones", parts=4)
    S.op("dve", lambda e: e.memset(ones.ap, 1.0), writes=[ones])
    zero1 = self.AF.alloc(1, "z1", parts=4)
    S.op("dve", lambda e: e.memset(zero1.ap, 0.0), writes=[zero1])
    names = ("I", "F", "T", "BC", "G", "GE", "GP", "KS", "WIN", "E3", "CAR")
    rr = {n: Ring(self.AF.allocn(2, NB, n, parts=4)) for n in names}
    cring = Ring(self.AF.allocn(6, 2, "car", parts=4))
    for d in range(2):
        carry = None
        segs = _segments(cfg)
        blks = []
        for (s0, s1) in segs:
            b = blocks(s0, s1, NB)
            blks += b if d == 0 else b[::-1]
        for (g0, nb) in blks:
            I, F, T, BC, G, GE, GP, KS, WIN, E3, CAR = [rr[n].next() for n in names]
            v = lambda X: X.ap[:, 0:nb]
            f = (lambda a: a) if d == 0 else (lambda a: rev_ap(a, nb))
            self.dma("sp", v(I), self.pfm[C_ML_G + d * 8:C_ML_G + d * 8 + 4, g0:g0 + nb], writes=[I])
            self.dma("act", v(F), self.pfm[C_ML_G + d * 8 + 4:C_ML_G + d * 8 + 8, g0:g0 + nb], writes=[F])
            S.op("dve", lambda e: e.tensor_scalar_add(out=v(I), in0=v(I), scalar1=pr.ap[:, d:d + 1]), reads=[I, pr], writes=[I])
            S.op("dve", lambda e: e.tensor_scalar(out=v(F), in0=v(F), scalar1=pr.ap[:, 2 + d:3 + d], scalar2=-1.0, op0=ALU.add, op1=ALU.mult), reads=[F, pr], writes=[F])
            softplus_ops(self, F, T, nb, 4)
            cb_ap = zero1.ap[:, 0:1] if carry is None else carry.ap[:, 0:1]
            cg_ap = zero1.ap[:, 0:1] if carry is None else carry.ap[:, 1:2]
            cdeps = [zero1] if carry is None else [carry]
            S.op("dve", lambda e: e.tensor_tensor_scan(out=f(v(BC)), data0=f(v(ones)), data1=f(v(F)), initial=cb_ap, op0=ALU.mult, op1=ALU.subtract), reads=[F, ones] + cdeps, writes=[BC])
            S.op("dve", lambda e: e.tensor_tensor(out=v(I), in0=v(I), in1=v(BC), op=ALU.subtract), reads=[I, BC], writes=[I])
            S.op("dve", lambda e: e.tensor_tensor_scan(out=f(v(G)), data0=f(v(ones)), data1=f(v(I)), initial=cg_ap, op0=ALU.mult, op1=ALU.max), reads=[I, ones] + cdeps, writes=[G])
            endpos = 127 if d == 0 else 0
            nch = nb // 128
            gv = v(G).rearrange("p (c t) -> p c t", t=128)
            S.op("dve", lambda e: e.tensor_copy(out=v(GE).rearrange("p (c t) -> p c t", t=128), in_=gv[:, :, endpos:endpos + 1].to_broadcast([4, nch, 128])), reads=[G], writes=[GE])
            if d == 0:
                if nch > 1:
                    S.op("dve", lambda e: e.tensor_copy(out=GP.ap[:, 128:nb], in_=GE.ap[:, 0:nb - 128]), reads=[GE], writes=[GP])
                S.op("dve", lambda e: e.tensor_copy(out=GP.ap[:, 0:128], in_=cg_ap.to_broadcast([4, 128])), reads=cdeps, writes=[GP], acc=True)
            else:
                if nch > 1:
                    S.op("dve", lambda e: e.tensor_copy(out=GP.ap[:, 0:nb - 128], in_=GE.ap[:, 128:nb]), reads=[GE], writes=[GP])
                S.op("dve", lambda e: e.tensor_copy(out=GP.ap[:, nb - 128:nb], in_=cg_ap.to_broadcast([4, 128])), reads=cdeps, writes=[GP], acc=True)
            S.op("dve", lambda e: e.tensor_tensor(out=v(KS), in0=v(I), in1=v(GE), op=ALU.subtract), reads=[I, GE], writes=[KS])
            S.op("act", lambda e: e.activation(out=v(KS), in_=v(KS), func=AF.Exp), reads=[KS], writes=[KS])
            S.op("dve", lambda e: e.tensor_tensor(out=v(WIN), in0=v(GP), in1=v(G), op=ALU.subtract), reads=[GP, G], writes=[WIN])
            S.op("act", lambda e: e.activation(out=v(WIN), in_=v(WIN), func=AF.Exp), reads=[WIN], writes=[WIN])
            S.op("dve", lambda e: e.tensor_tensor(out=v(E3), in0=v(BC), in1=v(G), op=ALU.add), reads=[BC, G], writes=[E3])
            S.op("act", lambda e: e.activation(out=v(E3), in_=v(E3), func=AF.Exp, scale=-1.0), reads=[E3], writes=[E3])
            S.op("dve", lambda e: e.tensor_tensor(out=v(CAR), in0=v(GP), in1=v(GE), op=ALU.subtract), reads=[GP, GE], writes=[CAR])
            S.op("act", lambda e: e.activation(out=v(CAR), in_=v(CAR), func=AF.Exp), reads=[CAR], writes=[CAR])
            ncar = cring.next()
            lastpos = nb - 1 if d == 0 else 0
            S.op("dve", lambda e: e.tensor_copy(out=ncar.ap[:, 0:1], in_=BC.ap[:, lastpos:lastpos + 1]), reads=[BC], writes=[ncar])
            S.op("dve", lambda e: e.tensor_copy(out=ncar.ap[:, 1:2], in_=G.ap[:, lastpos:lastpos + 1]), reads=[G], writes=[ncar], acc=True)
            carry = ncar
            for qi, X in enumerate((I, KS, WIN, E3, CAR, G)):
                self.dma("sp" if qi % 2 else "act", self.tabM[d, qi, :, g0:g0 + nb], v(X), reads=[X])


def mlstm_main_phase(self, l, V):
    cfg, S = self.cfg, self.S
    NG = cfg.NG
    if not hasattr(self, "htm"):
        self.htm = self.dram("htm", [NG, 512])
    for d in range(2):
        self.phase()
        AFa, ABa = self.AF, self.AB
        tri = AFa.alloc(128, "tri"); self.dma("sp", tri.ap, V["tri"][d], writes=[tri])
        sel = AFa.alloc(4 * 128, "sel", parts=24); self.dma("sp", sel.ap, V["sel24"].rearrange("r h p -> r (h p)"), writes=[sel])
        Cst = AFa.alloc(4 * 129, "C"); S.op("dve", lambda e: e.memset(Cst.ap, 0.0), writes=[Cst])
        Cb = Cst
        rQK = Ring(AFa.allocn(2, 8 * 128, "qk")); rVF = Ring(AFa.allocn(2, 512, "vf")); rTAB = Ring(AFa.allocn(2, 128, "tabm", parts=24)); rTT = Ring(AFa.allocn(2, 24, "TTm"))
        vas = AFa.allocn(2, 4 * 129, "VA")
        for va in vas:
            S.op("dve", lambda e, va=va: e.memset(va.ap, 1.0), writes=[va])
        rVA = Ring(vas); rKS = Ring(AFa.allocn(2, 512, "KSt"))
        rDm = Ring(AFa.allocn(3, 128, "Dm")); rST = Ring(AFa.allocn(3, 128, "ST")); rN = Ring(AFa.allocn(3, 132, "N")); rs = Ring(AFa.allocn(4, 2, "s"))
        rHS = Ring(AFa.allocn(2, 512, "HS")); rH2 = Ring(AFa.allocn(2, 512, "H2")); rO = Ring(AFa.allocn(2, 512, "Of")); rOs = Ring(AFa.allocn(2, 512, "Os")); rYo = Ring(ABa.allocn(2, 512, "Yo"))
        psq = self.psr
        col = lambda q, h: q * 4 + h
        for c in chunk_order(cfg, d):
            g0 = c * 128
            QK = rQK.next(); VF = rVF.next(); TAB = rTAB.next(); TT = rTT.next(); VA = rVA.next(); KS = rKS.next(); HS = rHS.next()
            qv = QK.ap.rearrange("p (c t) -> p c t", c=8)
            self.dma("sp", qv, self.qkT[:, g0:g0 + 128].rearrange("(c p) t -> p c t", p=128), writes=[QK])
            vv = VF.ap.rearrange("p (c t) -> p c t", c=4)
            self.dma("act", vv, self.pfm[C_ML_V:C_ML_V + 512, g0:g0 + 128].rearrange("(c p) t -> p c t", p=128), writes=[VF])
            self.dma("act", TAB.ap, self.tabM[d, :, :, g0:g0 + 128].rearrange("q h t -> (q h) t"), writes=[TAB])
            ps = psq.next()
            S.op("pe", lambda e: e.transpose(ps.ap[:, 0:24], TAB.ap, self.ident.ap[0:24, 0:24]), reads=[TAB, self.ident], writes=[ps])
            S.op("act", lambda e: e.copy(out=TT.ap, in_=ps.ap[:, 0:24]), reads=[ps], writes=[TT])
            ps = psq.next()
            for j in range(4):
                S.op("pe", lambda e: e.transpose(ps.ap[:, j * 128:(j + 1) * 128], vv[:, j, :], self.ident.ap), reads=[VF, self.ident], writes=[ps], acc=j > 0)
            va3 = VA.ap.rearrange("p (h q) -> p h q", h=4)
            S.op("act", lambda e: e.copy(out=va3[:, :, 0:128], in_=ps.ap.rearrange("p (h q) -> p h q", h=4)), reads=[ps], writes=[VA], acc=True)
            ps = psq.next()
            pb = ps.ap
            for j in range(4):
                S.op("pe", lambda e: e.transpose(pb[:, j * 128:(j + 1) * 128], qv[:, 4 + j, :], self.ident.ap), reads=[QK, self.ident], writes=[ps], acc=j > 0)
            for h in range(4):
                S.op("act", lambda e: e.activation(out=KS.ap[:, h * 128:(h + 1) * 128], in_=pb[:, h * 128:(h + 1) * 128], func=AF.Copy, scale=TT.ap[:, col(1, h):col(1, h) + 1]), reads=[ps, TT], writes=[KS], acc=h > 0)
            for h in range(4):
                psS = psq.next()
                S.op("pe", lambda e: e.matmul(psS.ap[:, 0:128], lhsT=qv[:, 4 + h, :], rhs=qv[:, h, :], start=True, stop=True), reads=[QK], writes=[psS])
                psR = psq.next()
                S.op("pe", lambda e: e.matmul(psR.ap[:, 0:128], lhsT=sel.ap[:, h * 128:(h + 1) * 128], rhs=TAB.ap, start=True, stop=True), reads=[sel, TAB], writes=[psR])
                Dm = rDm.next(); ST = rST.next(); N = rN.next(); s = rs.next()
                S.op("dve", lambda e: e.tensor_scalar(out=Dm.ap, in0=psR.ap[:, 0:128], scalar1=TT.ap[:, col(0, h):col(0, h) + 1], scalar2=0.0, op0=ALU.subtract, op1=ALU.max), reads=[psR, TT], writes=[Dm])
                S.op("act", lambda e: e.activation(out=Dm.ap, in_=Dm.ap, func=AF.Exp, scale=-1.0), reads=[Dm], writes=[Dm])
                S.op("pool", lambda e: e.tensor_tensor(out=Dm.ap, in0=Dm.ap, in1=tri.ap, op=ALU.mult), reads=[Dm, tri], writes=[Dm])
                S.op("dve", lambda e: e.tensor_tensor(out=ST.ap, in0=Dm.ap, in1=psS.ap[:, 0:128], op=ALU.mult), reads=[Dm, psS], writes=[ST])
                psA = psq.next()
                S.op("pe", lambda e: e.matmul(psA.ap[:, 0:129], lhsT=ST.ap, rhs=VA.ap[:, h * 129:(h + 1) * 129], start=True, stop=True), reads=[ST, VA], writes=[psA])
                psB = psq.next()
                S.op("pe", lambda e: e.matmul(psB.ap[:, 0:129], lhsT=qv[:, h, :], rhs=Cb.ap[:, h * 129:(h + 1) * 129], start=True, stop=True), reads=[QK, Cb], writes=[psB])
                S.op("dve", lambda e: e.tensor_scalar_mul(out=N.ap[:, 0:129], in0=psB.ap[:, 0:129], scalar1=TT.ap[:, col(2, h):col(2, h) + 1]), reads=[psB, TT], writes=[N])
                S.op("dve", lambda e: e.tensor_tensor(out=N.ap[:, 0:129], in0=N.ap[:, 0:129], in1=psA.ap[:, 0:129], op=ALU.add), reads=[N, psA], writes=[N])
                S.op("act", lambda e: e.activation(out=s.ap[:, 0:1], in_=N.ap[:, 128:129], func=AF.Abs), reads=[N], writes=[s])
                S.op("dve", lambda e: e.tensor_tensor(out=s.ap[:, 0:1], in0=s.ap[:, 0:1], in1=TT.ap[:, col(3, h):col(3, h) + 1], op=ALU.max), reads=[s, TT], writes=[s])
                S.op("dve", lambda e: e.reciprocal(out=s.ap[:, 1:2], in_=s.ap[:, 0:1]), reads=[s], writes=[s])
                S.op("dve", lambda e: e.tensor_scalar_mul(out=HS.ap[:, h * 128:(h + 1) * 128], in0=N.ap[:, 0:128], scalar1=s.ap[:, 1:2]), reads=[N, s], writes=[HS], acc=h > 0)
                psU = psq.next()
                S.op("pe", lambda e: e.matmul(psU.ap[:, 0:129], lhsT=KS.ap[:, h * 128:(h + 1) * 128], rhs=VA.ap[:, h * 129:(h + 1) * 129], start=True, stop=True), reads=[KS, VA], writes=[psU])
                cs = Cst.ap[:, h * 129:(h + 1) * 129]
                S.op("dve", lambda e: e.scalar_tensor_tensor(out=cs, in0=cs, scalar=TT.ap[:, col(4, h):col(4, h) + 1], in1=psU.ap[:, 0:129], op0=ALU.mult, op1=ALU.add), reads=[Cst, TT, psU, psB], writes=[Cst])
            if d == 0:
                self.dma("sp", self.htm[g0:g0 + 128, :], HS.ap, reads=[HS])
            else:
                H2 = rH2.next(); Of = rO.next(); Os = rOs.next()
                self.dma("sp", H2.ap, self.htm[g0:g0 + 128, :], writes=[H2])
                S.op("pool", lambda e: e.tensor_tensor(out=HS.ap, in0=HS.ap, in1=H2.ap, op=ALU.add), reads=[HS, H2], writes=[HS])
                ov = Of.ap.rearrange("p (c t) -> p c t", c=4)
                self.dma("act", ov, self.pfm[C_ML_O:C_ML_O + 512, g0:g0 + 128].rearrange("(c p) t -> p c t", p=128), writes=[Of])
                psZ = psq.next()
                for j in range(4):
                    S.op("pe", lambda e: e.transpose(psZ.ap[:, j * 128:(j + 1) * 128], ov[:, j, :], self.ident.ap), reads=[Of, self.ident], writes=[psZ], acc=j > 0)
                S.op("act", lambda e: e.activation(out=Os.ap, in_=psZ.ap, func=AF.Sigmoid), reads=[psZ], writes=[Os])
                S.op("dve", lambda e: e.tensor_tensor(out=HS.ap, in0=HS.ap, in1=Os.ap, op=ALU.mult), reads=[HS, Os], writes=[HS])
                self.store_tm_as_fm(HS, 3, g0, rYo)


def mlstm_phase(self, l, V):
    cfg, S = self.cfg, self.S
    if not hasattr(self, "qkT"):
        self.qkT = self.dram("qkT", [1024, cfg.NG])
    st = {}

    def post(c, s0, g0, nb, SG, O, ctxp):
        scale = (128.0 ** -0.5) if c < 4 else 1.0
        if "init" not in ctxp:
            ctxp["init"] = True
            ctxp["P"] = self.AB.alloc(128, "ropeP")
            pf = self.AF.alloc(128, "ropePf")
            self.dma("sp", pf.ap, V["ropeP"], writes=[pf])
            S.op("dve", lambda e: e.tensor_copy(out=ctxp["P"].ap, in_=pf.ap), reads=[pf], writes=[ctxp["P"]])
            ctxp["sgb"] = Ring(self.AB.allocn(2, 1024, "sgb"))
            ctxp["cos"] = Ring(self.AF.allocn(2, 1024, "cos")); ctxp["sin"] = Ring(self.AF.allocn(2, 1024, "sin")); ctxp["t1"] = Ring(self.AF.allocn(2, 1024, "t1"))
        if s0 == 0:
            S.op("act", lambda e: e.activation(out=O.ap[:, 0:nb], in_=SG.ap[:, 0:nb], func=AF.Copy, scale=scale), reads=[SG], writes=[O])
            return
        P = ctxp["P"]; sgb = ctxp["sgb"].next(); cs = ctxp["cos"].next(); sn = ctxp["sin"].next(); t1 = ctxp["t1"].next()
        t0 = g0 - CTX
        self.dma("sp", cs.ap[:, 0:nb], V["rope_cos"][:, t0:t0 + nb], writes=[cs])
        self.dma("sp", sn.ap[:, 0:nb], V["rope_sin"][:, t0:t0 + nb], writes=[sn])
        S.op("act", lambda e: e.copy(out=sgb.ap[:, 0:nb], in_=SG.ap[:, 0:nb]), reads=[SG], writes=[sgb])
        S.op("pool", lambda e: e.tensor_tensor(out=t1.ap[:, 0:nb], in0=SG.ap[:, 0:nb], in1=cs.ap[:, 0:nb], op=ALU.mult), reads=[SG, cs], writes=[t1])
        for (o, n) in blocks(0, nb, 512):
            ps = self.psr.next()
            S.op("pe", lambda e: e.matmul(ps.ap[:, 0:n], lhsT=P.ap, rhs=sgb.ap[:, o:o + n], start=True, stop=True), reads=[P, sgb], writes=[ps])
            S.op("dve", lambda e: e.tensor_tensor(out=sn.ap[:, o:o + n], in0=ps.ap[:, 0:n], in1=sn.ap[:, o:o + n], op=ALU.mult), reads=[ps, sn], writes=[sn], acc=True)
        S.op("dve", lambda e: e.tensor_tensor(out=t1.ap[:, 0:nb], in0=t1.ap[:, 0:nb], in1=sn.ap[:, 0:nb], op=ALU.add), reads=[t1, sn], writes=[t1])
        S.op("act", lambda e: e.activation(out=O.ap[:, 0:nb], in_=t1.ap[:, 0:nb], func=AF.Copy, scale=scale), reads=[t1], writes=[O])
    conv_silu_phase(self, l, C_ML_Q, 8, V["ml_cw"][l], V["ml_cb"][l], self.qkT, post=post, out_f32=True)
    mlstm_tab_phase(self, l, V)
    mlstm_main_phase(self, l, V)


K.mlstm_phase = mlstm_phase


def na_rowinfo(ROWS):
    info = []
    classes = {}
    for r in range(ROWS):
        ws = min(max(r - 4, 0), ROWS - 8)
        as_ = min(2 * (ws // 2), ROWS - 10)
        key = (ws - as_, r - ws)
        if key not in classes:
            classes[key] = len(classes)
        info.append((ws, as_, classes[key]))
    return info, classes


def na_phase(self, l, V):
    cfg, S = self.cfg, self.S
    NG, NT, ROWS = cfg.NG, cfg.NT, cfg.ROWS
    info, classes = na_rowinfo(ROWS)
    ncls = len(classes)
    if not hasattr(self, "naq"):
        self.naq = self.dram("naq", [512, NG], BF16)
        self.nak = self.dram("nak", [512, NG], BF16)
        self.nav = self.dram("nav", [NG, 8 * 65], BF16)
        self.natm = self.dram("natm", [NG, 512])
    self.phase()
    NB = 2048
    rin = Ring(self.AF.allocn(2, NB, "nin")); rout = Ring(self.AB.allocn(2, NB, "nout"))
    for (row0, dst, sc) in ((C_NA_Q, self.naq, 0.125), (C_NA_K, self.nak, 1.0)):
        for c in range(4):
            for (g0, nb) in blocks(0, NG, NB):
                ti = rin.next(); to = rout.next()
                self.dma("sp", ti.ap[:, 0:nb], self.pfm[row0 + c * 128:row0 + (c + 1) * 128, g0:g0 + nb], writes=[ti])
                S.op("act", lambda e: e.activation(out=to.ap[:, 0:nb], in_=ti.ap[:, 0:nb], func=AF.Copy, scale=sc), reads=[ti], writes=[to])
                self.dma("act", dst[c * 128:(c + 1) * 128, g0:g0 + nb], to.ap[:, 0:nb], reads=[to])
    rvf = Ring(self.AF.allocn(2, 512, "vf"))
    vas = self.AB.allocn(2, 8 * 65, "va")
    for va in vas:
        S.op("dve", lambda e, va=va: e.memset(va.ap, 1.0), writes=[va])
    rva = Ring(vas)
    for ti_ in range(NT):
        g0 = ti_ * 128
        VF = rvf.next(); VA = rva.next()
        vv = VF.ap.rearrange("p (c t) -> p c t", c=4)
        self.dma("sp", vv, self.pfm[C_NA_V:C_NA_V + 512, g0:g0 + 128].rearrange("(c p) t -> p c t", p=128), writes=[VF])
        ps = self.psr.next()
        for j in range(4):
            S.op("pe", lambda e: e.transpose(ps.ap[:, j * 128:(j + 1) * 128], vv[:, j, :], self.ident.ap), reads=[VF, self.ident], writes=[ps], acc=j > 0)
        S.op("act", lambda e: e.copy(out=VA.ap.rearrange("p (h q) -> p h q", h=8)[:, :, 0:64], in_=ps.ap.rearrange("p (h q) -> p h q", h=8)), reads=[ps], writes=[VA], acc=True)
        self.dma("act", self.nav[g0:g0 + 128, :], VA.ap, reads=[VA])
    for hc in range(4):
        self.phase()
        QT = self.AB.alloc(NG, "QT"); KT = self.AB.alloc(NG, "KT"); VT = self.AB.alloc(NT * 130, "VT"); BM = self.AB.alloc(ncls * 640, "BM")
        self.dma("sp", QT.ap, self.naq[hc * 128:(hc + 1) * 128, :], writes=[QT])
        self.dma("act", KT.ap, self.nak[hc * 128:(hc + 1) * 128, :], writes=[KT])
        self.dma("sp", VT.ap.rearrange("p (t q) -> p t q", t=NT), self.nav[:, hc * 130:(hc + 1) * 130].rearrange("(t p) q -> p t q", p=128), writes=[VT])
        for hh in range(2):
            self.dma("pool", BM.ap[hh * 64:(hh + 1) * 64, :].rearrange("p (c k) -> p c k", c=ncls), V["na_bm"][l, :, 2 * hc + hh].rearrange("c p k -> p c k"), writes=[BM], reads=[BM] if hh else [])
        vt3 = VT.ap.rearrange("p (t q) -> p t q", t=NT)
        bm3 = BM.ap.rearrange("p (c k) -> p c k", c=ncls)
        rPT = Ring(self.AB.allocn(3, 448, "PT")); rO = Ring(self.AF.allocn(3, 128, "NO", parts=64)); rs = Ring(self.AF.allocn(4, 2, "ns", parts=64))
        qrows = [("c", i) for i in range(CTX // 64)] + [("l", r) for r in range(ROWS)]
        for (kind, r) in qrows:
            OUT = rO.next()
            for hh in range(2):
                pb_ = hh * 64
                if kind == "c":
                    q0 = r * 64
                    tiles = [(None, t * 128) for t in range(CTX // 128)]
                else:
                    q0 = CTX + r * 64
                    ws, as_, cls = info[r]
                    tiles = [(j, CTX + (as_ + 2 * j) * 64) for j in range(5)] + [(None, t * 128) for t in range(CTX // 128)]
                nt_ = len(tiles)
                psS = self.psr.next()
                for ti_, (j, k0) in enumerate(tiles):
                    S.op("pe", lambda e: e.matmul(psS.ap[:, ti_ * 64:(ti_ + 1) * 64], lhsT=KT.ap[pb_:pb_ + 64, k0:k0 + 128], rhs=QT.ap[pb_:pb_ + 64, q0:q0 + 64], start=True, stop=(j is None)), reads=[KT, QT], writes=[psS], acc=ti_ > 0)
                    if j is not None:
                        S.op("pe", lambda e: e.matmul(psS.ap[:, ti_ * 64:(ti_ + 1) * 64], lhsT=bm3[pb_:pb_ + 64, cls, j * 128:(j + 1) * 128], rhs=self.identb.ap[pb_:pb_ + 64, pb_:pb_ + 64], start=False, stop=True), reads=[BM, self.identb], writes=[psS], acc=True)
                PT = rPT.next()
                S.op("act", lambda e: e.activation(out=PT.ap[:, 0:nt_ * 64], in_=psS.ap[:, 0:nt_ * 64], func=AF.Exp), reads=[psS], writes=[PT])
                psO = self.psr.next()
                for ti_, (j, k0) in enumerate(tiles):
                    S.op("pe", lambda e: e.matmul(psO.ap[0:64, 0:65], lhsT=PT.ap[:, ti_ * 64:(ti_ + 1) * 64], rhs=vt3[:, k0 // 128, hh * 65:(hh + 1) * 65], start=(ti_ == 0), stop=(ti_ == nt_ - 1)), reads=[PT, VT], writes=[psO], acc=ti_ > 0)
                s = rs.next()
                S.op("dve", lambda e: e.reciprocal(out=s.ap[:, 0:1], in_=psO.ap[0:64, 64:65]), reads=[psO], writes=[s])
                S.op("dve", lambda e: e.tensor_scalar_mul(out=OUT.ap[:, hh * 64:(hh + 1) * 64], in0=psO.ap[0:64, 0:64], scalar1=s.ap[:, 0:1]), reads=[psO, s], writes=[OUT], acc=hh > 0)
            self.dma("sp", self.natm[q0:q0 + 64, hc * 128:(hc + 1) * 128], OUT.ap, reads=[OUT])
    self.phase()
    rY = Ring(self.AF.allocn(2, 512, "naY")); rYo = Ring(self.AB.allocn(2, 512, "Yo"))
    for ti_ in range(NT):
        g0 = ti_ * 128
        Y = rY.next()
        self.dma("sp", Y.ap, self.natm[g0:g0 + 128, :], writes=[Y])
        self.store_tm_as_fm(Y, 1, g0, rYo)


K.na_phase = na_phase


import numpy as np
def pmaj(v, p=128):
    v = np.asarray(v)
    return np.ascontiguousarray(v.reshape(-1, p).T)
def base_inputs(inp, b):
    m = {}
    m["x"] = np.ascontiguousarray(inp["x"][b])
    m["ctx"] = np.ascontiguousarray(inp["ctx"][b])
    cT = np.stack([pmaj(inp["c"][b]), pmaj(inp["c_ctx"])], axis=-1)
    m["cT"] = np.ascontiguousarray(cT.astype(np.float32))
    m["ident"] = np.eye(128, dtype=np.float32)
    for k in ("w_ada", "b_ada", "w_in"):
        m[k] = np.ascontiguousarray(inp[k])
    return m

def lru_inputs(inp):
    m = {}
    L = inp["lru_conv_w"].shape[0]
    m["lru_cw"] = np.ascontiguousarray(np.stack([np.stack([pmaj(inp["lru_conv_w"][l, k]) for k in range(4)], -1) for l in range(L)]))
    m["lru_cb"] = np.ascontiguousarray(np.stack([pmaj(inp["lru_conv_b"][l]) for l in range(L)]))
    vec = []
    for l in range(L):
        vec.append(np.stack([pmaj(inp[nm][l, d]) for nm in ("lru_ba", "lru_bx", "lru_lambda") for d in range(2)], 1))
    m["lru_vec"] = np.ascontiguousarray(np.stack(vec))
    m["lru_wa"] = np.ascontiguousarray(inp["lru_wa"]); m["lru_wx"] = np.ascontiguousarray(inp["lru_wx"])
    return m

def ssd_inputs(inp):
    m = {}
    L = inp["ssd_conv_w"].shape[0]
    m["ssd_cw"] = np.ascontiguousarray(np.stack([np.stack([pmaj(inp["ssd_conv_w"][l, k]) for k in range(4)], -1) for l in range(L)]))
    m["ssd_cb"] = np.ascontiguousarray(np.stack([pmaj(inp["ssd_conv_b"][l]) for l in range(L)]))
    m["ssd_hp"] = np.ascontiguousarray(np.stack([np.stack([inp["ssd_dt_bias"][l,0], inp["ssd_dt_bias"][l,1], inp["ssd_a_log"][l,0], inp["ssd_a_log"][l,1]], -1) for l in range(L)]))
    m["ssd_dskip"] = np.ascontiguousarray(np.repeat(inp["ssd_d"], 64, axis=-1))
    m["ssd_gn"] = np.ascontiguousarray(inp["ssd_norm_g"])
    j = np.arange(128)[:, None]; i = np.arange(128)[None, :]
    m["tri"] = np.stack([(j <= i), (j >= i)]).astype(np.float32)
    sel = np.zeros((40, 8, 128), np.float32)
    for h in range(8): sel[h, h, :] = 1.0
    m["sel40"] = sel
    return m

def mlstm_inputs(inp, SEQ):
    m = {}
    L = inp["ml_conv_w"].shape[0]
    m["ml_cw"] = np.ascontiguousarray(np.stack([np.stack([pmaj(inp["ml_conv_w"][l, k]) for k in range(4)], -1) for l in range(L)]))
    m["ml_cb"] = np.ascontiguousarray(np.stack([pmaj(inp["ml_conv_b"][l]) for l in range(L)]))
    m["ml_hp"] = np.ascontiguousarray(np.stack([np.stack([inp["ml_i_bias"][l,0], inp["ml_i_bias"][l,1], inp["ml_f_bias"][l,0], inp["ml_f_bias"][l,1]], -1) for l in range(L)]))
    sel = np.zeros((24, 4, 128), np.float32)
    for h in range(4): sel[5 * 4 + h, h, :] = 1.0
    m["sel24"] = sel
    t = np.arange(SEQ, dtype=np.int32)
    pos = np.stack([t // 64, t % 64], axis=-1).astype(np.float32)
    nf = 32
    inv_freq = (np.float32(10000.0) ** (-np.arange(nf, dtype=np.float32) / nf)).astype(np.float32)
    ang = np.broadcast_to(pos[:, :, None, None] * inv_freq, (SEQ, 2, 2, nf)).reshape(SEQ, 128).astype(np.float32)
    m["rope_cos"] = np.ascontiguousarray(np.cos(ang).T.astype(np.float32))
    m["rope_sin"] = np.ascontiguousarray(np.sin(ang).T.astype(np.float32))
    P = np.zeros((128, 128), np.float32)
    for dp in range(128):
        pair = (dp % 64) // 32
        if pair == 0: P[dp + 32, dp] = -1.0
        else: P[dp - 32, dp] = 1.0
    m["ropeP"] = P
    return m

def na_inputs(inp, ROWS):
    info = []; classes = {}
    for r in range(ROWS):
        ws = min(max(r - 4, 0), ROWS - 8); as_ = min(2 * (ws // 2), ROWS - 10)
        key = (ws - as_, r - ws)
        if key not in classes: classes[key] = (len(classes), r, ws, as_)
    ncls = len(classes)
    rpb = inp["na_rpb"]; L = rpb.shape[0]
    wq = np.arange(64)[:, None]; wk = np.arange(64)[None, :]
    cs = np.clip(wq - 8, 0, 48)
    col_ok = (wk >= cs) & (wk < cs + 16)
    dc = np.clip(wk - wq + 15, 0, 30)
    bm = np.full((L, ncls, 8, 64, 640), -30000.0, np.float32)
    for key, (ci, r, ws, as_) in classes.items():
        for j in range(5):
            for par in range(2):
                kr = as_ + 2 * j + par
                if ws <= kr < ws + 8:
                    dr = kr - r + 7
                    vals = rpb[:, :, dr][:, :, dc]
                    blk = np.where(col_ok[None, None], vals, np.float32(-30000.0))
                    bm[:, ci, :, :, j * 128 + par * 64: j * 128 + par * 64 + 64] = blk
    return {"na_bm": np.ascontiguousarray(bm)}

def moe_inputs(inp):
    wr = inp["moe_w_router"]
    m = {}
    m["moe_wr_p"] = np.ascontiguousarray(wr.reshape(wr.shape[0], 16, 128, 8).transpose(0, 2, 1, 3).reshape(wr.shape[0], 128, 128))
    m["moe_br_p"] = np.ascontiguousarray(np.broadcast_to(inp["moe_b_router"][:, None, :], (wr.shape[0], 128, 8)))
    for k in ("moe_w_gate", "moe_w_up", "moe_w_down"):
        m[k] = np.ascontiguousarray(inp[k])
    return m


def kernel(**inputs):
    inp = {k: np.asarray(v) for k, v in inputs.items()}
    B, SEQ, _ = inp["x"].shape
    cfg = Cfg(SEQ, inp["ffn_w_gate"].shape[-1], inp["moe_w_gate"].shape[-1], layers=(0, 1), dbg=())
    k = K(cfg)
    nc = k.build()
    maps = []
    shared = {}
    shared.update(lru_inputs(inp)); shared.update(ssd_inputs(inp)); shared.update(mlstm_inputs(inp, SEQ))
    shared.update(na_inputs(inp, SEQ // 64)); shared.update(moe_inputs(inp))
    for kk in ("w_branch", "w_out", "ln_g", "ln_b", "ffn_w_gate", "ffn_w_up", "ffn_w_down"):
        shared[kk] = np.ascontiguousarray(inp[kk])
    for b in range(B):
        m = base_inputs(inp, b)
        m.update(shared)
        maps.append({kk: m[kk] for kk in k.ins})
    res = run_bass_kernel_spmd(nc, maps, core_ids=list(range(B)))
    return np.stack([res.results[b]["y_out"] for b in range(B)], axis=0).astype(np.float32)
```

```python
import numpy as np
from contextlib import ExitStack
import concourse.bass as bass
import concourse.mybir as mybir
from concourse.ap import AP

F32 = mybir.dt.float32
BF16 = mybir.dt.bfloat16
AF = mybir.ActivationFunctionType
ALU = mybir.AluOpType
AX = mybir.AxisListType

ENGS = ("pe", "act", "dve", "pool", "sp")
DMA_Q = ("sp", "act", "pool")
NSLOT = 8


class Tile:
    __slots__ = ("ap", "w", "r", "name")

    def __init__(self, ap, name=""):
        self.ap = ap
        self.w = {}
        self.r = {}
        self.name = name

    def __getitem__(self, k):
        return self.ap[k]


class _Rec:
    def __init__(self):
        self.call = None

    def __getattr__(self, name):
        def f(*a, **kw):
            self.call = (name, a, kw)
            return None
        return f


class Sched:
    def __init__(self, nc, es):
        self.nc = nc
        self.es = es
        self.ops = {e: [] for e in ENGS}
        self.sem = {e: es.enter_context(nc.semaphore("s_" + e)) for e in ENGS}
        self.cnt = {e: 0 for e in ENGS}
        self.slots = {q: [es.enter_context(nc.semaphore(f"d_{q}{i}")) for i in range(NSLOT)] for q in DMA_Q}
        self.slot_cnt = {q: [0] * NSLOT for q in DMA_Q}
        self.slot_i = {q: 0 for q in DMA_Q}
        self.bar = es.enter_context(nc.semaphore("bar"))
        self.bar_cnt = 0
        self.seen = {e: {} for e in ENGS}
        self.touched = set()
        self.n_ops = 0
        self.pre = {}

    def _need(self, eng, deps):
        out = []
        seen = self.seen[eng]
        for s, v in deps.items():
            if seen.get(s, 0) < v:
                seen[s] = v
                out.append((s, v))
        return out

    def op(self, eng, fn, reads=(), writes=(), dma=False, acc=False, late=False):
        if not late:
            rec = _Rec()
            fn(rec)
            assert rec.call is not None
            name, a, kw = rec.call
            fn = (lambda h, name=name, a=a, kw=kw: getattr(h, name)(*a, **kw))
        deps = {}
        own = self.sem[eng]

        def add(d):
            for s, v in d.items():
                if deps.get(s, 0) < v:
                    deps[s] = v
        for t in reads:
            add(t.w)
        for t in writes:
            if acc and eng == "pe":
                add({s: v for s, v in t.w.items() if s is not own})
            else:
                add(t.w)
            add(t.r)
        if dma:
            q = eng
            i = self.slot_i[q]
            self.slot_i[q] = (i + 1) % NSLOT
            ssem = self.slots[q][i]
            add({ssem: self.slot_cnt[q][i]})
            self.slot_cnt[q][i] += 16
            ticket = (ssem, self.slot_cnt[q][i])
            inc = (ssem, 16)
        else:
            self.cnt[eng] += 1
            ticket = (own, self.cnt[eng])
            inc = (own, 1)
        waits = self._need(eng, deps)
        self.ops[eng].append((waits, fn, inc))
        self.n_ops += 1
        for t in reads:
            if t.r.get(ticket[0], 0) < ticket[1]:
                t.r[ticket[0]] = ticket[1]
            self.touched.add(t)
        for t in writes:
            if acc:
                t.w[ticket[0]] = ticket[1]
            else:
                t.w = {ticket[0]: ticket[1]}
                t.r = {}
            self.touched.add(t)
        return ticket

    def barrier(self):
        deps = {}
        for e in ENGS:
            if self.cnt[e] > 0:
                deps[self.sem[e]] = self.cnt[e]
        for q in DMA_Q:
            for i in range(NSLOT):
                if self.slot_cnt[q][i] > 0:
                    deps[self.slots[q][i]] = self.slot_cnt[q][i]
        waits = self._need("sp", deps)
        self.bar_cnt += 1
        bar, bc = self.bar, self.bar_cnt
        self.ops["sp"].append((waits, lambda e, bar=bar: e.sem_inc(bar, 1), None))
        for e in ENGS:
            if e != "sp":
                self.ops[e].append(([(bar, bc)], None, None))
            for s, v in deps.items():
                if self.seen[e].get(s, 0) < v:
                    self.seen[e][s] = v
        for t in self.touched:
            t.w = {}
            t.r = {}
        self.touched = set()

    def emit(self, block):
        nc = self.nc
        handles = {"pe": "tensor", "act": "scalar", "dve": "vector", "pool": "gpsimd", "sp": "sync"}

        def make(e):
            def body(h):
                if e in self.pre:
                    self.pre[e](h)
                for waits, fn, inc in self.ops[e]:
                    for s, v in waits:
                        h.wait_ge(s, v)
                    if fn is not None:
                        ins = fn(h)
                        if inc is not None:
                            ins.then_inc(inc[0], inc[1])
            return body
        for e in ENGS:
            getattr(block, handles[e])(make(e))


class Arena:
    def __init__(self, nc, es, name, cols, dtype):
        self.t = es.enter_context(nc.sbuf_tensor(name, [128, cols], dtype))
        self.cols = cols
        self.off = 0
        self.name = name

    def alloc(self, cols, name="", parts=128):
        assert self.off + cols <= self.cols, f"arena {self.name} overflow {self.off}+{cols}>{self.cols} ({name})"
        ap = self.t[0:parts, self.off:self.off + cols]
        self.off += cols
        return Tile(ap, name)

    def allocn(self, n, cols, name="", parts=128):
        return [self.alloc(cols, f"{name}{i}", parts) for i in range(n)]

    def reset(self, off=0):
        self.off = off


class Ring:
    def __init__(self, tiles):
        self.tiles = tiles
        self.i = 0

    def next(self):
        t = self.tiles[self.i]
        self.i = (self.i + 1) % len(self.tiles)
        return t


import numpy as np
from contextlib import ExitStack
import concourse.bass as bass
import concourse.mybir as mybir
from concourse.ap import AP
from concourse.bass_utils import run_bass_kernel_spmd

D = 2048
KC = 16
CTX = 256
GW = 64
N_IN = 14112
C_LRU_X, C_LRU_G, C_NA_Q, C_NA_K, C_NA_V = 0, 512, 1024, 1536, 2048
C_SSD_Z, C_SSD_X, C_SSD_B, C_SSD_C, C_SSD_DT = 2560, 3072, 3584, 3712, 3840
C_ML_Q, C_ML_K, C_ML_V, C_ML_O, C_ML_G, C_GATE = 3856, 4368, 4880, 5392, 5904, 5920


class Cfg:
    def __init__(self, SEQ, FD, FE, layers=(0, 1), dbg=(), NS=1):
        self.SEQ, self.FD, self.FE, self.layers, self.dbg = SEQ, FD, FE, tuple(layers), tuple(dbg)
        self.NS = NS
        self.TL = SEQ // NS
        self.NG = CTX + SEQ
        self.NT = self.NG // 128
        self.ROWS = SEQ // GW


def blocks(lo, hi, step):
    return [(a, min(step, hi - a)) for a in range(lo, hi, step)]


class K:
    def __init__(self, cfg):
        self.cfg = cfg
        self.es = ExitStack()
        nc = self.nc = bass.Bass("TRN2", target_bir_lowering=False)
        self.S = Sched(nc, self.es)
        self.ins = {}
        self.outs = {}
        self.scr = {}
        self.AF = Arena(nc, self.es, "af32", 24 * 1024, F32)
        self.AB = Arena(nc, self.es, "abf16", 55 * 1024, BF16)
        self.ps = [Tile(self.es.enter_context(nc.psum_tensor(f"ps{i}", [128, 512], F32))[:], f"ps{i}") for i in range(8)]
        self.psr = Ring(self.ps)

    def inp(self, name, shape, dt=F32):
        t = self.nc.dram_tensor(name, list(shape), dt, kind="ExternalInput").ap()
        self.ins[name] = t
        return t

    def out(self, name, shape, dt=F32):
        t = self.nc.dram_tensor(name, list(shape), dt, kind="ExternalOutput").ap()
        self.outs[name] = t
        return t

    def dram(self, name, shape, dt=F32):
        if name in self.cfg.dbg:
            return self.out(name, shape, dt)
        t = self.nc.dram_tensor(name, list(shape), dt, kind="Internal").ap()
        self.scr[name] = t
        return t

    def tbuf(self, name, shape, dt=F32):
        key = (name, self.TN)
        if not hasattr(self, "_tb"):
            self._tb = {}
        if key not in self._tb:
            self._tb[key] = self.dram(f"{name}_{self.TN}", shape, dt)
        return self._tb[key]

    def dma(self, q, out, in_, reads=(), writes=(), **kw):
        return self.S.op(q, lambda e: e.dma_start(out=out, in_=in_, **kw), reads=reads, writes=writes, dma=True)

    def phase(self):
        self.S.barrier()
        self.AF.reset(self.af_base)
        self.AB.reset(self.ab_base)

    def build(self):
        cfg = self.cfg
        S, nc = self.S, self.nc
        NG = cfg.NG
        x_in = self.inp("x", [cfg.SEQ, D])
        ctx_in = self.inp("ctx", [CTX, D])
        cT = self.inp("cT", [128, KC, 2])
        ident_in = self.inp("ident", [128, 128])
        w_ada = self.inp("w_ada", [2, D, 6 * D])
        b_ada = self.inp("b_ada", [2, 6 * D])
        w_in = self.inp("w_in", [2, D, N_IN])
        self.xmT = self.dram("xmT", [128, KC, NG], BF16)
        self.modD = self.dram("modD", [2, 2, 6 * D])
        self.ident = self.AF.alloc(128, "ident")
        self.identb = self.AB.alloc(128, "identb")
        self.af_base, self.ab_base = self.AF.off, self.AB.off
        self.dma("sp", self.ident.ap, ident_in, writes=[self.ident])
        S.op("dve", lambda e: e.tensor_copy(out=self.identb.ap, in_=self.ident.ap), reads=[self.ident], writes=[self.identb])
        V = self.V = {}
        V["lru_cw"] = self.inp("lru_cw", [2, 128, 4, 4]); V["lru_cb"] = self.inp("lru_cb", [2, 128, 4]); V["lru_vec"] = self.inp("lru_vec", [2, 128, 6, 4])
        V["lru_wa"] = self.inp("lru_wa", [2, 2, 8, 64, 64]); V["lru_wx"] = self.inp("lru_wx", [2, 2, 8, 64, 64])
        V["ssd_cw"] = self.inp("ssd_cw", [2, 128, 6, 4]); V["ssd_cb"] = self.inp("ssd_cb", [2, 128, 6]); V["ssd_hp"] = self.inp("ssd_hp", [2, 8, 4])
        V["ssd_dskip"] = self.inp("ssd_dskip", [2, 512]); V["ssd_gn"] = self.inp("ssd_gn", [2, 512]); V["tri"] = self.inp("tri", [2, 128, 128]); V["sel40"] = self.inp("sel40", [40, 8, 128])
        V["ml_cw"] = self.inp("ml_cw", [2, 128, 8, 4]); V["ml_cb"] = self.inp("ml_cb", [2, 128, 8]); V["ml_hp"] = self.inp("ml_hp", [2, 4, 4]); V["sel24"] = self.inp("sel24", [24, 4, 128])
        V["rope_cos"] = self.inp("rope_cos", [128, cfg.SEQ]); V["rope_sin"] = self.inp("rope_sin", [128, cfg.SEQ]); V["ropeP"] = self.inp("ropeP", [128, 128])
        V["na_bm"] = self.inp("na_bm", [2, len(na_rowinfo(cfg.ROWS)[1]), 8, 64, 640])
        V["moe_wr_p"] = self.inp("moe_wr_p", [1, 128, KC * 8]); V["moe_br_p"] = self.inp("moe_br_p", [1, 128, 8])
        V["moe_w_gate"] = self.inp("moe_w_gate", [1, 8, D, cfg.FE]); V["moe_w_up"] = self.inp("moe_w_up", [1, 8, D, cfg.FE]); V["moe_w_down"] = self.inp("moe_w_down", [1, 8, cfg.FE, D])
        V["w_branch"] = self.inp("w_branch", [2, 4, 512, D]); V["w_out"] = self.inp("w_out", [2, D, D])
        V["ln_g"] = self.inp("ln_g", [2, 2, D]); V["ln_b"] = self.inp("ln_b", [2, 2, D])
        V["ffn_w_gate"] = self.inp("ffn_w_gate", [1, D, cfg.FD]); V["ffn_w_up"] = self.inp("ffn_w_up", [1, D, cfg.FD]); V["ffn_w_down"] = self.inp("ffn_w_down", [1, cfg.FD, D])
        self.y_out = self.out("y_out", [cfg.TL, D])
        self.TN, self.t_ctx, self.dyn_base = NG, CTX, None
        if cfg.NS > 1:
            def _pre(h, self=self, cfg=cfg):
                self.sval = (h.partition_id() % cfg.NS) * cfg.TL
            S.pre["sp"] = _pre
        self.phase_mod(cT, w_ada, b_ada)
        for l in cfg.layers:
            self.layer(l, x_in, ctx_in, w_in)
        S.barrier()
        with nc.Block() as block:
            S.emit(block)
        return nc

    def phase_mod(self, cT, w_ada, b_ada):
        S = self.S
        self.phase()
        ct = self.AF.alloc(KC * 2, "ct")
        self.dma("sp", ct.ap, cT.rearrange("p k m -> p (k m)"), writes=[ct])
        ca = self.AF.alloc(KC * 2, "ca")
        S.op("act", lambda e: e.activation(out=ca.ap, in_=ct.ap, func=AF.Silu), reads=[ct], writes=[ca])
        cav = ca.ap.rearrange("p (k m) -> p k m", m=2)
        wr = Ring(self.AF.allocn(2, KC * 512, "wada"))
        orow = Ring(self.AF.allocn(2, 512, "orow", parts=2))
        brow = Ring(self.AF.allocn(2, 512, "brow", parts=2))
        for l in self.cfg.layers:
            for nb in range(6 * D // 512):
                wt = wr.next()
                wv = wt.ap.rearrange("p (k n) -> p k n", k=KC)
                self.dma("sp" if nb % 2 == 0 else "act", wv, w_ada[l, :, nb * 512:(nb + 1) * 512].rearrange("(k p) n -> p k n", p=128), writes=[wt])
                bt = brow.next()
                for m in range(2):
                    self.dma("pool", bt.ap[m:m + 1, :], b_ada[l:l + 1, nb * 512:(nb + 1) * 512], writes=[bt])
                ps = self.psr.next()
                for k in range(KC):
                    S.op("pe", lambda e, k=k, wv=wv, ps=ps: e.matmul(ps.ap[0:2, :], lhsT=cav[:, k, :], rhs=wv[:, k, :], start=(k == 0), stop=(k == KC - 1)),
                         reads=[ca, wt], writes=[ps], acc=k > 0)
                ot = orow.next()
                S.op("dve", lambda e, ot=ot, ps=ps, bt=bt: e.tensor_tensor(out=ot.ap, in0=ps.ap[0:2, :], in1=bt.ap, op=ALU.add), reads=[ps, bt], writes=[ot])
                self.dma("sp", self.modD[l, :, nb * 512:(nb + 1) * 512], ot.ap, reads=[ot])

    def bcast_row(self, q, tile, src_row_ap, n):
        self.dma(q, tile.ap[:, 0:n], src_row_ap.to_broadcast([128, n]) if hasattr(src_row_ap, "to_broadcast") else src_row_ap, writes=[tile])

    def phase_xt(self, l, x_src, ctx_src, shift_i, scale_i, router=None, N=None, tctx=CTX, dst=None):
        S, cfg = self.S, self.cfg
        N = cfg.NG if N is None else N
        dst = self.xmT if dst is None else dst
        self.phase()
        rows = {}
        for kind in range(2):
            sc = self.AF.alloc(D, f"sc{kind}")
            sh = self.AF.alloc(D, f"sh{kind}")
            self.dma("sp", sc.ap, self.modD[l, kind:kind + 1, scale_i * D:(scale_i + 1) * D].to_broadcast([128, D]), writes=[sc])
            self.dma("act", sh.ap, self.modD[l, kind:kind + 1, shift_i * D:(shift_i + 1) * D].to_broadcast([128, D]), writes=[sh])
            S.op("dve", lambda e, sc=sc: e.tensor_scalar_add(out=sc.ap, in0=sc.ap, scalar1=1.0), reads=[sc], writes=[sc])
            rows[kind] = (sc, sh)
        xr = Ring(self.AF.allocn(2, D, "xtile"))
        xo = Ring(self.AB.allocn(2, KC * 128, "xmt"))
        if router is not None:
            wr_ap, br_ap = router
            self.combT = self.tbuf("combT", [8, N])
            WR = self.AF.alloc(KC * 8, "WR")
            self.dma("sp", WR.ap, wr_ap, writes=[WR])
            BR = self.AF.alloc(8, "BR")
            self.dma("sp", BR.ap, br_ap, writes=[BR])
            hr = Ring(self.AF.allocn(2, D, "hT"))
            lr = Ring(self.AF.allocn(2, 40, "lg"))
            cr = Ring(self.AF.allocn(4, 128, "cT", parts=8))
        for ti in range(N // 128):
            kind = 1 if ti * 128 < tctx else 0
            src = ctx_src[ti * 128:(ti + 1) * 128, :] if kind == 1 else x_src[ti * 128 - tctx:(ti + 1) * 128 - tctx, :]
            xt = xr.next()
            self.dma("sp" if ti % 2 == 0 else "act", xt.ap, src, writes=[xt])
            sc, sh = rows[kind]
            S.op("dve", lambda e, xt=xt, sc=sc: e.tensor_tensor(out=xt.ap, in0=xt.ap, in1=sc.ap, op=ALU.mult), reads=[xt, sc], writes=[xt])
            S.op("pool", lambda e, xt=xt, sh=sh: e.tensor_tensor(out=xt.ap, in0=xt.ap, in1=sh.ap, op=ALU.add), reads=[xt, sh], writes=[xt])
            ot = xo.next()
            for g in range(4):
                ps = self.psr.next()
                for j in range(4):
                    k = g * 4 + j
                    S.op("pe", lambda e, ps=ps, xt=xt, j=j, k=k: e.transpose(ps.ap[:, j * 128:(j + 1) * 128], xt.ap[:, k * 128:(k + 1) * 128], self.ident.ap),
                         reads=[xt, self.ident], writes=[ps], acc=j > 0)
                if router is not None:
                    if g == 0:
                        HT = hr.next()
                    if g % 2 == 0:
                        S.op("act", lambda e, ps=ps, HT=HT, g=g: e.copy(out=HT.ap[:, g * 512:(g + 1) * 512], in_=ps.ap), reads=[ps], writes=[HT], acc=True)
                    else:
                        S.op("dve", lambda e, ps=ps, HT=HT, g=g: e.tensor_copy(out=HT.ap[:, g * 512:(g + 1) * 512], in_=ps.ap), reads=[ps], writes=[HT], acc=True)
                eng = "act" if g % 2 == 0 else "dve"
                if eng == "act":
                    S.op("act", lambda e, ps=ps, ot=ot, g=g: e.copy(out=ot.ap[:, g * 512:(g + 1) * 512], in_=ps.ap), reads=[ps], writes=[ot], acc=True)
                else:
                    S.op("dve", lambda e, ps=ps, ot=ot, g=g: e.tensor_copy(out=ot.ap[:, g * 512:(g + 1) * 512], in_=ps.ap), reads=[ps], writes=[ot], acc=True)
            self.dma("sp", dst[:, :, ti * 128:(ti + 1) * 128], ot.ap.rearrange("p (k t) -> p k t", k=KC), reads=[ot])
            if router is not None:
                if "r0" in cfg.dbg: continue
                psL = self.psr.next()
                for k in range(KC):
                    S.op("pe", lambda e, psL=psL, HT=HT, k=k: e.matmul(psL.ap[0:8, 0:128], lhsT=WR.ap[:, k * 8:(k + 1) * 8], rhs=HT.ap[:, k * 128:(k + 1) * 128], start=(k == 0), stop=(k == KC - 1)), reads=[HT, WR], writes=[psL], acc=k > 0)
                if "r05" in cfg.dbg: continue
                LT = cr.next()
                S.op("act", lambda e: e.copy(out=LT.ap, in_=psL.ap[0:8, 0:128]), reads=[psL], writes=[LT])
                psL = self.psr.next()
                S.op("pe", lambda e: e.transpose(psL.ap[:, 0:8], LT.ap, self.ident.ap[0:8, 0:8]), reads=[LT, self.ident], writes=[psL])
                if "r1" in cfg.dbg: continue
                Lg = lr.next()
                L = Lg.ap[:, 0:8]; E1 = Lg.ap[:, 8:16]; L2 = Lg.ap[:, 16:24]; Pm = Lg.ap[:, 24:32]; sc_ = Lg.ap[:, 32:40]
                S.op("dve", lambda e: e.tensor_tensor(out=L, in0=psL.ap[:, 0:8], in1=BR.ap, op=ALU.add), reads=[psL, BR], writes=[Lg])
                S.op("dve", lambda e: e.reduce_max(out=sc_[:, 0:1], in_=L, axis=AX.X), reads=[Lg], writes=[Lg])
                S.op("dve", lambda e: e.tensor_scalar(out=E1, in0=L, scalar1=sc_[:, 0:1], scalar2=None, op0=ALU.is_equal), reads=[Lg], writes=[Lg])
                S.op("dve", lambda e: e.scalar_tensor_tensor(out=L2, in0=E1, scalar=-1e30, in1=L, op0=ALU.mult, op1=ALU.add), reads=[Lg], writes=[Lg])
                S.op("dve", lambda e: e.reduce_max(out=sc_[:, 1:2], in_=L2, axis=AX.X), reads=[Lg], writes=[Lg])
                S.op("dve", lambda e: e.tensor_scalar(out=E1, in0=L, scalar1=sc_[:, 1:2], scalar2=None, op0=ALU.is_ge), reads=[Lg], writes=[Lg])
                S.op("dve", lambda e: e.tensor_scalar_mul(out=sc_[:, 2:3], in0=sc_[:, 0:1], scalar1=-1.0), reads=[Lg], writes=[Lg])
                S.op("act", lambda e: e.activation(out=Pm, in_=L, func=AF.Exp, bias=sc_[:, 2:3]), reads=[Lg], writes=[Lg])
                S.op("dve", lambda e: e.tensor_tensor(out=Pm, in0=Pm, in1=E1, op=ALU.mult), reads=[Lg], writes=[Lg])
                S.op("dve", lambda e: e.reduce_sum(out=sc_[:, 3:4], in_=Pm, axis=AX.X), reads=[Lg], writes=[Lg])
                S.op("dve", lambda e: e.reciprocal(out=sc_[:, 4:5], in_=sc_[:, 3:4]), reads=[Lg], writes=[Lg])
                S.op("dve", lambda e: e.tensor_scalar_mul(out=Pm, in0=Pm, scalar1=sc_[:, 4:5]), reads=[Lg], writes=[Lg])
                if "r2" in cfg.dbg: continue
                psT = self.psr.next()
                S.op("pe", lambda e: e.transpose(psT.ap[0:8, 0:128], Pm, self.ident.ap), reads=[Lg, self.ident], writes=[psT])
                CT = cr.next()
                S.op("act", lambda e: e.copy(out=CT.ap, in_=psT.ap[0:8, 0:128]), reads=[psT], writes=[CT])
                self.dma("act", self.combT[:, ti * 128:(ti + 1) * 128], CT.ap, reads=[CT])

    def proj_fm(self, W, c0, ncols, tok_blocks, sink, wring, xring):
        S = self.S
        wt = wring.next()
        wv = wt.ap.rearrange("p (k n) -> p k n", k=KC)
        self.dma("pool", wv[:, :, 0:ncols], W[:, c0:c0 + ncols].rearrange("(k p) n -> p k n", p=128), writes=[wt])
        chunks = blocks(0, ncols, 128)
        for (g0, nb) in tok_blocks:
            xb = xring.next()
            xv = xb.ap.rearrange("p (k t) -> p k t", k=KC)
            self.dma("sp", xv[:, :, 0:nb], self.xmT[:, :, g0:g0 + nb], writes=[xb])
            for ci, (cc0, cn) in enumerate(chunks):
                ps = self.psr.next()
                for k in range(KC):
                    S.op("pe", lambda e, ps=ps, wv=wv, xv=xv, k=k, cc0=cc0, cn=cn, nb=nb: e.matmul(ps.ap[0:cn, 0:nb], lhsT=wv[:, k, cc0:cc0 + cn], rhs=xv[:, k, 0:nb], start=(k == 0), stop=(k == KC - 1)),
                         reads=[wt, xb], writes=[ps], acc=k > 0)
                sink(ci, c0 + cc0, cn, g0, nb, ps)

    def layer(self, l, x_in, ctx_in, w_in):
        cfg, S = self.cfg, self.S
        NG = cfg.NG
        if l == cfg.layers[0]:
            self.phase_xt(l, x_in, ctx_in, 0, 1)
        else:
            self.phase_xt(l, self.xres[1][CTX:NG, :], self.xres[1][0:CTX, :], 0, 1)
        self.phase()
        W = w_in[l]
        if not hasattr(self, "pfm"):
            self.pfm = self.dram("pfm", [N_IN - 8192, NG])
        wring = Ring(self.AB.allocn(2, KC * 512, "wt"))
        xring = Ring(self.AB.allocn(2, KC * 512, "xb"))
        oring = Ring(self.AF.allocn(4, 512, "ost"))
        cnt = [0]

        def sink(ci, col, cn, g0, nb, ps):
            ot = oring.next()
            if cnt[0] % 2 == 0:
                S.op("act", lambda e: e.copy(out=ot.ap[0:cn, 0:nb], in_=ps.ap[0:cn, 0:nb]), reads=[ps], writes=[ot])
            else:
                S.op("dve", lambda e: e.tensor_copy(out=ot.ap[0:cn, 0:nb], in_=ps.ap[0:cn, 0:nb]), reads=[ps], writes=[ot])
            cnt[0] += 1
            self.dma("act", self.pfm[col:col + cn, g0:g0 + nb], ot.ap[0:cn, 0:nb], reads=[ot])
        tb = blocks(0, NG, 512)
        for c0 in range(0, C_GATE, 512):
            self.proj_fm(W, c0, min(512, C_GATE - c0), tb, sink, wring, xring)
        V = self.V
        if "inj" in cfg.dbg:
            if not hasattr(self, "yT"):
                self.yT = self.inp("yT_in", [4, 512, NG], BF16)
        else:
            if "nolru" not in cfg.dbg:
                self.lru_phase(l, V)
            elif not hasattr(self, "yT"):
                self.yT = self.dram("yT", [4, 512, NG], BF16)
            if "nossd" not in cfg.dbg:
                self.ssd_phase(l, V)
            if "noml" not in cfg.dbg:
                self.mlstm_phase(l, V)
            zt = []
            if "nolru" in cfg.dbg: zt.append(self.yT[0])
            if "nossd" in cfg.dbg: zt.append(self.yT[2])
            if "noml" in cfg.dbg: zt.append(self.yT[3])
            if "nona" not in cfg.dbg:
                self.na_phase(l, V)
            else:
                zt.append(self.yT[1])
            if zt:
                self.zero_fill_phase(zt)
        if "stopmix" in cfg.dbg:
            return
        if not hasattr(self, "xres"):
            self.xres = [self.dram("xres0", [NG, D]), self.dram("xres1", [NG, D])]
        last = (l == cfg.layers[-1])
        own = last and cfg.NS > 1
        router = (V["moe_wr_p"][l // 2], V["moe_br_p"][l // 2]) if (l % 2 == 1 and "norouter" not in cfg.dbg) else None
        xs, cs = (x_in, ctx_in) if l == cfg.layers[0] else (self.xres[1][CTX:NG, :], self.xres[1][0:CTX, :])
        if not own:
            self.TN, self.t_ctx, self.dyn_base = NG, CTX, None
            self.merge_phase(l, W, V["w_branch"][l], V["w_out"][l])
            self.epi_phase(l, xs, cs, 2, 0, self.xres[0], V["ln_g"], V["ln_b"])
            self.xmT2 = self.xmT
            self.phase_xt(l, self.xres[0][CTX:NG, :], self.xres[0][0:CTX, :], 3, 4, router=router)
            x1x, x1c = self.xres[0][CTX:NG, :], self.xres[0][0:CTX, :]
            dst2 = self.xres[1]
        else:
            TL = cfg.TL
            self.phase()
            yT_o = self.tbuf("yT_o", [4, 512, TL], BF16); xmT_o = self.tbuf("xmT_o", [128, KC, TL], BF16); x_o = self.tbuf("x_o", [TL, D])
            for n in range(4):
                S.op("sp", lambda h, n=n: h.dma_start(out=yT_o[n], in_=self.yT_full[n][:, CTX:NG][:, bass.ds(self.sval, TL)]), dma=True, late=True)
            for kh in range(2):
                S.op("sp", lambda h, kh=kh: h.dma_start(out=xmT_o[:, kh * 8:(kh + 1) * 8, :], in_=self.xmT_full[:, kh * 8:(kh + 1) * 8, CTX:NG][:, :, bass.ds(self.sval, TL)]), dma=True, late=True)
            for q in range(4):
                S.op("sp", lambda h, q=q: h.dma_start(out=x_o[q * (TL // 4):(q + 1) * (TL // 4), :], in_=xs[bass.ds(self.sval, TL), :][q * (TL // 4):(q + 1) * (TL // 4), :]), dma=True, late=True)
            self.yT_full, self.xmT_full = self.yT, self.xmT
            self.yT, self.xmT = yT_o, xmT_o
            self.TN, self.t_ctx, self.dyn_base = TL, 0, None
            self.merge_phase(l, W, V["w_branch"][l], V["w_out"][l])
            xo = self.tbuf("xres_o0", [TL, D])
            self.epi_phase(l, x_o, None, 2, 0, xo, V["ln_g"], V["ln_b"])
            self.xmT2 = self.tbuf("xmT2", [128, KC, TL], BF16)
            self.phase_xt(l, xo, None, 3, 4, router=router, N=TL, tctx=0, dst=self.xmT2)
            self.yT, self.xmT = self.yT_full, self.xmT_full
            x1x, x1c = xo, None
            dst2 = self.tbuf("xres_o1", [TL, D])
        if "stopxt2" in cfg.dbg and l % 2 == 1:
            return
        if l % 2 == 0:
            self.ffn_dense_phase(l, V["ffn_w_gate"][l // 2], V["ffn_w_up"][l // 2], V["ffn_w_down"][l // 2], cfg.FD)
        else:
            self.moe_phase(l, V)
        self.epi_phase(l, x1x, x1c, 5, 1, dst2, V["ln_g"], V["ln_b"], out_ext=self.y_out if last else None)
        self.TN, self.t_ctx, self.dyn_base = NG, CTX, None


def rev_ap(ap2d, n):
    return AP(ap2d.tensor, ap2d.offset + (n - 1) * ap2d.ap[-1][0], [list(ap2d.ap[0]), [-ap2d.ap[-1][0], n]])


def _segments(cfg):
    return [(0, CTX), (CTX, cfg.NG)]


def lru_phase(self, l, V):
    cfg, S = self.cfg, self.S
    NG = cfg.NG
    NB = 1024
    if not hasattr(self, "hD"):
        self.hD = self.dram("hD", [2, 512, NG])
        self.yT = self.dram("yT", [4, 512, NG], BF16)
    self.phase()
    AFa, ABa = self.AF, self.AB
    cw = AFa.alloc(16, "cw"); self.dma("sp", cw.ap, V["lru_cw"][l].rearrange("p c k -> p (c k)"), writes=[cw])
    cb = AFa.alloc(4, "cb"); self.dma("sp", cb.ap, V["lru_cb"][l], writes=[cb])
    pv = AFa.alloc(24, "pv"); self.dma("sp", pv.ap, V["lru_vec"][l].rearrange("p a c -> p (a c)"), writes=[pv])
    spc = AFa.alloc(8, "spc")
    S.op("act", lambda e: e.activation(out=spc.ap, in_=pv.ap[:, 16:24], func=AF.Exp, scale=-1.0), reads=[pv], writes=[spc])
    S.op("act", lambda e: e.activation(out=spc.ap, in_=spc.ap, func=AF.Ln, bias=1.0), reads=[spc], writes=[spc])
    S.op("dve", lambda e: e.tensor_scalar_mul(out=spc.ap, in0=spc.ap, scalar1=-8.0), reads=[spc], writes=[spc])
    wbd = {}
    for d in range(2):
        for wi, nm in enumerate(("lru_wa", "lru_wx")):
            for c in range(4):
                t = ABa.alloc(128, f"wbd{d}{wi}{c}")
                S.op("pool", lambda e, t=t: e.memset(t.ap, 0.0), writes=[t])
                for hblk in range(2):
                    self.dma("pool", t.ap[hblk * 64:(hblk + 1) * 64, hblk * 64:(hblk + 1) * 64], V[nm][l, d, 2 * c + hblk], writes=[t], reads=[t])
                wbd[(d, wi, c)] = t
    zero1 = AFa.alloc(1, "zero1")
    S.op("dve", lambda e: e.memset(zero1.ap, 0.0), writes=[zero1])
    names = ("XH", "XC", "R", "I", "A", "U", "H")
    rings = {n: Ring(AFa.allocn(2, NB + 4, n)) for n in names}
    xbr = Ring(ABa.allocn(2, NB, "XB"))
    carr = Ring(AFa.allocn(4, 1, "carry"))
    for c in range(4):
        for d in range(2):
            carry = zero1
            for (s0, s1) in _segments(cfg):
                blks = blocks(s0, s1, NB)
                if d == 1:
                    blks = blks[::-1]
                for (g0, nb) in blks:
                    XH, XC, R, I, A, U, H = [rings[n].next() for n in names]
                    XB = xbr.next()
                    lo = max(g0 - 2, s0); hi = min(g0 + nb + 1, s1)
                    S.op("pool", lambda e, XH=XH: e.memset(XH.ap[:, 0:NB + 4], 0.0), writes=[XH])
                    self.dma("sp", XH.ap[:, lo - (g0 - 2):hi - (g0 - 2)], self.pfm[c * 128:(c + 1) * 128, lo:hi], writes=[XH], reads=[XH])
                    S.op("dve", lambda e, XH=XH, XC=XC, nb=nb, c=c: e.tensor_scalar(out=XC.ap[:, 0:nb], in0=XH.ap[:, 0:nb], scalar1=cw.ap[:, c * 4:c * 4 + 1], scalar2=cb.ap[:, c:c + 1], op0=ALU.mult, op1=ALU.add), reads=[XH, cw, cb], writes=[XC])
                    for k in range(1, 4):
                        S.op("dve", lambda e, XH=XH, XC=XC, nb=nb, c=c, k=k: e.scalar_tensor_tensor(out=XC.ap[:, 0:nb], in0=XH.ap[:, k:k + nb], scalar=cw.ap[:, c * 4 + k:c * 4 + k + 1], in1=XC.ap[:, 0:nb], op0=ALU.mult, op1=ALU.add), reads=[XH, XC, cw], writes=[XC])
                    S.op("act", lambda e, XB=XB, XC=XC, nb=nb: e.copy(out=XB.ap[:, 0:nb], in_=XC.ap[:, 0:nb]), reads=[XC], writes=[XB])
                    for wi, (dst, bcol) in enumerate(((R, 0 + d), (I, 2 + d))):
                        for (o, n) in blocks(0, nb, 512):
                            ps = self.psr.next()
                            w = wbd[(d, wi, c)]
                            S.op("pe", lambda e, ps=ps, w=w, XB=XB, o=o, n=n: e.matmul(ps.ap[:, 0:n], lhsT=w.ap, rhs=XB.ap[:, o:o + n], start=True, stop=True), reads=[w, XB], writes=[ps])
                            S.op("act", lambda e, ps=ps, dst=dst, o=o, n=n, bcol=bcol, c=c: e.activation(out=dst.ap[:, o:o + n], in_=ps.ap[:, 0:n], func=AF.Sigmoid, bias=pv.ap[:, bcol * 4 + c:bcol * 4 + c + 1]), reads=[ps, pv], writes=[dst], acc=True)
                    S.op("act", lambda e, A=A, R=R, nb=nb, c=c, d=d: e.activation(out=A.ap[:, 0:nb], in_=R.ap[:, 0:nb], func=AF.Exp, scale=spc.ap[:, d * 4 + c:d * 4 + c + 1]), reads=[R, spc], writes=[A])
                    S.op("dve", lambda e, A=A, R=R, nb=nb: e.tensor_tensor(out=R.ap[:, 0:nb], in0=A.ap[:, 0:nb], in1=A.ap[:, 0:nb], op=ALU.mult), reads=[A], writes=[R])
                    S.op("act", lambda e, R=R, nb=nb: e.activation(out=R.ap[:, 0:nb], in_=R.ap[:, 0:nb], func=AF.Sqrt, scale=-1.0, bias=1.0), reads=[R], writes=[R])
                    S.op("dve", lambda e, U=U, R=R, I=I, nb=nb: e.tensor_tensor(out=U.ap[:, 0:nb], in0=R.ap[:, 0:nb], in1=I.ap[:, 0:nb], op=ALU.mult), reads=[R, I], writes=[U])
                    S.op("dve", lambda e, U=U, XC=XC, nb=nb: e.tensor_tensor(out=U.ap[:, 0:nb], in0=U.ap[:, 0:nb], in1=XC.ap[:, 0:nb], op=ALU.mult), reads=[U, XC], writes=[U])
                    f = (lambda a, nb=nb: a[:, 0:nb]) if d == 0 else (lambda a, nb=nb: rev_ap(a[:, 0:nb], nb))
                    S.op("dve", lambda e, H=H, A=A, U=U, f=f, carry=carry: e.tensor_tensor_scan(out=f(H.ap), data0=f(A.ap), data1=f(U.ap), initial=carry.ap[:, 0:1], op0=ALU.mult, op1=ALU.add), reads=[A, U, carry], writes=[H])
                    ncar = carr.next()
                    lastcol = nb - 1 if d == 0 else 0
                    S.op("act", lambda e, ncar=ncar, H=H, lastcol=lastcol: e.copy(out=ncar.ap, in_=H.ap[:, lastcol:lastcol + 1]), reads=[H], writes=[ncar])
                    carry = ncar
                    self.dma("act", self.hD[d, c * 128:(c + 1) * 128, g0:g0 + nb], H.ap[:, 0:nb], reads=[H])
    self.phase()
    rr = {n: Ring(self.AF.allocn(2, NB, n)) for n in ("H0", "H1", "G", "T")}
    yo = Ring(self.AB.allocn(2, NB, "Y"))
    for c in range(4):
        for (g0, nb) in blocks(0, NG, NB):
            H0, H1, G, T = [rr[n].next() for n in ("H0", "H1", "G", "T")]
            Y = yo.next()
            self.dma("sp", H0.ap[:, 0:nb], self.hD[0, c * 128:(c + 1) * 128, g0:g0 + nb], writes=[H0])
            self.dma("act", H1.ap[:, 0:nb], self.hD[1, c * 128:(c + 1) * 128, g0:g0 + nb], writes=[H1])
            self.dma("sp", G.ap[:, 0:nb], self.pfm[C_LRU_G + c * 128:C_LRU_G + (c + 1) * 128, g0:g0 + nb], writes=[G])
            S.op("pool", lambda e, H0=H0, H1=H1, nb=nb: e.tensor_tensor(out=H0.ap[:, 0:nb], in0=H0.ap[:, 0:nb], in1=H1.ap[:, 0:nb], op=ALU.add), reads=[H0, H1], writes=[H0])
            S.op("dve", lambda e, T=T, G=G, nb=nb: e.tensor_tensor(out=T.ap[:, 0:nb], in0=G.ap[:, 0:nb], in1=G.ap[:, 0:nb], op=ALU.mult), reads=[G], writes=[T])
            S.op("dve", lambda e, T=T, nb=nb: e.tensor_scalar(out=T.ap[:, 0:nb], in0=T.ap[:, 0:nb], scalar1=0.044715, scalar2=1.0, op0=ALU.mult, op1=ALU.add), reads=[T], writes=[T])
            S.op("dve", lambda e, T=T, G=G, nb=nb: e.tensor_tensor(out=T.ap[:, 0:nb], in0=T.ap[:, 0:nb], in1=G.ap[:, 0:nb], op=ALU.mult), reads=[T, G], writes=[T])
            S.op("act", lambda e, T=T, nb=nb: e.activation(out=T.ap[:, 0:nb], in_=T.ap[:, 0:nb], func=AF.Sigmoid, scale=1.5957691216057308), reads=[T], writes=[T])
            S.op("dve", lambda e, T=T, G=G, nb=nb: e.tensor_tensor(out=T.ap[:, 0:nb], in0=T.ap[:, 0:nb], in1=G.ap[:, 0:nb], op=ALU.mult), reads=[T, G], writes=[T])
            S.op("dve", lambda e, T=T, H0=H0, Y=Y, nb=nb: e.tensor_tensor(out=Y.ap[:, 0:nb], in0=T.ap[:, 0:nb], in1=H0.ap[:, 0:nb], op=ALU.mult), reads=[T, H0], writes=[Y])
            self.dma("sp", self.yT[0, c * 128:(c + 1) * 128, g0:g0 + nb], Y.ap[:, 0:nb], reads=[Y])


K.lru_phase = lru_phase


def conv_silu_phase(self, l, row0, nchunks, cwv, cbv, dst, post=None, out_f32=False):
    cfg, S = self.cfg, self.S
    NB = 1024
    self.phase()
    cw = self.AF.alloc(nchunks * 4, "cw"); self.dma("sp", cw.ap, cwv.rearrange("p c k -> p (c k)"), writes=[cw])
    cb = self.AF.alloc(nchunks, "cb"); self.dma("sp", cb.ap, cbv, writes=[cb])
    rXH = Ring(self.AF.allocn(2, NB + 4, "XH")); rXC = Ring(self.AF.allocn(2, NB, "XC")); rSG = Ring(self.AF.allocn(2, NB, "SG"))
    rO = Ring((self.AF if out_f32 else self.AB).allocn(2, NB, "O"))
    ctxp = dict(rings={})
    for c in range(nchunks):
        for (s0, s1) in _segments(cfg):
            for (g0, nb) in blocks(s0, s1, NB):
                XH, XC, SG, O = rXH.next(), rXC.next(), rSG.next(), rO.next()
                lo = max(g0 - 2, s0); hi = min(g0 + nb + 1, s1)
                S.op("pool", lambda e, XH=XH: e.memset(XH.ap[:, 0:NB + 4], 0.0), writes=[XH])
                self.dma("sp", XH.ap[:, lo - (g0 - 2):hi - (g0 - 2)], self.pfm[row0 + c * 128:row0 + (c + 1) * 128, lo:hi], writes=[XH], reads=[XH])
                S.op("dve", lambda e, XH=XH, XC=XC, nb=nb, c=c: e.tensor_scalar(out=XC.ap[:, 0:nb], in0=XH.ap[:, 0:nb], scalar1=cw.ap[:, c * 4:c * 4 + 1], scalar2=cb.ap[:, c:c + 1], op0=ALU.mult, op1=ALU.add), reads=[XH, cw, cb], writes=[XC])
                for k in range(1, 4):
                    S.op("dve", lambda e, XH=XH, XC=XC, nb=nb, c=c, k=k: e.scalar_tensor_tensor(out=XC.ap[:, 0:nb], in0=XH.ap[:, k:k + nb], scalar=cw.ap[:, c * 4 + k:c * 4 + k + 1], in1=XC.ap[:, 0:nb], op0=ALU.mult, op1=ALU.add), reads=[XH, XC, cw], writes=[XC])
                if post is None:
                    S.op("act", lambda e, O=O, XC=XC, nb=nb: e.activation(out=O.ap[:, 0:nb], in_=XC.ap[:, 0:nb], func=AF.Silu), reads=[XC], writes=[O])
                else:
                    S.op("act", lambda e, SG=SG, XC=XC, nb=nb: e.activation(out=SG.ap[:, 0:nb], in_=XC.ap[:, 0:nb], func=AF.Silu), reads=[XC], writes=[SG])
                    post(c, s0, g0, nb, SG, O, ctxp)
                self.dma("act", dst[c * 128:(c + 1) * 128, g0:g0 + nb], O.ap[:, 0:nb], reads=[O])


def softplus_ops(self, X, T, n, parts):
    S = self.S
    x = X.ap[0:parts, 0:n]; t = T.ap[0:parts, 0:n]
    S.op("act", lambda e: e.activation(out=t, in_=x, func=AF.Abs), reads=[X], writes=[T])
    S.op("act", lambda e: e.activation(out=t, in_=t, func=AF.Exp, scale=-1.0), reads=[T], writes=[T])
    S.op("act", lambda e: e.activation(out=t, in_=t, func=AF.Ln, bias=1.0), reads=[T], writes=[T])
    S.op("dve", lambda e: e.tensor_scalar_max(out=x, in0=x, scalar1=0.0), reads=[X], writes=[X])
    S.op("dve", lambda e: e.tensor_tensor(out=x, in0=x, in1=t, op=ALU.add), reads=[X, T], writes=[X])


def ssd_tab_phase(self, l, V):
    cfg, S = self.cfg, self.S
    NG = cfg.NG
    NB = 1024
    if not hasattr(self, "tabS"):
        self.tabS = self.dram("tabS", [2, 5, 8, NG])
    self.phase()
    pr = self.AF.alloc(4, "ssdp", parts=8)
    self.dma("sp", pr.ap, V["ssd_hp"][l], writes=[pr])
    Aneg = self.AF.alloc(2, "Aneg", parts=8)
    S.op("act", lambda e: e.activation(out=Aneg.ap, in_=pr.ap[:, 2:4], func=AF.Exp), reads=[pr], writes=[Aneg])
    S.op("dve", lambda e: e.tensor_scalar_mul(out=Aneg.ap, in0=Aneg.ap, scalar1=-1.0), reads=[Aneg], writes=[Aneg])
    msk = {}
    for d in range(2):
        m = self.AF.alloc(NB, f"rmask{d}", parts=8)
        S.op("dve", lambda e, m=m: e.memset(m.ap, 1.0), writes=[m])
        mv = m.ap.rearrange("p (c t) -> p c t", t=128)
        pos = 0 if d == 0 else 127
        S.op("dve", lambda e, mv=mv, pos=pos: e.memset(mv[:, :, pos:pos + 1], 0.0), writes=[m], reads=[m])
        msk[d] = m
    names = ("DT", "T", "DA", "CUM", "LAST", "W", "E", "DL")
    rr = {n: Ring(self.AF.allocn(2, NB, n, parts=8)) for n in names}
    for d in range(2):
        for (g0, nb) in blocks(0, NG, NB):
            DT, T, DA, CUM, LAST, W, E, DL = [rr[n].next() for n in names]
            sl = lambda X: X.ap[:, 0:nb]
            self.dma("sp", sl(DT), self.pfm[C_SSD_DT + d * 8:C_SSD_DT + d * 8 + 8, g0:g0 + nb], writes=[DT])
            S.op("dve", lambda e, DT=DT: e.tensor_scalar_add(out=DT.ap[:, 0:nb], in0=DT.ap[:, 0:nb], scalar1=pr.ap[:, d:d + 1]), reads=[DT, pr], writes=[DT])
            softplus_ops(self, DT, T, nb, 8)
            S.op("dve", lambda e, DA=DA, DT=DT: e.tensor_scalar_mul(out=DA.ap[:, 0:nb], in0=DT.ap[:, 0:nb], scalar1=Aneg.ap[:, d:d + 1]), reads=[DT, Aneg], writes=[DA])
            f = (lambda a: a) if d == 0 else (lambda a: rev_ap(a, nb))
            S.op("dve", lambda e, CUM=CUM, DA=DA, f=f, m=msk[d]: e.tensor_tensor_scan(out=f(CUM.ap[:, 0:nb]), data0=f(m.ap[:, 0:nb]), data1=f(DA.ap[:, 0:nb]), initial=0.0, op0=ALU.mult, op1=ALU.add), reads=[DA, msk[d]], writes=[CUM])
            endpos = 127 if d == 0 else 0
            cv = CUM.ap[:, 0:nb].rearrange("p (c t) -> p c t", t=128)
            lv = LAST.ap[:, 0:nb].rearrange("p (c t) -> p c t", t=128)
            S.op("dve", lambda e, cv=cv, lv=lv, CUM=CUM, LAST=LAST: e.tensor_copy(out=lv, in_=cv[:, :, endpos:endpos + 1].to_broadcast([8, nb // 128, 128])), reads=[CUM], writes=[LAST])
            S.op("dve", lambda e, W=W, LAST=LAST, CUM=CUM: e.tensor_tensor(out=W.ap[:, 0:nb], in0=LAST.ap[:, 0:nb], in1=CUM.ap[:, 0:nb], op=ALU.subtract), reads=[LAST, CUM], writes=[W])
            S.op("act", lambda e, W=W: e.activation(out=W.ap[:, 0:nb], in_=W.ap[:, 0:nb], func=AF.Exp), reads=[W], writes=[W])
            S.op("dve", lambda e, W=W, DT=DT: e.tensor_tensor(out=W.ap[:, 0:nb], in0=W.ap[:, 0:nb], in1=DT.ap[:, 0:nb], op=ALU.mult), reads=[W, DT], writes=[W])
            S.op("act", lambda e, E=E, CUM=CUM: e.activation(out=E.ap[:, 0:nb], in_=CUM.ap[:, 0:nb], func=AF.Exp), reads=[CUM], writes=[E])
            S.op("act", lambda e, DL=DL, LAST=LAST: e.activation(out=DL.ap[:, 0:nb], in_=LAST.ap[:, 0:nb], func=AF.Exp), reads=[LAST], writes=[DL])
            for qi, X in enumerate((CUM, DT, W, E, DL)):
                self.dma("sp" if qi % 2 else "act", self.tabS[d, qi, :, g0:g0 + nb], X.ap[:, 0:nb], reads=[X])


def chunk_order(cfg, d):
    nctx = CTX // 128
    a = list(range(0, nctx)); b = list(range(nctx, cfg.NT))
    return a + b if d == 0 else a[::-1] + b[::-1]


def ssd_main_phase(self, l, V):
    cfg, S = self.cfg, self.S
    NG = cfg.NG
    if not hasattr(self, "ytm"):
        self.ytm = self.dram("ytm", [NG, 512])
    for d in range(2):
        self.phase()
        AFa, ABa = self.AF, self.AB
        tri = AFa.alloc(128, "tri"); self.dma("sp", tri.ap, V["tri"][d], writes=[tri])
        sel = AFa.alloc(8 * 128, "sel", parts=40); self.dma("sp", sel.ap, V["sel40"].rearrange("r h p -> r (h p)"), writes=[sel])
        dsk = AFa.alloc(512, "dsk"); self.dma("sp", dsk.ap, V["ssd_dskip"][l:l + 1, :].to_broadcast([128, 512]), writes=[dsk])
        gn = AFa.alloc(512, "gn"); self.dma("sp", gn.ap, V["ssd_gn"][l:l + 1, :].to_broadcast([128, 512]), writes=[gn])
        H = AFa.alloc(512, "H"); S.op("dve", lambda e: e.memset(H.ap, 0.0), writes=[H])
        Hb = ABa.alloc(512, "Hb"); S.op("dve", lambda e: e.memset(Hb.ap, 0.0), writes=[Hb])
        rXB = Ring(ABa.allocn(2, 6 * 128, "xbc")); rTAB = Ring(AFa.allocn(2, 128, "tab", parts=40)); rTT = Ring(AFa.allocn(2, 40, "TT"))
        rXT = Ring(ABa.allocn(2, 512, "Xtm")); rBT = Ring(ABa.allocn(2, 128, "Btm")); rXW = Ring(ABa.allocn(2, 512, "XW"))
        rDm = Ring(AFa.allocn(3, 128, "Dm")); rMT = Ring(ABa.allocn(3, 128, "MT"))
        rY = Ring(AFa.allocn(2, 512, "Y")); rY2 = Ring(AFa.allocn(2, 512, "Y2")); rZ = Ring(AFa.allocn(2, 512, "Z")); rZs = Ring(AFa.allocn(2, 512, "Zs"))
        rS1 = Ring(AFa.allocn(4, 2, "s1")); rYo = Ring(ABa.allocn(2, 512, "Yo"))
        col = lambda q, h: q * 8 + h
        psq = Ring(self.ps[4:8])
        for c in chunk_order(cfg, d):
            g0 = c * 128
            XB = rXB.next(); TAB = rTAB.next(); TT = rTT.next(); XT = rXT.next(); BT = rBT.next(); XW = rXW.next()
            xv = XB.ap.rearrange("p (c t) -> p c t", c=6)
            self.dma("sp", xv, self.xbcT[:, g0:g0 + 128].rearrange("(c p) t -> p c t", p=128), writes=[XB])
            self.dma("act", TAB.ap, self.tabS[d, :, :, g0:g0 + 128].rearrange("q h t -> (q h) t"), writes=[TAB])
            if "m0" in cfg.dbg: continue
            ps = psq.next()
            S.op("pe", lambda e, ps=ps, TAB=TAB: e.transpose(ps.ap[:, 0:40], TAB.ap, self.ident.ap[0:40, 0:40]), reads=[TAB, self.ident], writes=[ps])
            S.op("act", lambda e, ps=ps, TT=TT: e.copy(out=TT.ap, in_=ps.ap[:, 0:40]), reads=[ps], writes=[TT])
            if "m05" in cfg.dbg: continue
            ps = psq.next()
            pb = ps.ap.bitcast(BF16)
            for j in range(5):
                S.op("pe", lambda e, pb=pb, xv=xv, j=j: e.transpose(pb[:, j * 128:(j + 1) * 128], xv[:, j, :], self.identb.ap), reads=[XB, self.identb], writes=[ps], acc=j > 0)
            S.op("act", lambda e, pb=pb, XT=XT: e.copy(out=XT.ap, in_=pb[:, 0:512]), reads=[ps], writes=[XT])
            S.op("act", lambda e, pb=pb, BT=BT: e.copy(out=BT.ap, in_=pb[:, 512:640]), reads=[ps], writes=[BT])
            if "m1" in cfg.dbg: continue
            xt3 = XT.ap.rearrange("p (h q) -> p h q", h=8)
            S.op("dve", lambda e, XW=XW, xt3=xt3, TT=TT: e.tensor_tensor(out=XW.ap.rearrange("p (h q) -> p h q", h=8), in0=xt3, in1=TT.ap[:, col(2, 0):col(2, 0) + 8].unsqueeze(2).to_broadcast([128, 8, 64]), op=ALU.mult), reads=[XT, TT], writes=[XW])
            if "m15" in cfg.dbg: continue
            psI = self.ps[0]
            S.op("pe", lambda e, psI=psI, xv=xv: e.matmul(psI.ap, lhsT=xv[:, 5, :], rhs=Hb.ap, start=True, stop=True), reads=[XB, Hb], writes=[psI])
            if "m17" in cfg.dbg: continue
            psCs = [self.ps[1], self.ps[2]]
            for g in range(2):
                S.op("pe", lambda e, psC=psCs[g], xv=xv, g=g: e.matmul(psC.ap[:, 0:128], lhsT=xv[g * 64:(g + 1) * 64, 4, :], rhs=xv[g * 64:(g + 1) * 64, 5, :], start=True, stop=True), reads=[XB], writes=[psCs[g]])
            if "m2" in cfg.dbg: continue
            psY = self.ps[3]
            for h in range(8):
                g = h // 4
                psR = psq.next()
                S.op("pe", lambda e, psR=psR, TAB=TAB, h=h: e.matmul(psR.ap[:, 0:128], lhsT=sel.ap[:, h * 128:(h + 1) * 128], rhs=TAB.ap, start=True, stop=True), reads=[sel, TAB], writes=[psR])
                Dm = rDm.next(); MT = rMT.next()
                S.op("dve", lambda e, Dm=Dm, psR=psR, TT=TT, h=h: e.tensor_scalar(out=Dm.ap, in0=psR.ap[:, 0:128], scalar1=TT.ap[:, col(0, h):col(0, h) + 1], scalar2=0.0, op0=ALU.subtract, op1=ALU.min), reads=[psR, TT], writes=[Dm])
                S.op("act", lambda e, Dm=Dm: e.activation(out=Dm.ap, in_=Dm.ap, func=AF.Exp), reads=[Dm], writes=[Dm])
                S.op("dve", lambda e, Dm=Dm, TT=TT, h=h: e.scalar_tensor_tensor(out=Dm.ap, in0=Dm.ap, scalar=TT.ap[:, col(1, h):col(1, h) + 1], in1=tri.ap, op0=ALU.mult, op1=ALU.mult), reads=[Dm, TT, tri], writes=[Dm])
                S.op("dve", lambda e, MT=MT, Dm=Dm, psC=psCs[g]: e.tensor_tensor(out=MT.ap, in0=Dm.ap, in1=psC.ap[:, 0:128], op=ALU.mult), reads=[Dm, psCs[g]], writes=[MT])
                S.op("pe", lambda e, psY=psY, MT=MT, XT=XT, h=h: e.matmul(psY.ap[:, h * 64:(h + 1) * 64], lhsT=MT.ap, rhs=XT.ap[:, h * 64:(h + 1) * 64], start=True, stop=True), reads=[MT, XT], writes=[psY], acc=h > 0)
            if "m3" in cfg.dbg: continue
            psS = psq.next()
            S.op("pe", lambda e, psS=psS, BT=BT, XW=XW: e.matmul(psS.ap, lhsT=BT.ap, rhs=XW.ap, start=True, stop=True), reads=[BT, XW], writes=[psS])
            for g in range(2):
                hb = H.ap[g * 64:(g + 1) * 64, g * 256:(g + 1) * 256]
                S.op("dve", lambda e, hb=hb, TT=TT, g=g: e.tensor_tensor(out=hb.rearrange("p (h q) -> p h q", h=4), in0=hb.rearrange("p (h q) -> p h q", h=4), in1=TT.ap[g * 64:(g + 1) * 64, col(4, 4 * g):col(4, 4 * g) + 4].unsqueeze(2).to_broadcast([64, 4, 64]), op=ALU.mult), reads=[H, TT, psI], writes=[H])
                S.op("dve", lambda e, hb=hb, psS=psS, g=g: e.tensor_tensor(out=hb, in0=hb, in1=psS.ap[g * 64:(g + 1) * 64, g * 256:(g + 1) * 256], op=ALU.add), reads=[H, psS], writes=[H])
            S.op("act", lambda e: e.copy(out=Hb.ap, in_=H.ap), reads=[H, psI], writes=[Hb])
            if "m4" in cfg.dbg: continue
            Y = rY.next(); Y2 = rY2.next()
            S.op("dve", lambda e, Y=Y, psI=psI, TT=TT: e.tensor_tensor(out=Y.ap.rearrange("p (h q) -> p h q", h=8), in0=psI.ap.rearrange("p (h q) -> p h q", h=8), in1=TT.ap[:, col(3, 0):col(3, 0) + 8].unsqueeze(2).to_broadcast([128, 8, 64]), op=ALU.mult), reads=[psI, TT], writes=[Y])
            S.op("dve", lambda e, Y=Y, psY=psY: e.tensor_tensor(out=Y.ap, in0=Y.ap, in1=psY.ap, op=ALU.add), reads=[Y, psY], writes=[Y])
            if d == 0:
                S.op("pool", lambda e, Y2=Y2, XT=XT: e.tensor_tensor(out=Y2.ap, in0=XT.ap, in1=dsk.ap, op=ALU.mult), reads=[XT, dsk], writes=[Y2])
                S.op("pool", lambda e, Y=Y, Y2=Y2: e.tensor_tensor(out=Y.ap, in0=Y.ap, in1=Y2.ap, op=ALU.add), reads=[Y, Y2], writes=[Y])
                self.dma("sp", self.ytm[g0:g0 + 128, :], Y.ap, reads=[Y])
            else:
                self.dma("sp", Y2.ap, self.ytm[g0:g0 + 128, :], writes=[Y2])
                S.op("pool", lambda e, Y=Y, Y2=Y2: e.tensor_tensor(out=Y.ap, in0=Y.ap, in1=Y2.ap, op=ALU.add), reads=[Y, Y2], writes=[Y])
                Z = rZ.next(); Zs = rZs.next()
                zv = Z.ap.rearrange("p (c t) -> p c t", c=4)
                self.dma("act", zv, self.pfm[C_SSD_Z:C_SSD_Z + 512, g0:g0 + 128].rearrange("(c p) t -> p c t", p=128), writes=[Z])
                psZ = psq.next()
                for j in range(4):
                    S.op("pe", lambda e, psZ=psZ, zv=zv, j=j: e.transpose(psZ.ap[:, j * 128:(j + 1) * 128], zv[:, j, :], self.ident.ap), reads=[Z, self.ident], writes=[psZ], acc=j > 0)
                S.op("act", lambda e, Zs=Zs, psZ=psZ: e.activation(out=Zs.ap, in_=psZ.ap, func=AF.Silu), reads=[psZ], writes=[Zs])
                S.op("dve", lambda e, Y=Y, Zs=Zs: e.tensor_tensor(out=Y.ap, in0=Y.ap, in1=Zs.ap, op=ALU.mult), reads=[Y, Zs], writes=[Y])
                s1 = rS1.next()
                S.op("dve", lambda e, Zs=Zs, Y=Y: e.tensor_tensor(out=Zs.ap, in0=Y.ap, in1=Y.ap, op=ALU.mult), reads=[Y], writes=[Zs])
                S.op("dve", lambda e, s1=s1, Zs=Zs: e.reduce_sum(out=s1.ap[:, 0:1], in_=Zs.ap, axis=AX.X), reads=[Zs], writes=[s1])
                S.op("dve", lambda e, s1=s1: e.tensor_scalar(out=s1.ap[:, 0:1], in0=s1.ap[:, 0:1], scalar1=1.0 / 512, scalar2=1e-5, op0=ALU.mult, op1=ALU.add), reads=[s1], writes=[s1])
                S.op("act", lambda e, s1=s1: e.activation(out=s1.ap[:, 0:1], in_=s1.ap[:, 0:1], func=AF.Sqrt), reads=[s1], writes=[s1])
                S.op("dve", lambda e, s1=s1: e.reciprocal(out=s1.ap[:, 1:2], in_=s1.ap[:, 0:1]), reads=[s1], writes=[s1])
                S.op("dve", lambda e, Y=Y, s1=s1: e.scalar_tensor_tensor(out=Y.ap, in0=Y.ap, scalar=s1.ap[:, 1:2], in1=gn.ap, op0=ALU.mult, op1=ALU.mult), reads=[Y, s1, gn], writes=[Y])
                self.store_tm_as_fm(Y, 2, g0, rYo, psq)


def store_tm_as_fm(self, Y, branch, g0, rYo, psq=None):
    S = self.S
    ps = (psq or self.psr).next()
    for j in range(4):
        S.op("pe", lambda e, ps=ps, j=j: e.transpose(ps.ap[:, j * 128:(j + 1) * 128], Y.ap[:, j * 128:(j + 1) * 128], self.ident.ap), reads=[Y, self.ident], writes=[ps], acc=j > 0)
    Yo = rYo.next()
    S.op("act", lambda e: e.copy(out=Yo.ap, in_=ps.ap), reads=[ps], writes=[Yo])
    self.dma("sp", self.yT[branch, :, g0:g0 + 128].rearrange("(c p) t -> p c t", p=128), Yo.ap.rearrange("p (c t) -> p c t", c=4), reads=[Yo])


def ssd_phase(self, l, V):
    if not hasattr(self, "xbcT"):
        self.xbcT = self.dram("xbcT", [768, self.cfg.NG], BF16)
    conv_silu_phase(self, l, C_SSD_X, 6, V["ssd_cw"][l], V["ssd_cb"][l], self.xbcT)
    if "stop1" in self.cfg.dbg: return
    ssd_tab_phase(self, l, V)
    if "stop2" in self.cfg.dbg: return
    ssd_main_phase(self, l, V)


K.ssd_phase = ssd_phase
K.store_tm_as_fm = store_tm_as_fm


def gemm_fm(self, ranges, src, kc, tok_blocks, sink, wring, xring, wcols, tbs=512, dyn=False):
    S = self.S
    wt = wring.next()
    wv = wt.ap[:, 0:kc * wcols].rearrange("p (k n) -> p k n", k=kc)
    off = 0
    offs = []
    for (W, c0, n) in ranges:
        self.dma("pool", wv[:, :, off:off + n], W[:, c0:c0 + n].rearrange("(k p) n -> p k n", p=128), writes=[wt], reads=[wt] if off else [])
        offs.append(off)
        off += n
    for (g0, nb) in tok_blocks:
        xb = xring.next()
        xv = xb.ap[:, 0:kc * tbs].rearrange("p (k t) -> p k t", k=kc)
        if dyn:
            base = self.dyn_base
            S.op("sp", lambda h, xv=xv, nb=nb, g0=g0, base=base: h.dma_start(out=xv[:, :, 0:nb], in_=src[:, 0:kc, base:base + self.cfg.SEQ][:, :, bass.ds(self.sval, self.cfg.TL)][:, :, g0:g0 + nb]), writes=[xb], dma=True, late=True)
        else:
            self.dma("sp", xv[:, :, 0:nb], src[:, 0:kc, g0:g0 + nb], writes=[xb])
        for ci, (W, c0, n) in enumerate(ranges):
            ps = self.psr.next()
            o = offs[ci]
            for k in range(kc):
                S.op("pe", lambda e: e.matmul(ps.ap[0:n, 0:nb], lhsT=wv[:, k, o:o + n], rhs=xv[:, k, 0:nb], start=(k == 0), stop=(k == kc - 1)),
                     reads=[wt, xb], writes=[ps], acc=k > 0)
            sink(ci, g0, nb, ps)


def merge_phase(self, l, W_in_l, wb, wo):
    cfg, S = self.cfg, self.S
    NG = self.TN
    dyn = self.dyn_base is not None
    self.PB = [self.tbuf(f"PB{n}", [D, NG]) for n in range(4)]
    self.mT = self.tbuf("mT", [128, KC, NG], BF16)
    self.yfm = self.tbuf("yfm", [D, NG])
    tb = blocks(0, NG, 512)
    self.phase()
    wring = Ring(self.AB.allocn(2, 4 * 512, "wt")); xring = Ring(self.AB.allocn(2, 4 * 512, "xb")); oring = Ring(self.AF.allocn(4, 512, "ost"))
    for n in range(4):
        src = self.yT[n].rearrange("(k p) t -> p k t", p=128)
        for c0 in range(0, D, 512):
            rngs = [(wb[n], c0 + j * 128, 128) for j in range(4)]

            def sink(ci, g0, nb, ps, n=n, c0=c0):
                ot = oring.next()
                S.op("act" if ci % 2 else "dve", (lambda e: e.copy(out=ot.ap[:, 0:nb], in_=ps.ap[:, 0:nb])) if ci % 2 else (lambda e: e.tensor_copy(out=ot.ap[:, 0:nb], in_=ps.ap[:, 0:nb])), reads=[ps], writes=[ot])
                self.dma("act", self.PB[n][c0 + ci * 128:c0 + (ci + 1) * 128, g0:g0 + nb], ot.ap[:, 0:nb], reads=[ot])
            gemm_fm(self, rngs, src, 4, tb, sink, wring, xring, 512, dyn=dyn)
    self.phase()
    wring = Ring(self.AB.allocn(1, KC * 512, "wt")); xring = Ring(self.AB.allocn(2, KC * 512, "xb"))
    sgr = Ring(self.AF.allocn(2, 512, "sg")); pbr = Ring(self.AF.allocn(2, 512, "pb")); accr = Ring(self.AF.allocn(2, 512, "acc")); mor = Ring(self.AB.allocn(2, 512, "mo"))
    for m in range(KC):
        rngs = [(W_in_l, C_GATE + n * D + m * 128, 128) for n in range(4)]
        st = {}

        def sink(ci, g0, nb, ps, m=m, st=st):
            sg = sgr.next(); pb = pbr.next()
            self.dma("act", pb.ap[:, 0:nb], self.PB[ci][m * 128:(m + 1) * 128, g0:g0 + nb], writes=[pb])
            S.op("act", lambda e: e.activation(out=sg.ap[:, 0:nb], in_=ps.ap[:, 0:nb], func=AF.Sigmoid), reads=[ps], writes=[sg])
            if ci == 0:
                st["acc"] = accr.next()
                acc = st["acc"]
                S.op("dve", lambda e: e.tensor_tensor(out=acc.ap[:, 0:nb], in0=sg.ap[:, 0:nb], in1=pb.ap[:, 0:nb], op=ALU.mult), reads=[sg, pb], writes=[acc])
            else:
                acc = st["acc"]
                S.op("dve", lambda e: e.tensor_tensor(out=sg.ap[:, 0:nb], in0=sg.ap[:, 0:nb], in1=pb.ap[:, 0:nb], op=ALU.mult), reads=[sg, pb], writes=[sg])
                S.op("pool", lambda e: e.tensor_tensor(out=acc.ap[:, 0:nb], in0=acc.ap[:, 0:nb], in1=sg.ap[:, 0:nb], op=ALU.add), reads=[acc, sg], writes=[acc])
            if ci == 3:
                mo = mor.next()
                S.op("act", lambda e: e.copy(out=mo.ap[:, 0:nb], in_=acc.ap[:, 0:nb]), reads=[acc], writes=[mo])
                self.dma("sp", self.mT[:, m, g0:g0 + nb], mo.ap[:, 0:nb], reads=[mo])
        gemm_fm(self, rngs, self.xmT, KC, tb, sink, wring, xring, 512, dyn=dyn)
    self.gemm_to_yfm(wo, self.mT, KC)


def gemm_to_yfm(self, W, src, kc, accumulate=False):
    cfg, S = self.cfg, self.S
    tb = blocks(0, self.TN, 256)
    self.phase()
    wring = Ring(self.AB.allocn(1, kc * 512, "wt")); xring = Ring(self.AB.allocn(2, kc * 256, "xb")); oring = Ring(self.AF.allocn(4, 512, "ost"))
    for c0 in range(0, D, 512):
        rngs = [(W, c0 + j * 128, 128) for j in range(4)]

        def sink(ci, g0, nb, ps, c0=c0):
            ot = oring.next()
            if accumulate:
                self.dma("sp", ot.ap[:, 0:nb], self.yfm[c0 + ci * 128:c0 + (ci + 1) * 128, g0:g0 + nb], writes=[ot])
                S.op("dve", lambda e: e.tensor_tensor(out=ot.ap[:, 0:nb], in0=ot.ap[:, 0:nb], in1=ps.ap[:, 0:nb], op=ALU.add), reads=[ps, ot], writes=[ot])
            else:
                S.op("act" if ci % 2 else "dve", (lambda e: e.copy(out=ot.ap[:, 0:nb], in_=ps.ap[:, 0:nb])) if ci % 2 else (lambda e: e.tensor_copy(out=ot.ap[:, 0:nb], in_=ps.ap[:, 0:nb])), reads=[ps], writes=[ot])
            self.dma("act", self.yfm[c0 + ci * 128:c0 + (ci + 1) * 128, g0:g0 + nb], ot.ap[:, 0:nb], reads=[ot])
        gemm_fm(self, rngs, src, kc, tb, sink, wring, xring, 512, tbs=256)


def epi_phase(self, l, x_src, ctx_src, gate_i, ln_i, dst, lng, lnb, out_ext=None, x_dyn=False):
    cfg, S = self.cfg, self.S
    alpha = float((2.0 * 2) ** 0.25)
    self.phase()
    gm = {}
    for kind in range(2):
        t = self.AF.alloc(D, f"gm{kind}")
        self.dma("sp", t.ap, self.modD[l, kind:kind + 1, gate_i * D:(gate_i + 1) * D].to_broadcast([128, D]), writes=[t])
        gm[kind] = t
    G = self.AF.alloc(D, "lng"); self.dma("sp", G.ap, lng[l, ln_i:ln_i + 1, :].to_broadcast([128, D]), writes=[G])
    Bt = self.AF.alloc(D, "lnb"); self.dma("sp", Bt.ap, lnb[l, ln_i:ln_i + 1, :].to_broadcast([128, D]), writes=[Bt])
    xr = Ring(self.AF.allocn(2, D, "x")); yr = Ring(self.AF.allocn(2, D, "y")); rr = Ring(self.AF.allocn(2, D, "r")); sr = Ring(self.AF.allocn(4, 4, "s"))
    tctx = self.t_ctx
    for ti in range(self.TN // 128):
        g0 = ti * 128
        kind = 1 if g0 < tctx else 0
        X = xr.next(); Y = yr.next(); R = rr.next(); s = sr.next()
        if x_dyn:
            S.op("sp", lambda h, X=X, g0=g0: h.dma_start(out=X.ap, in_=x_src[bass.ds(self.sval, self.cfg.TL), :][g0:g0 + 128, :]), writes=[X], dma=True, late=True)
        else:
            src = ctx_src[g0:g0 + 128, :] if kind == 1 else x_src[g0 - tctx:g0 + 128 - tctx, :]
            self.dma("sp", X.ap, src, writes=[X])
        yv = Y.ap.rearrange("p (c t) -> p c t", c=KC)
        self.dma("act", yv, self.yfm[:, g0:g0 + 128].rearrange("(c p) t -> p c t", p=128), writes=[Y])
        for g in range(4):
            ps = self.psr.next()
            for j in range(4):
                S.op("pe", lambda e: e.transpose(ps.ap[:, j * 128:(j + 1) * 128], yv[:, g * 4 + j, :], self.ident.ap), reads=[Y, self.ident], writes=[ps], acc=j > 0)
            S.op("dve", lambda e: e.tensor_tensor(out=R.ap[:, g * 512:(g + 1) * 512], in0=ps.ap, in1=gm[kind].ap[:, g * 512:(g + 1) * 512], op=ALU.mult), reads=[ps, gm[kind]], writes=[R], acc=True)
        S.op("dve", lambda e: e.scalar_tensor_tensor(out=R.ap, in0=X.ap, scalar=alpha, in1=R.ap, op0=ALU.mult, op1=ALU.add), reads=[X, R], writes=[R])
        S.op("dve", lambda e: e.reduce_sum(out=s.ap[:, 0:1], in_=R.ap, axis=AX.X), reads=[R], writes=[s])
        S.op("dve", lambda e: e.tensor_scalar_mul(out=s.ap[:, 0:1], in0=s.ap[:, 0:1], scalar1=1.0 / D), reads=[s], writes=[s])
        S.op("dve", lambda e: e.tensor_scalar_sub(out=R.ap, in0=R.ap, scalar1=s.ap[:, 0:1]), reads=[R, s], writes=[R])
        S.op("pool", lambda e: e.tensor_tensor(out=X.ap, in0=R.ap, in1=R.ap, op=ALU.mult), reads=[R], writes=[X])
        S.op("dve", lambda e: e.reduce_sum(out=s.ap[:, 1:2], in_=X.ap, axis=AX.X), reads=[X], writes=[s])
        S.op("dve", lambda e: e.tensor_scalar(out=s.ap[:, 1:2], in0=s.ap[:, 1:2], scalar1=1.0 / D, scalar2=1e-5, op0=ALU.mult, op1=ALU.add), reads=[s], writes=[s])
        S.op("act", lambda e: e.activation(out=s.ap[:, 2:3], in_=s.ap[:, 1:2], func=AF.Sqrt), reads=[s], writes=[s])
        S.op("dve", lambda e: e.reciprocal(out=s.ap[:, 3:4], in_=s.ap[:, 2:3]), reads=[s], writes=[s])
        S.op("dve", lambda e: e.scalar_tensor_tensor(out=R.ap, in0=R.ap, scalar=s.ap[:, 3:4], in1=G.ap, op0=ALU.mult, op1=ALU.mult), reads=[R, s, G], writes=[R])
        S.op("pool", lambda e: e.tensor_tensor(out=R.ap, in0=R.ap, in1=Bt.ap, op=ALU.add), reads=[R, Bt], writes=[R])
        self.dma("sp", dst[g0:g0 + 128, :], R.ap, reads=[R])
        if out_ext is not None and kind == 0:
            self.dma("act", out_ext[g0 - tctx:g0 - tctx + 128, :], R.ap, reads=[R])


def ffn_dense_phase(self, l, wg, wu, wd, FD, comb_row=None, accumulate=False):
    cfg, S = self.cfg, self.S
    nf = FD // 128
    self.UT = self.tbuf("UT", [128, max(cfg.FD, cfg.FE) // 128, self.TN], BF16)
    self.phase()
    tb = blocks(0, self.TN, 512)
    wring = Ring(self.AB.allocn(1, KC * 512, "wt")); xring = Ring(self.AB.allocn(2, KC * 512, "xb"))
    sgr = Ring(self.AF.allocn(2, 512, "sg")); uor = Ring(self.AB.allocn(2, 512, "uo"))
    cbr = Ring(self.AF.allocn(2, 512, "cb")) if comb_row is not None else None
    for f0 in range(0, nf, 2):
        fcs = [f0, f0 + 1] if f0 + 1 < nf else [f0]
        rngs = []
        for fc in fcs:
            rngs += [(wg, fc * 128, 128), (wu, fc * 128, 128)]
        st = {}

        def sink(ci, g0, nb, ps, fcs=fcs, st=st):
            if ci == 0 and comb_row is not None:
                st["cb"] = cbr.next()
                self.dma("act", st["cb"].ap[:, 0:nb], comb_row[:, g0:g0 + nb].to_broadcast([128, nb]), writes=[st["cb"]])
            if ci % 2 == 0:
                st["sg"] = sgr.next()
                sg = st["sg"]
                S.op("act", lambda e: e.activation(out=sg.ap[:, 0:nb], in_=ps.ap[:, 0:nb], func=AF.Silu), reads=[ps], writes=[sg])
                if comb_row is not None:
                    cb = st["cb"]
                    S.op("pool", lambda e: e.tensor_tensor(out=sg.ap[:, 0:nb], in0=sg.ap[:, 0:nb], in1=cb.ap[:, 0:nb], op=ALU.mult), reads=[sg, cb], writes=[sg])
            else:
                sg = st["sg"]; uo = uor.next()
                S.op("dve", lambda e: e.tensor_tensor(out=uo.ap[:, 0:nb], in0=sg.ap[:, 0:nb], in1=ps.ap[:, 0:nb], op=ALU.mult), reads=[sg, ps], writes=[uo])
                self.dma("act", self.UT[:, fcs[ci // 2], g0:g0 + nb], uo.ap[:, 0:nb], reads=[uo])
        gemm_fm(self, rngs, self.xmT2, KC, tb, sink, wring, xring, 512)
    self.gemm_to_yfm(wd, self.UT, nf, accumulate=accumulate)


K.merge_phase = merge_phase
K.gemm_to_yfm = gemm_to_yfm
K.epi_phase = epi_phase
K.ffn_dense_phase = ffn_dense_phase


def zero_fill_phase(self, targets):
    S = self.S
    self.phase()
    zf = self.AF.alloc(2048, "zf"); S.op("dve", lambda e: e.memset(zf.ap, 0.0), writes=[zf])
    zb = self.AB.alloc(2048, "zb"); S.op("dve", lambda e: e.memset(zb.ap, 0.0), writes=[zb])
    i = 0
    for t in targets:
        z = zb if t.dtype == BF16 else zf
        R, Cn = t.shape
        for r0 in range(0, R, 128):
            for (c0, n) in blocks(0, Cn, 2048):
                self.dma("sp" if i % 2 == 0 else "act", t[r0:r0 + 128, c0:c0 + n], z.ap[:, 0:n], reads=[z])
                i += 1


def moe_phase(self, l, V):
    j = l // 2
    for e in range(8):
        ffn_dense_phase(self, l, V["moe_w_gate"][j, e], V["moe_w_up"][j, e], V["moe_w_down"][j, e], self.cfg.FE, comb_row=self.combT[e:e + 1, :], accumulate=(e > 0))


K.zero_fill_phase = zero_fill_phase
K.moe_phase = moe_phase


def mlstm_tab_phase(self, l, V):
    cfg, S = self.cfg, self.S
    NG = cfg.NG
    NB = 1024
    if not hasattr(self, "tabM"):
        self.tabM = self.dram("tabM", [2, 6, 4, NG])
    self.phase()
    pr = self.AF.alloc(4, "mlp", parts=4)
    self.dma("sp", pr.ap, V["ml_hp"][l], writes=[pr])
    ones = self.AF.alloc(NB, "ones", parts=4)
    S.op("dve", lambda e: e.memset(ones.ap, 1.0), writes=[ones])
    zero1 = self.AF.alloc(1, "z1", parts=4)
    S.op("dve", lambda e: e.memset(zero1.ap, 0.0), writes=[zero1])
    names = ("I", "F", "T", "BC", "G", "GE", "GP", "KS", "WIN", "E3", "CAR")
    rr = {n: Ring(self.AF.allocn(2, NB, n, parts=4)) for n in names}
    cring = Ring(self.AF.allocn(6, 2, "car", parts=4))
    for d in range(2):
        carry = None
        segs = _segments(cfg)
        blks = []
        for (s0, s1) in segs:
            b = blocks(s0, s1, NB)
            blks += b if d == 0 else b[::-1]
        for (g0, nb) in blks:
            I, F, T, BC, G, GE, GP, KS, WIN, E3, CAR = [rr[n].next() for n in names]
            v = lambda X: X.ap[:, 0:nb]
            f = (lambda a: a) if d == 0 else (lambda a: rev_ap(a, nb))
            self.dma("sp", v(I), self.pfm[C_ML_G + d * 8:C_ML_G + d * 8 + 4, g0:g0 + nb], writes=[I])
            self.dma("act", v(F), self.pfm[C_ML_G + d * 8 + 4:C_ML_G + d * 8 + 8, g0:g0 + nb], writes=[F])
            S.op("dve", lambda e: e.tensor_scalar_add(out=v(I), in0=v(I), scalar1=pr.ap[:, d:d + 1]), reads=[I, pr], writes=[I])
            S.op("dve", lambda e: e.tensor_scalar(out=v(F), in0=v(F), scalar1=pr.ap[:, 2 + d:3 + d], scalar2=-1.0, op0=ALU.add, op1=ALU.mult), reads=[F, pr], writes=[F])
            softplus_ops(self, F, T, nb, 4)
            cb_ap = zero1.ap[:, 0:1] if carry is None else carry.ap[:, 0:1]
            cg_ap = zero1.ap[:, 0:1] if carry is None else carry.ap[:, 1:2]
            cdeps = [zero1] if carry is None else [carry]
            S.op("dve", lambda e: e.tensor_tensor_scan(out=f(v(BC)), data0=f(v(ones)), data1=f(v(F)), initial=cb_ap, op0=ALU.mult, op1=ALU.subtract), reads=[F, ones] + cdeps, writes=[BC])
            S.op("dve", lambda e: e.tensor_tensor(out=v(I), in0=v(I), in1=v(BC), op=ALU.subtract), reads=[I, BC], writes=[I])
            S.op("dve", lambda e: e.tensor_tensor_scan(out=f(v(G)), data0=f(v(ones)), data1=f(v(I)), initial=cg_ap, op0=ALU.mult, op1=ALU.max), reads=[I, ones] + cdeps, writes=[G])
            endpos = 127 if d == 0 else 0
            nch = nb // 128
            gv = v(G).rearrange("p (c t) -> p c t", t=128)
            S.op("dve", lambda e: e.tensor_copy(out=v(GE).rearrange("p (c t) -> p c t", t=128), in_=gv[:, :, endpos:endpos + 1].to_broadcast([4, nch, 128])), reads=[G], writes=[GE])
            if d == 0:
                if nch > 1:
                    S.op("dve", lambda e: e.tensor_copy(out=GP.ap[:, 128:nb], in_=GE.ap[:, 0:nb - 128]), reads=[GE], writes=[GP])
                S.op("dve", lambda e: e.tensor_copy(out=GP.ap[:, 0:128], in_=cg_ap.to_broadcast([4, 128])), reads=cdeps, writes=[GP], acc=True)
            else:
                if nch > 1:
                    S.op("dve", lambda e: e.tensor_copy(out=GP.ap[:, 0:nb - 128], in_=GE.ap[:, 128:nb]), reads=[GE], writes=[GP])
                S.op("dve", lambda e: e.tensor_copy(out=GP.ap[:, nb - 128:nb], in_=cg_ap.to_broadcast([4, 128])), reads=cdeps, writes=[GP], acc=True)
            S.op("dve", lambda e: e.tensor_tensor(out=v(KS), in0=v(I), in1=v(GE), op=ALU.subtract), reads=[I, GE], writes=[KS])
            S.op("act", lambda e: e.activation(out=v(KS), in_=v(KS), func=AF.Exp), reads=[KS], writes=[KS])
            S.op("dve", lambda e: e.tensor_tensor(out=v(WIN), in0=v(GP), in1=v(G), op=ALU.subtract), reads=[GP, G], writes=[WIN])
            S.op("act", lambda e: e.activation(out=v(WIN), in_=v(WIN), func=AF.Exp), reads=[WIN], writes=[WIN])
            S.op("dve", lambda e: e.tensor_tensor(out=v(E3), in0=v(BC), in1=v(G), op=ALU.add), reads=[BC, G], writes=[E3])
            S.op("act", lambda e: e.activation(out=v(E3), in_=v(E3), func=AF.Exp, scale=-1.0), reads=[E3], writes=[E3])
            S.op("dve", lambda e: e.tensor_tensor(out=v(CAR), in0=v(GP), in1=v(GE), op=ALU.subtract), reads=[GP, GE], writes=[CAR])
            S.op("act", lambda e: e.activation(out=v(CAR), in_=v(CAR), func=AF.Exp), reads=[CAR], writes=[CAR])
            ncar = cring.next()
            lastpos = nb - 1 if d == 0 else 0
            S.op("dve", lambda e: e.tensor_copy(out=ncar.ap[:, 0:1], in_=BC.ap[:, lastpos:lastpos + 1]), reads=[BC], writes=[ncar])
            S.op("dve", lambda e: e.tensor_copy(out=ncar.ap[:, 1:2], in_=G.ap[:, lastpos:lastpos + 1]), reads=[G], writes=[ncar], acc=True)
            carry = ncar
            for qi, X in enumerate((I, KS, WIN, E3, CAR, G)):
                self.dma("sp" if qi % 2 else "act", self.tabM[d, qi, :, g0:g0 + nb], v(X), reads=[X])


def mlstm_main_phase(self, l, V):
    cfg, S = self.cfg, self.S
    NG = cfg.NG
    if not hasattr(self, "htm"):
        self.htm = self.dram("htm", [NG, 512])
    for d in range(2):
        self.phase()
        AFa, ABa = self.AF, self.AB
        tri = AFa.alloc(128, "tri"); self.dma("sp", tri.ap, V["tri"][d], writes=[tri])
        sel = AFa.alloc(4 * 128, "sel", parts=24); self.dma("sp", sel.ap, V["sel24"].rearrange("r h p -> r (h p)"), writes=[sel])
        Cst = AFa.alloc(4 * 129, "C"); S.op("dve", lambda e: e.memset(Cst.ap, 0.0), writes=[Cst])
        Cb = Cst
        rQK = Ring(AFa.allocn(2, 8 * 128, "qk")); rVF = Ring(AFa.allocn(2, 512, "vf")); rTAB = Ring(AFa.allocn(2, 128, "tabm", parts=24)); rTT = Ring(AFa.allocn(2, 24, "TTm"))
        vas = AFa.allocn(2, 4 * 129, "VA")
        for va in vas:
            S.op("dve", lambda e, va=va: e.memset(va.ap, 1.0), writes=[va])
        rVA = Ring(vas); rKS = Ring(AFa.allocn(2, 512, "KSt"))
        rDm = Ring(AFa.allocn(3, 128, "Dm")); rST = Ring(AFa.allocn(3, 128, "ST")); rN = Ring(AFa.allocn(3, 132, "N")); rs = Ring(AFa.allocn(4, 2, "s"))
        rHS = Ring(AFa.allocn(2, 512, "HS")); rH2 = Ring(AFa.allocn(2, 512, "H2")); rO = Ring(AFa.allocn(2, 512, "Of")); rOs = Ring(AFa.allocn(2, 512, "Os")); rYo = Ring(ABa.allocn(2, 512, "Yo"))
        psq = self.psr
        col = lambda q, h: q * 4 + h
        for c in chunk_order(cfg, d):
            g0 = c * 128
            QK = rQK.next(); VF = rVF.next(); TAB = rTAB.next(); TT = rTT.next(); VA = rVA.next(); KS = rKS.next(); HS = rHS.next()
            qv = QK.ap.rearrange("p (c t) -> p c t", c=8)
            self.dma("sp", qv, self.qkT[:, g0:g0 + 128].rearrange("(c p) t -> p c t", p=128), writes=[QK])
            vv = VF.ap.rearrange("p (c t) -> p c t", c=4)
            self.dma("act", vv, self.pfm[C_ML_V:C_ML_V + 512, g0:g0 + 128].rearrange("(c p) t -> p c t", p=128), writes=[VF])
            self.dma("act", TAB.ap, self.tabM[d, :, :, g0:g0 + 128].rearrange("q h t -> (q h) t"), writes=[TAB])
            ps = psq.next()
            S.op("pe", lambda e: e.transpose(ps.ap[:, 0:24], TAB.ap, self.ident.ap[0:24, 0:24]), reads=[TAB, self.ident], writes=[ps])
            S.op("act", lambda e: e.copy(out=TT.ap, in_=ps.ap[:, 0:24]), reads=[ps], writes=[TT])
            ps = psq.next()
            for j in range(4):
                S.op("pe", lambda e: e.transpose(ps.ap[:, j * 128:(j + 1) * 128], vv[:, j, :], self.ident.ap), reads=[VF, self.ident], writes=[ps], acc=j > 0)
            va3 = VA.ap.rearrange("p (h q) -> p h q", h=4)
            S.op("act", lambda e: e.copy(out=va3[:, :, 0:128], in_=ps.ap.rearrange("p (h q) -> p h q", h=4)), reads=[ps], writes=[VA], acc=True)
            ps = psq.next()
            pb = ps.ap
            for j in range(4):
                S.op("pe", lambda e: e.transpose(pb[:, j * 128:(j + 1) * 128], qv[:, 4 + j, :], self.ident.ap), reads=[QK, self.ident], writes=[ps], acc=j > 0)
            for h in range(4):
                S.op("act", lambda e: e.activation(out=KS.ap[:, h * 128:(h + 1) * 128], in_=pb[:, h * 128:(h + 1) * 128], func=AF.Copy, scale=TT.ap[:, col(1, h):col(1, h) + 1]), reads=[ps, TT], writes=[KS], acc=h > 0)
            for h in range(4):
                psS = psq.next()
                S.op("pe", lambda e: e.matmul(psS.ap[:, 0:128], lhsT=qv[:, 4 + h, :], rhs=qv[:, h, :], start=True, stop=True), reads=[QK], writes=[psS])
                psR = psq.next()
                S.op("pe", lambda e: e.matmul(psR.ap[:, 0:128], lhsT=sel.ap[:, h * 128:(h + 1) * 128], rhs=TAB.ap, start=True, stop=True), reads=[sel, TAB], writes=[psR])
                Dm = rDm.next(); ST = rST.next(); N = rN.next(); s = rs.next()
                S.op("dve", lambda e: e.tensor_scalar(out=Dm.ap, in0=psR.ap[:, 0:128], scalar1=TT.ap[:, col(0, h):col(0, h) + 1], scalar2=0.0, op0=ALU.subtract, op1=ALU.max), reads=[psR, TT], writes=[Dm])
                S.op("act", lambda e: e.activation(out=Dm.ap, in_=Dm.ap, func=AF.Exp, scale=-1.0), reads=[Dm], writes=[Dm])
                S.op("pool", lambda e: e.tensor_tensor(out=Dm.ap, in0=Dm.ap, in1=tri.ap, op=ALU.mult), reads=[Dm, tri], writes=[Dm])
                S.op("dve", lambda e: e.tensor_tensor(out=ST.ap, in0=Dm.ap, in1=psS.ap[:, 0:128], op=ALU.mult), reads=[Dm, psS], writes=[ST])
                psA = psq.next()
                S.op("pe", lambda e: e.matmul(psA.ap[:, 0:129], lhsT=ST.ap, rhs=VA.ap[:, h * 129:(h + 1) * 129], start=True, stop=True), reads=[ST, VA], writes=[psA])
                psB = psq.next()
                S.op("pe", lambda e: e.matmul(psB.ap[:, 0:129], lhsT=qv[:, h, :], rhs=Cb.ap[:, h * 129:(h + 1) * 129], start=True, stop=True), reads=[QK, Cb], writes=[psB])
                S.op("dve", lambda e: e.tensor_scalar_mul(out=N.ap[:, 0:129], in0=psB.ap[:, 0:129], scalar1=TT.ap[:, col(2, h):col(2, h) + 1]), reads=[psB, TT], writes=[N])
                S.op("dve", lambda e: e.tensor_tensor(out=N.ap[:, 0:129], in0=N.ap[:, 0:129], in1=psA.ap[:, 0:129], op=ALU.add), reads=[N, psA], writes=[N])
                S.op("act", lambda e: e.activation(out=s.ap[:, 0:1], in_=N.ap[:, 128:129], func=AF.Abs), reads=[N], writes=[s])
                S.op("dve", lambda e: e.tensor_tensor(out=s.ap[:, 0:1], in0=s.ap[:, 0:1], in1=TT.ap[:, col(3, h):col(3, h) + 1], op=ALU.max), reads=[s, TT], writes=[s])
                S.op("dve", lambda e: e.reciprocal(out=s.ap[:, 1:2], in_=s.ap[:, 0:1]), reads=[s], writes=[s])
                S.op("dve", lambda e: e.tensor_scalar_mul(out=HS.ap[:, h * 128:(h + 1) * 128], in0=N.ap[:, 0:128], scalar1=s.ap[:, 1:2]), reads=[N, s], writes=[HS], acc=h > 0)
                psU = psq.next()
                S.op("pe", lambda e: e.matmul(psU.ap[:, 0:129], lhsT=KS.ap[:, h * 128:(h + 1) * 128], rhs=VA.ap[:, h * 129:(h + 1) * 129], start=True, stop=True), reads=[KS, VA], writes=[psU])
                cs = Cst.ap[:, h * 129:(h + 1) * 129]
                S.op("dve", lambda e: e.scalar_tensor_tensor(out=cs, in0=cs, scalar=TT.ap[:, col(4, h):col(4, h) + 1], in1=psU.ap[:, 0:129], op0=ALU.mult, op1=ALU.add), reads=[Cst, TT, psU, psB], writes=[Cst])
            if d == 0:
                self.dma("sp", self.htm[g0:g0 + 128, :], HS.ap, reads=[HS])
            else:
                H2 = rH2.next(); Of = rO.next(); Os = rOs.next()
                self.dma("sp", H2.ap, self.htm[g0:g0 + 128, :], writes=[H2])
                S.op("pool", lambda e: e.tensor_tensor(out=HS.ap, in0=HS.ap, in1=H2.ap, op=ALU.add), reads=[HS, H2], writes=[HS])
                ov = Of.ap.rearrange("p (c t) -> p c t", c=4)
                self.dma("act", ov, self.pfm[C_ML_O:C_ML_O + 512, g0:g0 + 128].rearrange("(c p) t -> p c t", p=128), writes=[Of])
                psZ = psq.next()
                for j in range(4):
                    S.op("pe", lambda e: e.transpose(psZ.ap[:, j * 128:(j + 1) * 128], ov[:, j, :], self.ident.ap), reads=[Of, self.ident], writes=[psZ], acc=j > 0)
                S.op("act", lambda e: e.activation(out=Os.ap, in_=psZ.ap, func=AF.Sigmoid), reads=[psZ], writes=[Os])
                S.op("dve", lambda e: e.tensor_tensor(out=HS.ap, in0=HS.ap, in1=Os.ap, op=ALU.mult), reads=[HS, Os], writes=[HS])
                self.store_tm_as_fm(HS, 3, g0, rYo)


def mlstm_phase(self, l, V):
    cfg, S = self.cfg, self.S
    if not hasattr(self, "qkT"):
        self.qkT = self.dram("qkT", [1024, cfg.NG])
    st = {}

    def post(c, s0, g0, nb, SG, O, ctxp):
        scale = (128.0 ** -0.5) if c < 4 else 1.0
        if "init" not in ctxp:
            ctxp["init"] = True
            ctxp["P"] = self.AB.alloc(128, "ropeP")
            pf = self.AF.alloc(128, "ropePf")
            self.dma("sp", pf.ap, V["ropeP"], writes=[pf])
            S.op("dve", lambda e: e.tensor_copy(out=ctxp["P"].ap, in_=pf.ap), reads=[pf], writes=[ctxp["P"]])
            ctxp["sgb"] = Ring(self.AB.allocn(2, 1024, "sgb"))
            ctxp["cos"] = Ring(self.AF.allocn(2, 1024, "cos")); ctxp["sin"] = Ring(self.AF.allocn(2, 1024, "sin")); ctxp["t1"] = Ring(self.AF.allocn(2, 1024, "t1"))
        if s0 == 0:
            S.op("act", lambda e: e.activation(out=O.ap[:, 0:nb], in_=SG.ap[:, 0:nb], func=AF.Copy, scale=scale), reads=[SG], writes=[O])
            return
        P = ctxp["P"]; sgb = ctxp["sgb"].next(); cs = ctxp["cos"].next(); sn = ctxp["sin"].next(); t1 = ctxp["t1"].next()
        t0 = g0 - CTX
        self.dma("sp", cs.ap[:, 0:nb], V["rope_cos"][:, t0:t0 + nb], writes=[cs])
        self.dma("sp", sn.ap[:, 0:nb], V["rope_sin"][:, t0:t0 + nb], writes=[sn])
        S.op("act", lambda e: e.copy(out=sgb.ap[:, 0:nb], in_=SG.ap[:, 0:nb]), reads=[SG], writes=[sgb])
        S.op("pool", lambda e: e.tensor_tensor(out=t1.ap[:, 0:nb], in0=SG.ap[:, 0:nb], in1=cs.ap[:, 0:nb], op=ALU.mult), reads=[SG, cs], writes=[t1])
        for (o, n) in blocks(0, nb, 512):
            ps = self.psr.next()
            S.op("pe", lambda e: e.matmul(ps.ap[:, 0:n], lhsT=P.ap, rhs=sgb.ap[:, o:o + n], start=True, stop=True), reads=[P, sgb], writes=[ps])
            S.op("dve", lambda e: e.tensor_tensor(out=sn.ap[:, o:o + n], in0=ps.ap[:, 0:n], in1=sn.ap[:, o:o + n], op=ALU.mult), reads=[ps, sn], writes=[sn], acc=True)
        S.op("dve", lambda e: e.tensor_tensor(out=t1.ap[:, 0:nb], in0=t1.ap[:, 0:nb], in1=sn.ap[:, 0:nb], op=ALU.add), reads=[t1, sn], writes=[t1])
        S.op("act", lambda e: e.activation(out=O.ap[:, 0:nb], in_=t1.ap[:, 0:nb], func=AF.Copy, scale=scale), reads=[t1], writes=[O])
    conv_silu_phase(self, l, C_ML_Q, 8, V["ml_cw"][l], V["ml_cb"][l], self.qkT, post=post, out_f32=True)
    mlstm_tab_phase(self, l, V)
    mlstm_main_phase(self, l, V)


K.mlstm_phase = mlstm_phase


def na_rowinfo(ROWS):
    info = []
    classes = {}
    for r in range(ROWS):
        ws = min(max(r - 4, 0), ROWS - 8)
        as_ = min(2 * (ws // 2), ROWS - 10)
        key = (ws - as_, r - ws)
        if key not in classes:
            classes[key] = len(classes)
        info.append((ws, as_, classes[key]))
    return info, classes


def na_phase(self, l, V):
    cfg, S = self.cfg, self.S
    NG, NT, ROWS = cfg.NG, cfg.NT, cfg.ROWS
    info, classes = na_rowinfo(ROWS)
    ncls = len(classes)
    if not hasattr(self, "naq"):
        self.naq = self.dram("naq", [512, NG], BF16)
        self.nak = self.dram("nak", [512, NG], BF16)
        self.nav = self.dram("nav", [NG, 8 * 65], BF16)
        self.natm = self.dram("natm", [NG, 512])
    self.phase()
    NB = 2048
    rin = Ring(self.AF.allocn(2, NB, "nin")); rout = Ring(self.AB.allocn(2, NB, "nout"))
    for (row0, dst, sc) in ((C_NA_Q, self.naq, 0.125), (C_NA_K, self.nak, 1.0)):
        for c in range(4):
            for (g0, nb) in blocks(0, NG, NB):
                ti = rin.next(); to = rout.next()
                self.dma("sp", ti.ap[:, 0:nb], self.pfm[row0 + c * 128:row0 + (c + 1) * 128, g0:g0 + nb], writes=[ti])
                S.op("act", lambda e: e.activation(out=to.ap[:, 0:nb], in_=ti.ap[:, 0:nb], func=AF.Copy, scale=sc), reads=[ti], writes=[to])
                self.dma("act", dst[c * 128:(c + 1) * 128, g0:g0 + nb], to.ap[:, 0:nb], reads=[to])
    rvf = Ring(self.AF.allocn(2, 512, "vf"))
    vas = self.AB.allocn(2, 8 * 65, "va")
    for va in vas:
        S.op("dve", lambda e, va=va: e.memset(va.ap, 1.0), writes=[va])
    rva = Ring(vas)
    for ti_ in range(NT):
        g0 = ti_ * 128
        VF = rvf.next(); VA = rva.next()
        vv = VF.ap.rearrange("p (c t) -> p c t", c=4)
        self.dma("sp", vv, self.pfm[C_NA_V:C_NA_V + 512, g0:g0 + 128].rearrange("(c p) t -> p c t", p=128), writes=[VF])
        ps = self.psr.next()
        for j in range(4):
            S.op("pe", lambda e: e.transpose(ps.ap[:, j * 128:(j + 1) * 128], vv[:, j, :], self.ident.ap), reads=[VF, self.ident], writes=[ps], acc=j > 0)
        S.op("act", lambda e: e.copy(out=VA.ap.rearrange("p (h q) -> p h q", h=8)[:, :, 0:64], in_=ps.ap.rearrange("p (h q) -> p h q", h=8)), reads=[ps], writes=[VA], acc=True)
        self.dma("act", self.nav[g0:g0 + 128, :], VA.ap, reads=[VA])
    for hc in range(4):
        self.phase()
        QT = self.AB.alloc(NG, "QT"); KT = self.AB.alloc(NG, "KT"); VT = self.AB.alloc(NT * 130, "VT"); BM = self.AB.alloc(ncls * 640, "BM")
        self.dma("sp", QT.ap, self.naq[hc * 128:(hc + 1) * 128, :], writes=[QT])
        self.dma("act", KT.ap, self.nak[hc * 128:(hc + 1) * 128, :], writes=[KT])
        self.dma("sp", VT.ap.rearrange("p (t q) -> p t q", t=NT), self.nav[:, hc * 130:(hc + 1) * 130].rearrange("(t p) q -> p t q", p=128), writes=[VT])
        for hh in range(2):
            self.dma("pool", BM.ap[hh * 64:(hh + 1) * 64, :].rearrange("p (c k) -> p c k", c=ncls), V["na_bm"][l, :, 2 * hc + hh].rearrange("c p k -> p c k"), writes=[BM], reads=[BM] if hh else [])
        vt3 = VT.ap.rearrange("p (t q) -> p t q", t=NT)
        bm3 = BM.ap.rearrange("p (c k) -> p c k", c=ncls)
        rPT = Ring(self.AB.allocn(3, 448, "PT")); rO = Ring(self.AF.allocn(3, 128, "NO", parts=64)); rs = Ring(self.AF.allocn(4, 2, "ns", parts=64))
        qrows = [("c", i) for i in range(CTX // 64)] + [("l", r) for r in range(ROWS)]
        for (kind, r) in qrows:
            OUT = rO.next()
            for hh in range(2):
                pb_ = hh * 64
                if kind == "c":
                    q0 = r * 64
                    tiles = [(None, t * 128) for t in range(CTX // 128)]
                else:
                    q0 = CTX + r * 64
                    ws, as_, cls = info[r]
                    tiles = [(j, CTX + (as_ + 2 * j) * 64) for j in range(5)] + [(None, t * 128) for t in range(CTX // 128)]
                nt_ = len(tiles)
                psS = self.psr.next()
                for ti_, (j, k0) in enumerate(tiles):
                    S.op("pe", lambda e: e.matmul(psS.ap[:, ti_ * 64:(ti_ + 1) * 64], lhsT=KT.ap[pb_:pb_ + 64, k0:k0 + 128], rhs=QT.ap[pb_:pb_ + 64, q0:q0 + 64], start=True, stop=(j is None)), reads=[KT, QT], writes=[psS], acc=ti_ > 0)
                    if j is not None:
                        S.op("pe", lambda e: e.matmul(psS.ap[:, ti_ * 64:(ti_ + 1) * 64], lhsT=bm3[pb_:pb_ + 64, cls, j * 128:(j + 1) * 128], rhs=self.identb.ap[pb_:pb_ + 64, pb_:pb_ + 64], start=False, stop=True), reads=[BM, self.identb], writes=[psS], acc=True)
                PT = rPT.next()
                S.op("act", lambda e: e.activation(out=PT.ap[:, 0:nt_ * 64], in_=psS.ap[:, 0:nt_ * 64], func=AF.Exp), reads=[psS], writes=[PT])
                psO = self.psr.next()
                for ti_, (j, k0) in enumerate(tiles):
                    S.op("pe", lambda e: e.matmul(psO.ap[0:64, 0:65], lhsT=PT.ap[:, ti_ * 64:(ti_ + 1) * 64], rhs=vt3[:, k0 // 128, hh * 65:(hh + 1) * 65], start=(ti_ == 0), stop=(ti_ == nt_ - 1)), reads=[PT, VT], writes=[psO], acc=ti_ > 0)
                s = rs.next()
                S.op("dve", lambda e: e.reciprocal(out=s.ap[:, 0:1], in_=psO.ap[0:64, 64:65]), reads=[psO], writes=[s])
                S.op("dve", lambda e: e.tensor_scalar_mul(out=OUT.ap[:, hh * 64:(hh + 1) * 64], in0=psO.ap[0:64, 0:64], scalar1=s.ap[:, 0:1]), reads=[psO, s], writes=[OUT], acc=hh > 0)
            self.dma("sp", self.natm[q0:q0 + 64, hc * 128:(hc + 1) * 128], OUT.ap, reads=[OUT])
    self.phase()
    rY = Ring(self.AF.allocn(2, 512, "naY")); rYo = Ring(self.AB.allocn(2, 512, "Yo"))
    for ti_ in range(NT):
        g0 = ti_ * 128
        Y = rY.next()
        self.dma("sp", Y.ap, self.natm[g0:g0 + 128, :], writes=[Y])
        self.store_tm_as_fm(Y, 1, g0, rYo)


K.na_phase = na_phase


import numpy as np
def pmaj(v, p=128):
    v = np.asarray(v)
    return np.ascontiguousarray(v.reshape(-1, p).T)
def base_inputs(inp, b):
    m = {}
    m["x"] = np.ascontiguousarray(inp["x"][b])
    m["ctx"] = np.ascontiguousarray(inp["ctx"][b])
    cT = np.stack([pmaj(inp["c"][b]), pmaj(inp["c_ctx"])], axis=-1)
    m["cT"] = np.ascontiguousarray(cT.astype(np.float32))
    m["ident"] = np.eye(128, dtype=np.float32)
    for k in ("w_ada", "b_ada", "w_in"):
        m[k] = np.ascontiguousarray(inp[k])
    return m

def lru_inputs(inp):
    m = {}
    L = inp["lru_conv_w"].shape[0]
    m["lru_cw"] = np.ascontiguousarray(np.stack([np.stack([pmaj(inp["lru_conv_w"][l, k]) for k in range(4)], -1) for l in range(L)]))
    m["lru_cb"] = np.ascontiguousarray(np.stack([pmaj(inp["lru_conv_b"][l]) for l in range(L)]))
    vec = []
    for l in range(L):
        vec.append(np.stack([pmaj(inp[nm][l, d]) for nm in ("lru_ba", "lru_bx", "lru_lambda") for d in range(2)], 1))
    m["lru_vec"] = np.ascontiguousarray(np.stack(vec))
    m["lru_wa"] = np.ascontiguousarray(inp["lru_wa"]); m["lru_wx"] = np.ascontiguousarray(inp["lru_wx"])
    return m

def ssd_inputs(inp):
    m = {}
    L = inp["ssd_conv_w"].shape[0]
    m["ssd_cw"] = np.ascontiguousarray(np.stack([np.stack([pmaj(inp["ssd_conv_w"][l, k]) for k in range(4)], -1) for l in range(L)]))
    m["ssd_cb"] = np.ascontiguousarray(np.stack([pmaj(inp["ssd_conv_b"][l]) for l in range(L)]))
    m["ssd_hp"] = np.ascontiguousarray(np.stack([np.stack([inp["ssd_dt_bias"][l,0], inp["ssd_dt_bias"][l,1], inp["ssd_a_log"][l,0], inp["ssd_a_log"][l,1]], -1) for l in range(L)]))
    m["ssd_dskip"] = np.ascontiguousarray(np.repeat(inp["ssd_d"], 64, axis=-1))
    m["ssd_gn"] = np.ascontiguousarray(inp["ssd_norm_g"])
    j = np.arange(128)[:, None]; i = np.arange(128)[None, :]
    m["tri"] = np.stack([(j <= i), (j >= i)]).astype(np.float32)
    sel = np.zeros((40, 8, 128), np.float32)
    for h in range(8): sel[h, h, :] = 1.0
    m["sel40"] = sel
    return m

def mlstm_inputs(inp, SEQ):
    m = {}
    L = inp["ml_conv_w"].shape[0]
    m["ml_cw"] = np.ascontiguousarray(np.stack([np.stack([pmaj(inp["ml_conv_w"][l, k]) for k in range(4)], -1) for l in range(L)]))
    m["ml_cb"] = np.ascontiguousarray(np.stack([pmaj(inp["ml_conv_b"][l]) for l in range(L)]))
    m["ml_hp"] = np.ascontiguousarray(np.stack([np.stack([inp["ml_i_bias"][l,0], inp["ml_i_bias"][l,1], inp["ml_f_bias"][l,0], inp["ml_f_bias"][l,1]], -1) for l in range(L)]))
    sel = np.zeros((24, 4, 128), np.float32)
    for h in range(4): sel[5 * 4 + h, h, :] = 1.0
    m["sel24"] = sel
    t = np.arange(SEQ, dtype=np.int32)
    pos = np.stack([t // 64, t % 64], axis=-1).astype(np.float32)
    nf = 32
    inv_freq = (np.float32(10000.0) ** (-np.arange(nf, dtype=np.float32) / nf)).astype(np.float32)
    ang = np.broadcast_to(pos[:, :, None, None] * inv_freq, (SEQ, 2, 2, nf)).reshape(SEQ, 128).astype(np.float32)
    m["rope_cos"] = np.ascontiguousarray(np.cos(ang).T.astype(np.float32))
    m["rope_sin"] = np.ascontiguousarray(np.sin(ang).T.astype(np.float32))
    P = np.zeros((128, 128), np.float32)
    for dp in range(128):
        pair = (dp % 64) // 32
        if pair == 0: P[dp + 32, dp] = -1.0
        else: P[dp - 32, dp] = 1.0
    m["ropeP"] = P
    return m

def na_inputs(inp, ROWS):
    info = []; classes = {}
    for r in range(ROWS):
        ws = min(max(r - 4, 0), ROWS - 8); as_ = min(2 * (ws // 2), ROWS - 10)
        key = (ws - as_, r - ws)
        if key not in classes: classes[key] = (len(classes), r, ws, as_)
    ncls = len(classes)
    rpb = inp["na_rpb"]; L = rpb.shape[0]
    wq = np.arange(64)[:, None]; wk = np.arange(64)[None, :]
    cs = np.clip(wq - 8, 0, 48)
    col_ok = (wk >= cs) & (wk < cs + 16)
    dc = np.clip(wk - wq + 15, 0, 30)
    bm = np.full((L, ncls, 8, 64, 640), -30000.0, np.float32)
    for key, (ci, r, ws, as_) in classes.items():
        for j in range(5):
            for par in range(2):
                kr = as_ + 2 * j + par
                if ws <= kr < ws + 8:
                    dr = kr - r + 7
                    vals = rpb[:, :, dr][:, :, dc]
                    blk = np.where(col_ok[None, None], vals, np.float32(-30000.0))
                    bm[:, ci, :, :, j * 128 + par * 64: j * 128 + par * 64 + 64] = blk
    return {"na_bm": np.ascontiguousarray(bm)}

def moe_inputs(inp):
    wr = inp["moe_w_router"]
    m = {}
    m["moe_wr_p"] = np.ascontiguousarray(wr.reshape(wr.shape[0], 16, 128, 8).transpose(0, 2, 1, 3).reshape(wr.shape[0], 128, 128))
    m["moe_br_p"] = np.ascontiguousarray(np.broadcast_to(inp["moe_b_router"][:, None, :], (wr.shape[0], 128, 8)))
    for k in ("moe_w_gate", "moe_w_up", "moe_w_down"):
        m[k] = np.ascontiguousarray(inp[k])
    return m


def kernel(**inputs):
    inp = {k: np.asarray(v) for k, v in inputs.items()}
    B, SEQ, _ = inp["x"].shape
    NS = 4
    cfg = Cfg(SEQ, inp["ffn_w_gate"].shape[-1], inp["moe_w_gate"].shape[-1], layers=(0, 1), dbg=(), NS=NS)
    k = K(cfg)
    nc = k.build()
    maps = []
    shared = {}
    shared.update(lru_inputs(inp)); shared.update(ssd_inputs(inp)); shared.update(mlstm_inputs(inp, SEQ))
    shared.update(na_inputs(inp, SEQ // 64)); shared.update(moe_inputs(inp))
    for kk in ("w_branch", "w_out", "ln_g", "ln_b", "ffn_w_gate", "ffn_w_up", "ffn_w_down"):
        shared[kk] = np.ascontiguousarray(inp[kk])
    for b in range(B):
        m = base_inputs(inp, b)
        m.update(shared)
        mm = {kk: m[kk] for kk in k.ins}
        for s_ in range(NS):
            maps.append(mm)
    res = run_bass_kernel_spmd(nc, maps, core_ids=list(range(B * NS)))
    return np.stack([np.concatenate([res.results[b * NS + s_]["y_out"] for s_ in range(NS)], axis=0) for b in range(B)], axis=0).astype(np.float32)
```

```python
import numpy as np
from contextlib import ExitStack
import concourse.bass as bass
import concourse.mybir as mybir
from concourse.ap import AP

F32 = mybir.dt.float32
BF16 = mybir.dt.bfloat16
AF = mybir.ActivationFunctionType
ALU = mybir.AluOpType
AX = mybir.AxisListType

ENGS = ("pe", "act", "dve", "pool", "sp")
DMA_Q = ("sp", "act", "pool")
NSLOT = 8


class Tile:
    __slots__ = ("ap", "w", "r", "name")

    def __init__(self, ap, name=""):
        self.ap = ap
        self.w = {}
        self.r = {}
        self.name = name

    def __getitem__(self, k):
        return self.ap[k]


class _Rec:
    def __init__(self):
        self.call = None

    def __getattr__(self, name):
        def f(*a, **kw):
            self.call = (name, a, kw)
            return None
        return f


class Sched:
    def __init__(self, nc, es):
        self.nc = nc
        self.es = es
        self.ops = {e: [] for e in ENGS}
        self.sem = {e: es.enter_context(nc.semaphore("s_" + e)) for e in ENGS}
        self.cnt = {e: 0 for e in ENGS}
        self.slots = {q: [es.enter_context(nc.semaphore(f"d_{q}{i}")) for i in range(NSLOT)] for q in DMA_Q}
        self.slot_cnt = {q: [0] * NSLOT for q in DMA_Q}
        self.slot_i = {q: 0 for q in DMA_Q}
        self.bar = es.enter_context(nc.semaphore("bar"))
        self.bar_cnt = 0
        self.seen = {e: {} for e in ENGS}
        self.touched = set()
        self.n_ops = 0
        self.pre = {}

    def _need(self, eng, deps):
        out = []
        seen = self.seen[eng]
        for s, v in deps.items():
            if seen.get(s, 0) < v:
                seen[s] = v
                out.append((s, v))
        return out

    def op(self, eng, fn, reads=(), writes=(), dma=False, acc=False, late=False):
        if not late:
            rec = _Rec()
            fn(rec)
            assert rec.call is not None
            name, a, kw = rec.call
            fn = (lambda h, name=name, a=a, kw=kw: getattr(h, name)(*a, **kw))
        deps = {}
        own = self.sem[eng]

        def add(d):
            for s, v in d.items():
                if deps.get(s, 0) < v:
                    deps[s] = v
        for t in reads:
            add(t.w)
        for t in writes:
            if acc and eng == "pe":
                add({s: v for s, v in t.w.items() if s is not own})
            else:
                add(t.w)
            add(t.r)
        if dma:
            q = eng
            i = self.slot_i[q]
            self.slot_i[q] = (i + 1) % NSLOT
            ssem = self.slots[q][i]
            add({ssem: self.slot_cnt[q][i]})
            self.slot_cnt[q][i] += 16
            ticket = (ssem, self.slot_cnt[q][i])
            inc = (ssem, 16)
        else:
            self.cnt[eng] += 1
            ticket = (own, self.cnt[eng])
            inc = (own, 1)
        waits = self._need(eng, deps)
        self.ops[eng].append((waits, fn, inc))
        self.n_ops += 1
        for t in reads:
            if t.r.get(ticket[0], 0) < ticket[1]:
                t.r[ticket[0]] = ticket[1]
            self.touched.add(t)
        for t in writes:
            if acc:
                t.w[ticket[0]] = ticket[1]
            else:
                t.w = {ticket[0]: ticket[1]}
                t.r = {}
            self.touched.add(t)
        return ticket

    def barrier(self):
        deps = {}
        for e in ENGS:
            if self.cnt[e] > 0:
                deps[self.sem[e]] = self.cnt[e]
        for q in DMA_Q:
            for i in range(NSLOT):
                if self.slot_cnt[q][i] > 0:
                    deps[self.slots[q][i]] = self.slot_cnt[q][i]
        waits = self._need("sp", deps)
        self.bar_cnt += 1
        bar, bc = self.bar, self.bar_cnt
        self.ops["sp"].append((waits, lambda e, bar=bar: e.sem_inc(bar, 1), None))
        for e in ENGS:
            if e != "sp":
                self.ops[e].append(([(bar, bc)], None, None))
            for s, v in deps.items():
                if self.seen[e].get(s, 0) < v:
                    self.seen[e][s] = v
        for t in self.touched:
            t.w = {}
            t.r = {}
        self.touched = set()

    def emit(self, block):
        nc = self.nc
        handles = {"pe": "tensor", "act": "scalar", "dve": "vector", "pool": "gpsimd", "sp": "sync"}

        def make(e):
            def body(h):
                if e in self.pre:
                    self.pre[e](h)
                for waits, fn, inc in self.ops[e]:
                    for s, v in waits:
                        h.wait_ge(s, v)
                    if fn is not None:
                        ins = fn(h)
                        if inc is not None:
                            ins.then_inc(inc[0], inc[1])
            return body
        for e in ENGS:
            getattr(block, handles[e])(make(e))


class Arena:
    def __init__(self, nc, es, name, cols, dtype):
        self.t = es.enter_context(nc.sbuf_tensor(name, [128, cols], dtype))
        self.cols = cols
        self.off = 0
        self.name = name

    def alloc(self, cols, name="", parts=128):
        assert self.off + cols <= self.cols, f"arena {self.name} overflow {self.off}+{cols}>{self.cols} ({name})"
        ap = self.t[0:parts, self.off:self.off + cols]
        self.off += cols
        return Tile(ap, name)

    def allocn(self, n, cols, name="", parts=128):
        return [self.alloc(cols, f"{name}{i}", parts) for i in range(n)]

    def reset(self, off=0):
        self.off = off


class Ring:
    def __init__(self, tiles):
        self.tiles = tiles
        self.i = 0

    def next(self):
        t = self.tiles[self.i]
        self.i = (self.i + 1) % len(self.tiles)
        return t


import numpy as np
from contextlib import ExitStack
import concourse.bass as bass
import concourse.mybir as mybir
from concourse.ap import AP
from concourse.bass_utils import run_bass_kernel_spmd

D = 2048
KC = 16
CTX = 256
GW = 64
N_IN = 14112
C_LRU_X, C_LRU_G, C_NA_Q, C_NA_K, C_NA_V = 0, 512, 1024, 1536, 2048
C_SSD_Z, C_SSD_X, C_SSD_B, C_SSD_C, C_SSD_DT = 2560, 3072, 3584, 3712, 3840
C_ML_Q, C_ML_K, C_ML_V, C_ML_O, C_ML_G, C_GATE = 3856, 4368, 4880, 5392, 5904, 5920


class Cfg:
    def __init__(self, SEQ, FD, FE, layers=(0, 1), dbg=(), NS=1):
        self.SEQ, self.FD, self.FE, self.layers, self.dbg = SEQ, FD, FE, tuple(layers), tuple(dbg)
        self.NS = NS
        self.TL = SEQ // NS
        self.NG = CTX + SEQ
        self.NT = self.NG // 128
        self.ROWS = SEQ // GW


def blocks(lo, hi, step):
    return [(a, min(step, hi - a)) for a in range(lo, hi, step)]


class K:
    def __init__(self, cfg):
        self.cfg = cfg
        self.es = ExitStack()
        nc = self.nc = bass.Bass("TRN2", target_bir_lowering=False)
        self.S = Sched(nc, self.es)
        self.ins = {}
        self.outs = {}
        self.scr = {}
        self.AF = Arena(nc, self.es, "af32", 24 * 1024, F32)
        self.AB = Arena(nc, self.es, "abf16", 55 * 1024, BF16)
        self.ps = [Tile(self.es.enter_context(nc.psum_tensor(f"ps{i}", [128, 512], F32))[:], f"ps{i}") for i in range(8)]
        self.psr = Ring(self.ps)

    def inp(self, name, shape, dt=F32):
        t = self.nc.dram_tensor(name, list(shape), dt, kind="ExternalInput").ap()
        self.ins[name] = t
        return t

    def out(self, name, shape, dt=F32):
        t = self.nc.dram_tensor(name, list(shape), dt, kind="ExternalOutput").ap()
        self.outs[name] = t
        return t

    def dram(self, name, shape, dt=F32):
        if name in self.cfg.dbg:
            return self.out(name, shape, dt)
        t = self.nc.dram_tensor(name, list(shape), dt, kind="Internal").ap()
        self.scr[name] = t
        return t

    def tbuf(self, name, shape, dt=F32):
        key = (name, self.TN)
        if not hasattr(self, "_tb"):
            self._tb = {}
        if key not in self._tb:
            self._tb[key] = self.dram(f"{name}_{self.TN}", shape, dt)
        return self._tb[key]

    def dma(self, q, out, in_, reads=(), writes=(), **kw):
        return self.S.op(q, lambda e: e.dma_start(out=out, in_=in_, **kw), reads=reads, writes=writes, dma=True)

    def phase(self):
        self.S.barrier()
        self.AF.reset(self.af_base)
        self.AB.reset(self.ab_base)

    def build(self):
        cfg = self.cfg
        S, nc = self.S, self.nc
        NG = cfg.NG
        x_in = self.inp("x", [cfg.SEQ, D])
        ctx_in = self.inp("ctx", [CTX, D])
        cT = self.inp("cT", [128, KC, 2])
        ident_in = self.inp("ident", [128, 128])
        w_ada = self.inp("w_ada", [2, D, 6 * D])
        b_ada = self.inp("b_ada", [2, 6 * D])
        w_in = self.inp("w_in", [2, D, N_IN])
        self.xmT = self.dram("xmT", [128, KC, NG], BF16)
        self.modD = self.dram("modD", [2, 2, 6 * D])
        self.ident = self.AF.alloc(128, "ident")
        self.identb = self.AB.alloc(128, "identb")
        self.af_base, self.ab_base = self.AF.off, self.AB.off
        self.dma("sp", self.ident.ap, ident_in, writes=[self.ident])
        S.op("dve", lambda e: e.tensor_copy(out=self.identb.ap, in_=self.ident.ap), reads=[self.ident], writes=[self.identb])
        V = self.V = {}
        V["lru_cw"] = self.inp("lru_cw", [2, 128, 4, 4]); V["lru_cb"] = self.inp("lru_cb", [2, 128, 4]); V["lru_vec"] = self.inp("lru_vec", [2, 128, 6, 4])
        V["lru_wa"] = self.inp("lru_wa", [2, 2, 8, 64, 64]); V["lru_wx"] = self.inp("lru_wx", [2, 2, 8, 64, 64])
        V["ssd_cw"] = self.inp("ssd_cw", [2, 128, 6, 4]); V["ssd_cb"] = self.inp("ssd_cb", [2, 128, 6]); V["ssd_hp"] = self.inp("ssd_hp", [2, 8, 4])
        V["ssd_dskip"] = self.inp("ssd_dskip", [2, 512]); V["ssd_gn"] = self.inp("ssd_gn", [2, 512]); V["tri"] = self.inp("tri", [2, 128, 128]); V["sel40"] = self.inp("sel40", [40, 8, 128])
        V["ml_cw"] = self.inp("ml_cw", [2, 128, 8, 4]); V["ml_cb"] = self.inp("ml_cb", [2, 128, 8]); V["ml_hp"] = self.inp("ml_hp", [2, 4, 4]); V["sel24"] = self.inp("sel24", [24, 4, 128])
        V["rope_cos"] = self.inp("rope_cos", [128, cfg.SEQ]); V["rope_sin"] = self.inp("rope_sin", [128, cfg.SEQ]); V["ropeP"] = self.inp("ropeP", [128, 128])
        V["na_bm"] = self.inp("na_bm", [2, len(na_rowinfo(cfg.ROWS)[1]), 8, 64, 640])
        V["moe_wr_p"] = self.inp("moe_wr_p", [1, 128, KC * 8]); V["moe_br_p"] = self.inp("moe_br_p", [1, 128, 8])
        V["moe_w_gate"] = self.inp("moe_w_gate", [1, 8, D, cfg.FE]); V["moe_w_up"] = self.inp("moe_w_up", [1, 8, D, cfg.FE]); V["moe_w_down"] = self.inp("moe_w_down", [1, 8, cfg.FE, D])
        V["w_branch"] = self.inp("w_branch", [2, 4, 512, D]); V["w_out"] = self.inp("w_out", [2, D, D])
        V["ln_g"] = self.inp("ln_g", [2, 2, D]); V["ln_b"] = self.inp("ln_b", [2, 2, D])
        V["ffn_w_gate"] = self.inp("ffn_w_gate", [1, D, cfg.FD]); V["ffn_w_up"] = self.inp("ffn_w_up", [1, D, cfg.FD]); V["ffn_w_down"] = self.inp("ffn_w_down", [1, cfg.FD, D])
        self.y_out = self.out("y_out", [cfg.TL, D])
        self.TN, self.t_ctx, self.dyn_base = NG, CTX, None
        if cfg.NS > 1:
            def _pre(h, self=self, cfg=cfg):
                self.sval = (h.partition_id() % cfg.NS) * cfg.TL
            S.pre["sp"] = _pre
        self.phase_mod(cT, w_ada, b_ada)
        for l in cfg.layers:
            self.layer(l, x_in, ctx_in, w_in)
        S.barrier()
        with nc.Block() as block:
            S.emit(block)
        return nc

    def phase_mod(self, cT, w_ada, b_ada):
        S = self.S
        self.phase()
        ct = self.AF.alloc(KC * 2, "ct")
        self.dma("sp", ct.ap, cT.rearrange("p k m -> p (k m)"), writes=[ct])
        ca = self.AF.alloc(KC * 2, "ca")
        S.op("act", lambda e: e.activation(out=ca.ap, in_=ct.ap, func=AF.Silu), reads=[ct], writes=[ca])
        cav = ca.ap.rearrange("p (k m) -> p k m", m=2)
        wr = Ring(self.AF.allocn(2, KC * 512, "wada"))
        orow = Ring(self.AF.allocn(2, 512, "orow", parts=2))
        brow = Ring(self.AF.allocn(2, 512, "brow", parts=2))
        for l in self.cfg.layers:
            for nb in range(6 * D // 512):
                wt = wr.next()
                wv = wt.ap.rearrange("p (k n) -> p k n", k=KC)
                self.dma("sp" if nb % 2 == 0 else "act", wv, w_ada[l, :, nb * 512:(nb + 1) * 512].rearrange("(k p) n -> p k n", p=128), writes=[wt])
                bt = brow.next()
                for m in range(2):
                    self.dma("pool", bt.ap[m:m + 1, :], b_ada[l:l + 1, nb * 512:(nb + 1) * 512], writes=[bt])
                ps = self.psr.next()
                for k in range(KC):
                    S.op("pe", lambda e, k=k, wv=wv, ps=ps: e.matmul(ps.ap[0:2, :], lhsT=cav[:, k, :], rhs=wv[:, k, :], start=(k == 0), stop=(k == KC - 1)),
                         reads=[ca, wt], writes=[ps], acc=k > 0)
                ot = orow.next()
                S.op("dve", lambda e, ot=ot, ps=ps, bt=bt: e.tensor_tensor(out=ot.ap, in0=ps.ap[0:2, :], in1=bt.ap, op=ALU.add), reads=[ps, bt], writes=[ot])
                self.dma("sp", self.modD[l, :, nb * 512:(nb + 1) * 512], ot.ap, reads=[ot])

    def bcast_row(self, q, tile, src_row_ap, n):
        self.dma(q, tile.ap[:, 0:n], src_row_ap.to_broadcast([128, n]) if hasattr(src_row_ap, "to_broadcast") else src_row_ap, writes=[tile])

    def phase_xt(self, l, x_src, ctx_src, shift_i, scale_i, router=None, N=None, tctx=CTX, dst=None):
        S, cfg = self.S, self.cfg
        N = cfg.NG if N is None else N
        dst = self.xmT if dst is None else dst
        self.phase()
        rows = {}
        for kind in range(2):
            sc = self.AF.alloc(D, f"sc{kind}")
            sh = self.AF.alloc(D, f"sh{kind}")
            self.dma("sp", sc.ap, self.modD[l, kind:kind + 1, scale_i * D:(scale_i + 1) * D].to_broadcast([128, D]), writes=[sc])
            self.dma("act", sh.ap, self.modD[l, kind:kind + 1, shift_i * D:(shift_i + 1) * D].to_broadcast([128, D]), writes=[sh])
            S.op("dve", lambda e, sc=sc: e.tensor_scalar_add(out=sc.ap, in0=sc.ap, scalar1=1.0), reads=[sc], writes=[sc])
            rows[kind] = (sc, sh)
        xr = Ring(self.AF.allocn(2, D, "xtile"))
        xo = Ring(self.AB.allocn(2, KC * 128, "xmt"))
        if router is not None:
            wr_ap, br_ap = router
            self.combT = self.tbuf("combT", [8, N])
            WR = self.AF.alloc(KC * 8, "WR")
            self.dma("sp", WR.ap, wr_ap, writes=[WR])
            BR = self.AF.alloc(8, "BR")
            self.dma("sp", BR.ap, br_ap, writes=[BR])
            hr = Ring(self.AF.allocn(2, D, "hT"))
            lr = Ring(self.AF.allocn(2, 40, "lg"))
            cr = Ring(self.AF.allocn(4, 128, "cT", parts=8))
        for ti in range(N // 128):
            kind = 1 if ti * 128 < tctx else 0
            src = ctx_src[ti * 128:(ti + 1) * 128, :] if kind == 1 else x_src[ti * 128 - tctx:(ti + 1) * 128 - tctx, :]
            xt = xr.next()
            self.dma("sp" if ti % 2 == 0 else "act", xt.ap, src, writes=[xt])
            sc, sh = rows[kind]
            S.op("dve", lambda e, xt=xt, sc=sc: e.tensor_tensor(out=xt.ap, in0=xt.ap, in1=sc.ap, op=ALU.mult), reads=[xt, sc], writes=[xt])
            S.op("pool", lambda e, xt=xt, sh=sh: e.tensor_tensor(out=xt.ap, in0=xt.ap, in1=sh.ap, op=ALU.add), reads=[xt, sh], writes=[xt])
            ot = xo.next()
            for g in range(4):
                ps = self.psr.next()
                for j in range(4):
                    k = g * 4 + j
                    S.op("pe", lambda e, ps=ps, xt=xt, j=j, k=k: e.transpose(ps.ap[:, j * 128:(j + 1) * 128], xt.ap[:, k * 128:(k + 1) * 128], self.ident.ap),
                         reads=[xt, self.ident], writes=[ps], acc=j > 0)
                if router is not None:
                    if g == 0:
                        HT = hr.next()
                    if g % 2 == 0:
                        S.op("act", lambda e, ps=ps, HT=HT, g=g: e.copy(out=HT.ap[:, g * 512:(g + 1) * 512], in_=ps.ap), reads=[ps], writes=[HT], acc=True)
                    else:
                        S.op("dve", lambda e, ps=ps, HT=HT, g=g: e.tensor_copy(out=HT.ap[:, g * 512:(g + 1) * 512], in_=ps.ap), reads=[ps], writes=[HT], acc=True)
                eng = "act" if g % 2 == 0 else "dve"
                if eng == "act":
                    S.op("act", lambda e, ps=ps, ot=ot, g=g: e.copy(out=ot.ap[:, g * 512:(g + 1) * 512], in_=ps.ap), reads=[ps], writes=[ot], acc=True)
                else:
                    S.op("dve", lambda e, ps=ps, ot=ot, g=g: e.tensor_copy(out=ot.ap[:, g * 512:(g + 1) * 512], in_=ps.ap), reads=[ps], writes=[ot], acc=True)
            self.dma("sp", dst[:, :, ti * 128:(ti + 1) * 128], ot.ap.rearrange("p (k t) -> p k t", k=KC), reads=[ot])
            if router is not None:
                if "r0" in cfg.dbg: continue
                psL = self.psr.next()
                for k in range(KC):
                    S.op("pe", lambda e, psL=psL, HT=HT, k=k: e.matmul(psL.ap[0:8, 0:128], lhsT=WR.ap[:, k * 8:(k + 1) * 8], rhs=HT.ap[:, k * 128:(k + 1) * 128], start=(k == 0), stop=(k == KC - 1)), reads=[HT, WR], writes=[psL], acc=k > 0)
                if "r05" in cfg.dbg: continue
                LT = cr.next()
                S.op("act", lambda e: e.copy(out=LT.ap, in_=psL.ap[0:8, 0:128]), reads=[psL], writes=[LT])
                psL = self.psr.next()
                S.op("pe", lambda e: e.transpose(psL.ap[:, 0:8], LT.ap, self.ident.ap[0:8, 0:8]), reads=[LT, self.ident], writes=[psL])
                if "r1" in cfg.dbg: continue
                Lg = lr.next()
                L = Lg.ap[:, 0:8]; E1 = Lg.ap[:, 8:16]; L2 = Lg.ap[:, 16:24]; Pm = Lg.ap[:, 24:32]; sc_ = Lg.ap[:, 32:40]
                S.op("dve", lambda e: e.tensor_tensor(out=L, in0=psL.ap[:, 0:8], in1=BR.ap, op=ALU.add), reads=[psL, BR], writes=[Lg])
                S.op("dve", lambda e: e.reduce_max(out=sc_[:, 0:1], in_=L, axis=AX.X), reads=[Lg], writes=[Lg])
                S.op("dve", lambda e: e.tensor_scalar(out=E1, in0=L, scalar1=sc_[:, 0:1], scalar2=None, op0=ALU.is_equal), reads=[Lg], writes=[Lg])
                S.op("dve", lambda e: e.scalar_tensor_tensor(out=L2, in0=E1, scalar=-1e30, in1=L, op0=ALU.mult, op1=ALU.add), reads=[Lg], writes=[Lg])
                S.op("dve", lambda e: e.reduce_max(out=sc_[:, 1:2], in_=L2, axis=AX.X), reads=[Lg], writes=[Lg])
                S.op("dve", lambda e: e.tensor_scalar(out=E1, in0=L, scalar1=sc_[:, 1:2], scalar2=None, op0=ALU.is_ge), reads=[Lg], writes=[Lg])
                S.op("dve", lambda e: e.tensor_scalar_mul(out=sc_[:, 2:3], in0=sc_[:, 0:1], scalar1=-1.0), reads=[Lg], writes=[Lg])
                S.op("act", lambda e: e.activation(out=Pm, in_=L, func=AF.Exp, bias=sc_[:, 2:3]), reads=[Lg], writes=[Lg])
                S.op("dve", lambda e: e.tensor_tensor(out=Pm, in0=Pm, in1=E1, op=ALU.mult), reads=[Lg], writes=[Lg])
                S.op("dve", lambda e: e.reduce_sum(out=sc_[:, 3:4], in_=Pm, axis=AX.X), reads=[Lg], writes=[Lg])
                S.op("dve", lambda e: e.reciprocal(out=sc_[:, 4:5], in_=sc_[:, 3:4]), reads=[Lg], writes=[Lg])
                S.op("dve", lambda e: e.tensor_scalar_mul(out=Pm, in0=Pm, scalar1=sc_[:, 4:5]), reads=[Lg], writes=[Lg])
                if "r2" in cfg.dbg: continue
                psT = self.psr.next()
                S.op("pe", lambda e: e.transpose(psT.ap[0:8, 0:128], Pm, self.ident.ap), reads=[Lg, self.ident], writes=[psT])
                CT = cr.next()
                S.op("act", lambda e: e.copy(out=CT.ap, in_=psT.ap[0:8, 0:128]), reads=[psT], writes=[CT])
                self.dma("act", self.combT[:, ti * 128:(ti + 1) * 128], CT.ap, reads=[CT])

    def proj_fm(self, W, c0, ncols, tok_blocks, sink, wring, xring):
        S = self.S
        wt = wring.next()
        wv = wt.ap.rearrange("p (k n) -> p k n", k=KC)
        self.dma("pool", wv[:, :, 0:ncols], W[:, c0:c0 + ncols].rearrange("(k p) n -> p k n", p=128), writes=[wt])
        chunks = blocks(0, ncols, 128)
        for (g0, nb) in tok_blocks:
            xb = xring.next()
            xv = xb.ap.rearrange("p (k t) -> p k t", k=KC)
            self.dma("sp", xv[:, :, 0:nb], self.xmT[:, :, g0:g0 + nb], writes=[xb])
            for ci, (cc0, cn) in enumerate(chunks):
                ps = self.psr.next()
                for k in range(KC):
                    S.op("pe", lambda e, ps=ps, wv=wv, xv=xv, k=k, cc0=cc0, cn=cn, nb=nb: e.matmul(ps.ap[0:cn, 0:nb], lhsT=wv[:, k, cc0:cc0 + cn], rhs=xv[:, k, 0:nb], start=(k == 0), stop=(k == KC - 1)),
                         reads=[wt, xb], writes=[ps], acc=k > 0)
                sink(ci, c0 + cc0, cn, g0, nb, ps)

    def layer(self, l, x_in, ctx_in, w_in):
        cfg, S = self.cfg, self.S
        NG = cfg.NG
        if l == cfg.layers[0]:
            self.phase_xt(l, x_in, ctx_in, 0, 1)
        else:
            self.phase_xt(l, self.xres[1][CTX:NG, :], self.xres[1][0:CTX, :], 0, 1)
        self.phase()
        W = w_in[l]
        if not hasattr(self, "pfm"):
            self.pfm = self.dram("pfm", [N_IN - 8192, NG])
        wring = Ring(self.AB.allocn(2, KC * 512, "wt"))
        xring = Ring(self.AB.allocn(3, KC * 512, "xb"))
        oring = Ring(self.AF.allocn(4, 512, "ost"))
        cnt = [0]

        def sink(ci, col, cn, g0, nb, ps):
            ot = oring.next()
            if cnt[0] % 2 == 0:
                S.op("act", lambda e: e.copy(out=ot.ap[0:cn, 0:nb], in_=ps.ap[0:cn, 0:nb]), reads=[ps], writes=[ot])
            else:
                S.op("dve", lambda e: e.tensor_copy(out=ot.ap[0:cn, 0:nb], in_=ps.ap[0:cn, 0:nb]), reads=[ps], writes=[ot])
            cnt[0] += 1
            self.dma("act", self.pfm[col:col + cn, g0:g0 + nb], ot.ap[0:cn, 0:nb], reads=[ot])
        tb = blocks(0, NG, 512)
        for c0 in range(0, C_GATE, 512):
            self.proj_fm(W, c0, min(512, C_GATE - c0), tb, sink, wring, xring)
        V = self.V
        if "inj" in cfg.dbg:
            if not hasattr(self, "yT"):
                self.yT = self.inp("yT_in", [4, 512, NG], BF16)
        else:
            if "nolru" not in cfg.dbg:
                self.lru_phase(l, V)
            elif not hasattr(self, "yT"):
                self.yT = self.dram("yT", [4, 512, NG], BF16)
            if "nossd" not in cfg.dbg:
                self.ssd_phase(l, V)
            if "noml" not in cfg.dbg:
                self.mlstm_phase(l, V)
            zt = []
            if "nolru" in cfg.dbg: zt.append(self.yT[0])
            if "nossd" in cfg.dbg: zt.append(self.yT[2])
            if "noml" in cfg.dbg: zt.append(self.yT[3])
            if "nona" not in cfg.dbg:
                self.na_phase(l, V)
            else:
                zt.append(self.yT[1])
            if zt:
                self.zero_fill_phase(zt)
        if "stopmix" in cfg.dbg:
            return
        if not hasattr(self, "xres"):
            self.xres = [self.dram("xres0", [NG, D]), self.dram("xres1", [NG, D])]
        last = (l == cfg.layers[-1])
        own = last and cfg.NS > 1
        router = (V["moe_wr_p"][l // 2], V["moe_br_p"][l // 2]) if (l % 2 == 1 and "norouter" not in cfg.dbg) else None
        xs, cs = (x_in, ctx_in) if l == cfg.layers[0] else (self.xres[1][CTX:NG, :], self.xres[1][0:CTX, :])
        if not own:
            self.TN, self.t_ctx, self.dyn_base = NG, CTX, None
            self.merge_phase(l, W, V["w_branch"][l], V["w_out"][l])
            self.epi_phase(l, xs, cs, 2, 0, self.xres[0], V["ln_g"], V["ln_b"])
            self.xmT2 = self.xmT
            self.phase_xt(l, self.xres[0][CTX:NG, :], self.xres[0][0:CTX, :], 3, 4, router=router)
            x1x, x1c = self.xres[0][CTX:NG, :], self.xres[0][0:CTX, :]
            dst2 = self.xres[1]
        else:
            TL = cfg.TL
            self.phase()
            yT_o = self.tbuf("yT_o", [4, 512, TL], BF16); xmT_o = self.tbuf("xmT_o", [128, KC, TL], BF16); x_o = self.tbuf("x_o", [TL, D])
            for n in range(4):
                S.op("sp", lambda h, n=n: h.dma_start(out=yT_o[n], in_=self.yT_full[n][:, CTX:NG][:, bass.ds(self.sval, TL)]), dma=True, late=True)
            for kh in range(2):
                S.op("sp", lambda h, kh=kh: h.dma_start(out=xmT_o[:, kh * 8:(kh + 1) * 8, :], in_=self.xmT_full[:, kh * 8:(kh + 1) * 8, CTX:NG][:, :, bass.ds(self.sval, TL)]), dma=True, late=True)
            for q in range(4):
                S.op("sp", lambda h, q=q: h.dma_start(out=x_o[q * (TL // 4):(q + 1) * (TL // 4), :], in_=xs[bass.ds(self.sval, TL), :][q * (TL // 4):(q + 1) * (TL // 4), :]), dma=True, late=True)
            self.yT_full, self.xmT_full = self.yT, self.xmT
            self.yT, self.xmT = yT_o, xmT_o
            self.TN, self.t_ctx, self.dyn_base = TL, 0, None
            self.merge_phase(l, W, V["w_branch"][l], V["w_out"][l])
            xo = self.tbuf("xres_o0", [TL, D])
            self.epi_phase(l, x_o, None, 2, 0, xo, V["ln_g"], V["ln_b"])
            self.xmT2 = self.tbuf("xmT2", [128, KC, TL], BF16)
            self.phase_xt(l, xo, None, 3, 4, router=router, N=TL, tctx=0, dst=self.xmT2)
            self.yT, self.xmT = self.yT_full, self.xmT_full
            x1x, x1c = xo, None
            dst2 = self.tbuf("xres_o1", [TL, D])
        if "stopxt2" in cfg.dbg and l % 2 == 1:
            return
        if l % 2 == 0:
            self.ffn_dense_phase(l, V["ffn_w_gate"][l // 2], V["ffn_w_up"][l // 2], V["ffn_w_down"][l // 2], cfg.FD)
        else:
            self.moe_phase(l, V)
        self.epi_phase(l, x1x, x1c, 5, 1, dst2, V["ln_g"], V["ln_b"], out_ext=self.y_out if last else None)
        self.TN, self.t_ctx, self.dyn_base = NG, CTX, None


def rev_ap(ap2d, n):
    return AP(ap2d.tensor, ap2d.offset + (n - 1) * ap2d.ap[-1][0], [list(ap2d.ap[0]), [-ap2d.ap[-1][0], n]])


def _segments(cfg):
    return [(0, CTX), (CTX, cfg.NG)]


def lru_phase(self, l, V):
    cfg, S = self.cfg, self.S
    NG = cfg.NG
    NB = 1024
    if not hasattr(self, "hD"):
        self.hD = self.dram("hD", [2, 512, NG])
        self.yT = self.dram("yT", [4, 512, NG], BF16)
    self.phase()
    AFa, ABa = self.AF, self.AB
    cw = AFa.alloc(16, "cw"); self.dma("sp", cw.ap, V["lru_cw"][l].rearrange("p c k -> p (c k)"), writes=[cw])
    cb = AFa.alloc(4, "cb"); self.dma("sp", cb.ap, V["lru_cb"][l], writes=[cb])
    pv = AFa.alloc(24, "pv"); self.dma("sp", pv.ap, V["lru_vec"][l].rearrange("p a c -> p (a c)"), writes=[pv])
    spc = AFa.alloc(8, "spc")
    S.op("act", lambda e: e.activation(out=spc.ap, in_=pv.ap[:, 16:24], func=AF.Exp, scale=-1.0), reads=[pv], writes=[spc])
    S.op("act", lambda e: e.activation(out=spc.ap, in_=spc.ap, func=AF.Ln, bias=1.0), reads=[spc], writes=[spc])
    S.op("dve", lambda e: e.tensor_scalar_mul(out=spc.ap, in0=spc.ap, scalar1=-8.0), reads=[spc], writes=[spc])
    wbd = {}
    for d in range(2):
        for wi, nm in enumerate(("lru_wa", "lru_wx")):
            for c in range(4):
                t = ABa.alloc(128, f"wbd{d}{wi}{c}")
                S.op("pool", lambda e, t=t: e.memset(t.ap, 0.0), writes=[t])
                for hblk in range(2):
                    self.dma("pool", t.ap[hblk * 64:(hblk + 1) * 64, hblk * 64:(hblk + 1) * 64], V[nm][l, d, 2 * c + hblk], writes=[t], reads=[t])
                wbd[(d, wi, c)] = t
    zero1 = AFa.alloc(1, "zero1")
    S.op("dve", lambda e: e.memset(zero1.ap, 0.0), writes=[zero1])
    names = ("XH", "XC", "R", "I", "A", "U", "H")
    rings = {n: Ring(AFa.allocn(2, NB + 4, n)) for n in names}
    xbr = Ring(ABa.allocn(2, NB, "XB"))
    carr = Ring(AFa.allocn(4, 1, "carry"))
    for c in range(4):
        for d in range(2):
            carry = zero1
            for (s0, s1) in _segments(cfg):
                blks = blocks(s0, s1, NB)
                if d == 1:
                    blks = blks[::-1]
                for (g0, nb) in blks:
                    XH, XC, R, I, A, U, H = [rings[n].next() for n in names]
                    XB = xbr.next()
                    lo = max(g0 - 2, s0); hi = min(g0 + nb + 1, s1)
                    S.op("pool", lambda e, XH=XH: e.memset(XH.ap[:, 0:NB + 4], 0.0), writes=[XH])
                    self.dma("sp", XH.ap[:, lo - (g0 - 2):hi - (g0 - 2)], self.pfm[c * 128:(c + 1) * 128, lo:hi], writes=[XH], reads=[XH])
                    S.op("dve", lambda e, XH=XH, XC=XC, nb=nb, c=c: e.tensor_scalar(out=XC.ap[:, 0:nb], in0=XH.ap[:, 0:nb], scalar1=cw.ap[:, c * 4:c * 4 + 1], scalar2=cb.ap[:, c:c + 1], op0=ALU.mult, op1=ALU.add), reads=[XH, cw, cb], writes=[XC])
                    for k in range(1, 4):
                        S.op("dve", lambda e, XH=XH, XC=XC, nb=nb, c=c, k=k: e.scalar_tensor_tensor(out=XC.ap[:, 0:nb], in0=XH.ap[:, k:k + nb], scalar=cw.ap[:, c * 4 + k:c * 4 + k + 1], in1=XC.ap[:, 0:nb], op0=ALU.mult, op1=ALU.add), reads=[XH, XC, cw], writes=[XC])
                    S.op("act", lambda e, XB=XB, XC=XC, nb=nb: e.copy(out=XB.ap[:, 0:nb], in_=XC.ap[:, 0:nb]), reads=[XC], writes=[XB])
                    for wi, (dst, bcol) in enumerate(((R, 0 + d), (I, 2 + d))):
                        for (o, n) in blocks(0, nb, 512):
                            ps = self.psr.next()
                            w = wbd[(d, wi, c)]
                            S.op("pe", lambda e, ps=ps, w=w, XB=XB, o=o, n=n: e.matmul(ps.ap[:, 0:n], lhsT=w.ap, rhs=XB.ap[:, o:o + n], start=True, stop=True), reads=[w, XB], writes=[ps])
                            S.op("act", lambda e, ps=ps, dst=dst, o=o, n=n, bcol=bcol, c=c: e.activation(out=dst.ap[:, o:o + n], in_=ps.ap[:, 0:n], func=AF.Sigmoid, bias=pv.ap[:, bcol * 4 + c:bcol * 4 + c + 1]), reads=[ps, pv], writes=[dst], acc=True)
                    S.op("act", lambda e, A=A, R=R, nb=nb, c=c, d=d: e.activation(out=A.ap[:, 0:nb], in_=R.ap[:, 0:nb], func=AF.Exp, scale=spc.ap[:, d * 4 + c:d * 4 + c + 1]), reads=[R, spc], writes=[A])
                    S.op("dve", lambda e, A=A, R=R, nb=nb: e.tensor_tensor(out=R.ap[:, 0:nb], in0=A.ap[:, 0:nb], in1=A.ap[:, 0:nb], op=ALU.mult), reads=[A], writes=[R])
                    S.op("act", lambda e, R=R, nb=nb: e.activation(out=R.ap[:, 0:nb], in_=R.ap[:, 0:nb], func=AF.Sqrt, scale=-1.0, bias=1.0), reads=[R], writes=[R])
                    S.op("dve", lambda e, U=U, R=R, I=I, nb=nb: e.tensor_tensor(out=U.ap[:, 0:nb], in0=R.ap[:, 0:nb], in1=I.ap[:, 0:nb], op=ALU.mult), reads=[R, I], writes=[U])
                    S.op("dve", lambda e, U=U, XC=XC, nb=nb: e.tensor_tensor(out=U.ap[:, 0:nb], in0=U.ap[:, 0:nb], in1=XC.ap[:, 0:nb], op=ALU.mult), reads=[U, XC], writes=[U])
                    f = (lambda a, nb=nb: a[:, 0:nb]) if d == 0 else (lambda a, nb=nb: rev_ap(a[:, 0:nb], nb))
                    S.op("dve", lambda e, H=H, A=A, U=U, f=f, carry=carry: e.tensor_tensor_scan(out=f(H.ap), data0=f(A.ap), data1=f(U.ap), initial=carry.ap[:, 0:1], op0=ALU.mult, op1=ALU.add), reads=[A, U, carry], writes=[H])
                    ncar = carr.next()
                    lastcol = nb - 1 if d == 0 else 0
                    S.op("act", lambda e, ncar=ncar, H=H, lastcol=lastcol: e.copy(out=ncar.ap, in_=H.ap[:, lastcol:lastcol + 1]), reads=[H], writes=[ncar])
                    carry = ncar
                    self.dma("act", self.hD[d, c * 128:(c + 1) * 128, g0:g0 + nb], H.ap[:, 0:nb], reads=[H])
    self.phase()
    rr = {n: Ring(self.AF.allocn(2, NB, n)) for n in ("H0", "H1", "G", "T")}
    yo = Ring(self.AB.allocn(2, NB, "Y"))
    for c in range(4):
        for (g0, nb) in blocks(0, NG, NB):
            H0, H1, G, T = [rr[n].next() for n in ("H0", "H1", "G", "T")]
            Y = yo.next()
            self.dma("sp", H0.ap[:, 0:nb], self.hD[0, c * 128:(c + 1) * 128, g0:g0 + nb], writes=[H0])
            self.dma("act", H1.ap[:, 0:nb], self.hD[1, c * 128:(c + 1) * 128, g0:g0 + nb], writes=[H1])
            self.dma("sp", G.ap[:, 0:nb], self.pfm[C_LRU_G + c * 128:C_LRU_G + (c + 1) * 128, g0:g0 + nb], writes=[G])
            S.op("pool", lambda e, H0=H0, H1=H1, nb=nb: e.tensor_tensor(out=H0.ap[:, 0:nb], in0=H0.ap[:, 0:nb], in1=H1.ap[:, 0:nb], op=ALU.add), reads=[H0, H1], writes=[H0])
            S.op("dve", lambda e, T=T, G=G, nb=nb: e.tensor_tensor(out=T.ap[:, 0:nb], in0=G.ap[:, 0:nb], in1=G.ap[:, 0:nb], op=ALU.mult), reads=[G], writes=[T])
            S.op("dve", lambda e, T=T, nb=nb: e.tensor_scalar(out=T.ap[:, 0:nb], in0=T.ap[:, 0:nb], scalar1=0.044715, scalar2=1.0, op0=ALU.mult, op1=ALU.add), reads=[T], writes=[T])
            S.op("dve", lambda e, T=T, G=G, nb=nb: e.tensor_tensor(out=T.ap[:, 0:nb], in0=T.ap[:, 0:nb], in1=G.ap[:, 0:nb], op=ALU.mult), reads=[T, G], writes=[T])
            S.op("act", lambda e, T=T, nb=nb: e.activation(out=T.ap[:, 0:nb], in_=T.ap[:, 0:nb], func=AF.Sigmoid, scale=1.5957691216057308), reads=[T], writes=[T])
            S.op("dve", lambda e, T=T, G=G, nb=nb: e.tensor_tensor(out=T.ap[:, 0:nb], in0=T.ap[:, 0:nb], in1=G.ap[:, 0:nb], op=ALU.mult), reads=[T, G], writes=[T])
            S.op("dve", lambda e, T=T, H0=H0, Y=Y, nb=nb: e.tensor_tensor(out=Y.ap[:, 0:nb], in0=T.ap[:, 0:nb], in1=H0.ap[:, 0:nb], op=ALU.mult), reads=[T, H0], writes=[Y])
            self.dma("sp", self.yT[0, c * 128:(c + 1) * 128, g0:g0 + nb], Y.ap[:, 0:nb], reads=[Y])


K.lru_phase = lru_phase


def conv_silu_phase(self, l, row0, nchunks, cwv, cbv, dst, post=None, out_f32=False):
    cfg, S = self.cfg, self.S
    NB = 1024
    self.phase()
    cw = self.AF.alloc(nchunks * 4, "cw"); self.dma("sp", cw.ap, cwv.rearrange("p c k -> p (c k)"), writes=[cw])
    cb = self.AF.alloc(nchunks, "cb"); self.dma("sp", cb.ap, cbv, writes=[cb])
    rXH = Ring(self.AF.allocn(2, NB + 4, "XH")); rXC = Ring(self.AF.allocn(2, NB, "XC")); rSG = Ring(self.AF.allocn(2, NB, "SG"))
    rO = Ring((self.AF if out_f32 else self.AB).allocn(2, NB, "O"))
    ctxp = dict(rings={})
    for c in range(nchunks):
        for (s0, s1) in _segments(cfg):
            for (g0, nb) in blocks(s0, s1, NB):
                XH, XC, SG, O = rXH.next(), rXC.next(), rSG.next(), rO.next()
                lo = max(g0 - 2, s0); hi = min(g0 + nb + 1, s1)
                S.op("pool", lambda e, XH=XH: e.memset(XH.ap[:, 0:NB + 4], 0.0), writes=[XH])
                self.dma("sp", XH.ap[:, lo - (g0 - 2):hi - (g0 - 2)], self.pfm[row0 + c * 128:row0 + (c + 1) * 128, lo:hi], writes=[XH], reads=[XH])
                S.op("dve", lambda e, XH=XH, XC=XC, nb=nb, c=c: e.tensor_scalar(out=XC.ap[:, 0:nb], in0=XH.ap[:, 0:nb], scalar1=cw.ap[:, c * 4:c * 4 + 1], scalar2=cb.ap[:, c:c + 1], op0=ALU.mult, op1=ALU.add), reads=[XH, cw, cb], writes=[XC])
                for k in range(1, 4):
                    S.op("dve", lambda e, XH=XH, XC=XC, nb=nb, c=c, k=k: e.scalar_tensor_tensor(out=XC.ap[:, 0:nb], in0=XH.ap[:, k:k + nb], scalar=cw.ap[:, c * 4 + k:c * 4 + k + 1], in1=XC.ap[:, 0:nb], op0=ALU.mult, op1=ALU.add), reads=[XH, XC, cw], writes=[XC])
                if post is None:
                    S.op("act", lambda e, O=O, XC=XC, nb=nb: e.activation(out=O.ap[:, 0:nb], in_=XC.ap[:, 0:nb], func=AF.Silu), reads=[XC], writes=[O])
                else:
                    S.op("act", lambda e, SG=SG, XC=XC, nb=nb: e.activation(out=SG.ap[:, 0:nb], in_=XC.ap[:, 0:nb], func=AF.Silu), reads=[XC], writes=[SG])
                    post(c, s0, g0, nb, SG, O, ctxp)
                self.dma("act", dst[c * 128:(c + 1) * 128, g0:g0 + nb], O.ap[:, 0:nb], reads=[O])


def softplus_ops(self, X, T, n, parts):
    S = self.S
    x = X.ap[0:parts, 0:n]; t = T.ap[0:parts, 0:n]
    S.op("act", lambda e: e.activation(out=t, in_=x, func=AF.Abs), reads=[X], writes=[T])
    S.op("act", lambda e: e.activation(out=t, in_=t, func=AF.Exp, scale=-1.0), reads=[T], writes=[T])
    S.op("act", lambda e: e.activation(out=t, in_=t, func=AF.Ln, bias=1.0), reads=[T], writes=[T])
    S.op("dve", lambda e: e.tensor_scalar_max(out=x, in0=x, scalar1=0.0), reads=[X], writes=[X])
    S.op("dve", lambda e: e.tensor_tensor(out=x, in0=x, in1=t, op=ALU.add), reads=[X, T], writes=[X])


def ssd_tab_phase(self, l, V):
    cfg, S = self.cfg, self.S
    NG = cfg.NG
    NB = 1024
    if not hasattr(self, "tabS"):
        self.tabS = self.dram("tabS", [2, 5, 8, NG])
    self.phase()
    pr = self.AF.alloc(4, "ssdp", parts=8)
    self.dma("sp", pr.ap, V["ssd_hp"][l], writes=[pr])
    Aneg = self.AF.alloc(2, "Aneg", parts=8)
    S.op("act", lambda e: e.activation(out=Aneg.ap, in_=pr.ap[:, 2:4], func=AF.Exp), reads=[pr], writes=[Aneg])
    S.op("dve", lambda e: e.tensor_scalar_mul(out=Aneg.ap, in0=Aneg.ap, scalar1=-1.0), reads=[Aneg], writes=[Aneg])
    msk = {}
    for d in range(2):
        m = self.AF.alloc(NB, f"rmask{d}", parts=8)
        S.op("dve", lambda e, m=m: e.memset(m.ap, 1.0), writes=[m])
        mv = m.ap.rearrange("p (c t) -> p c t", t=128)
        pos = 0 if d == 0 else 127
        S.op("dve", lambda e, mv=mv, pos=pos: e.memset(mv[:, :, pos:pos + 1], 0.0), writes=[m], reads=[m])
        msk[d] = m
    names = ("DT", "T", "DA", "CUM", "LAST", "W", "E", "DL")
    rr = {n: Ring(self.AF.allocn(2, NB, n, parts=8)) for n in names}
    for d in range(2):
        for (g0, nb) in blocks(0, NG, NB):
            DT, T, DA, CUM, LAST, W, E, DL = [rr[n].next() for n in names]
            sl = lambda X: X.ap[:, 0:nb]
            self.dma("sp", sl(DT), self.pfm[C_SSD_DT + d * 8:C_SSD_DT + d * 8 + 8, g0:g0 + nb], writes=[DT])
            S.op("dve", lambda e, DT=DT: e.tensor_scalar_add(out=DT.ap[:, 0:nb], in0=DT.ap[:, 0:nb], scalar1=pr.ap[:, d:d + 1]), reads=[DT, pr], writes=[DT])
            softplus_ops(self, DT, T, nb, 8)
            S.op("dve", lambda e, DA=DA, DT=DT: e.tensor_scalar_mul(out=DA.ap[:, 0:nb], in0=DT.ap[:, 0:nb], scalar1=Aneg.ap[:, d:d + 1]), reads=[DT, Aneg], writes=[DA])
            f = (lambda a: a) if d == 0 else (lambda a: rev_ap(a, nb))
            S.op("dve", lambda e, CUM=CUM, DA=DA, f=f, m=msk[d]: e.tensor_tensor_scan(out=f(CUM.ap[:, 0:nb]), data0=f(m.ap[:, 0:nb]), data1=f(DA.ap[:, 0:nb]), initial=0.0, op0=ALU.mult, op1=ALU.add), reads=[DA, msk[d]], writes=[CUM])
            endpos = 127 if d == 0 else 0
            cv = CUM.ap[:, 0:nb].rearrange("p (c t) -> p c t", t=128)
            lv = LAST.ap[:, 0:nb].rearrange("p (c t) -> p c t", t=128)
            S.op("dve", lambda e, cv=cv, lv=lv, CUM=CUM, LAST=LAST: e.tensor_copy(out=lv, in_=cv[:, :, endpos:endpos + 1].to_broadcast([8, nb // 128, 128])), reads=[CUM], writes=[LAST])
            S.op("dve", lambda e, W=W, LAST=LAST, CUM=CUM: e.tensor_tensor(out=W.ap[:, 0:nb], in0=LAST.ap[:, 0:nb], in1=CUM.ap[:, 0:nb], op=ALU.subtract), reads=[LAST, CUM], writes=[W])
            S.op("act", lambda e, W=W: e.activation(out=W.ap[:, 0:nb], in_=W.ap[:, 0:nb], func=AF.Exp), reads=[W], writes=[W])
            S.op("dve", lambda e, W=W, DT=DT: e.tensor_tensor(out=W.ap[:, 0:nb], in0=W.ap[:, 0:nb], in1=DT.ap[:, 0:nb], op=ALU.mult), reads=[W, DT], writes=[W])
            S.op("act", lambda e, E=E, CUM=CUM: e.activation(out=E.ap[:, 0:nb], in_=CUM.ap[:, 0:nb], func=AF.Exp), reads=[CUM], writes=[E])
            S.op("act", lambda e, DL=DL, LAST=LAST: e.activation(out=DL.ap[:, 0:nb], in_=LAST.ap[:, 0:nb], func=AF.Exp), reads=[LAST], writes=[DL])
            for qi, X in enumerate((CUM, DT, W, E, DL)):
                self.dma("sp" if qi % 2 else "act", self.tabS[d, qi, :, g0:g0 + nb], X.ap[:, 0:nb], reads=[X])


def chunk_order(cfg, d):
    nctx = CTX // 128
    a = list(range(0, nctx)); b = list(range(nctx, cfg.NT))
    return a + b if d == 0 else a[::-1] + b[::-1]


def ssd_main_phase(self, l, V):
    cfg, S = self.cfg, self.S
    NG = cfg.NG
    if not hasattr(self, "ytm"):
        self.ytm = self.dram("ytm", [NG, 512])
    for d in range(2):
        self.phase()
        AFa, ABa = self.AF, self.AB
        tri = AFa.alloc(128, "tri"); self.dma("sp", tri.ap, V["tri"][d], writes=[tri])
        sel = AFa.alloc(8 * 128, "sel", parts=40); self.dma("sp", sel.ap, V["sel40"].rearrange("r h p -> r (h p)"), writes=[sel])
        dsk = AFa.alloc(512, "dsk"); self.dma("sp", dsk.ap, V["ssd_dskip"][l:l + 1, :].to_broadcast([128, 512]), writes=[dsk])
        gn = AFa.alloc(512, "gn"); self.dma("sp", gn.ap, V["ssd_gn"][l:l + 1, :].to_broadcast([128, 512]), writes=[gn])
        H = AFa.alloc(512, "H"); S.op("dve", lambda e: e.memset(H.ap, 0.0), writes=[H])
        Hb = ABa.alloc(512, "Hb"); S.op("dve", lambda e: e.memset(Hb.ap, 0.0), writes=[Hb])
        rXB = Ring(ABa.allocn(2, 6 * 128, "xbc")); rTAB = Ring(AFa.allocn(2, 128, "tab", parts=40)); rTT = Ring(AFa.allocn(2, 40, "TT"))
        rXT = Ring(ABa.allocn(2, 512, "Xtm")); rBT = Ring(ABa.allocn(2, 128, "Btm")); rXW = Ring(ABa.allocn(2, 512, "XW"))
        rDm = Ring(AFa.allocn(3, 128, "Dm")); rMT = Ring(ABa.allocn(3, 128, "MT"))
        rY = Ring(AFa.allocn(2, 512, "Y")); rY2 = Ring(AFa.allocn(2, 512, "Y2")); rZ = Ring(AFa.allocn(2, 512, "Z")); rZs = Ring(AFa.allocn(2, 512, "Zs"))
        rS1 = Ring(AFa.allocn(4, 2, "s1")); rYo = Ring(ABa.allocn(2, 512, "Yo"))
        col = lambda q, h: q * 8 + h
        psq = Ring(self.ps[4:8])
        for c in chunk_order(cfg, d):
            g0 = c * 128
            XB = rXB.next(); TAB = rTAB.next(); TT = rTT.next(); XT = rXT.next(); BT = rBT.next(); XW = rXW.next()
            xv = XB.ap.rearrange("p (c t) -> p c t", c=6)
            self.dma("sp", xv, self.xbcT[:, g0:g0 + 128].rearrange("(c p) t -> p c t", p=128), writes=[XB])
            self.dma("act", TAB.ap, self.tabS[d, :, :, g0:g0 + 128].rearrange("q h t -> (q h) t"), writes=[TAB])
            if "m0" in cfg.dbg: continue
            ps = psq.next()
            S.op("pe", lambda e, ps=ps, TAB=TAB: e.transpose(ps.ap[:, 0:40], TAB.ap, self.ident.ap[0:40, 0:40]), reads=[TAB, self.ident], writes=[ps])
            S.op("act", lambda e, ps=ps, TT=TT: e.copy(out=TT.ap, in_=ps.ap[:, 0:40]), reads=[ps], writes=[TT])
            if "m05" in cfg.dbg: continue
            ps = psq.next()
            pb = ps.ap.bitcast(BF16)
            for j in range(5):
                S.op("pe", lambda e, pb=pb, xv=xv, j=j: e.transpose(pb[:, j * 128:(j + 1) * 128], xv[:, j, :], self.identb.ap), reads=[XB, self.identb], writes=[ps], acc=j > 0)
            S.op("act", lambda e, pb=pb, XT=XT: e.copy(out=XT.ap, in_=pb[:, 0:512]), reads=[ps], writes=[XT])
            S.op("act", lambda e, pb=pb, BT=BT: e.copy(out=BT.ap, in_=pb[:, 512:640]), reads=[ps], writes=[BT])
            if "m1" in cfg.dbg: continue
            xt3 = XT.ap.rearrange("p (h q) -> p h q", h=8)
            S.op("dve", lambda e, XW=XW, xt3=xt3, TT=TT: e.tensor_tensor(out=XW.ap.rearrange("p (h q) -> p h q", h=8), in0=xt3, in1=TT.ap[:, col(2, 0):col(2, 0) + 8].unsqueeze(2).to_broadcast([128, 8, 64]), op=ALU.mult), reads=[XT, TT], writes=[XW])
            if "m15" in cfg.dbg: continue
            psI = self.ps[0]
            S.op("pe", lambda e, psI=psI, xv=xv: e.matmul(psI.ap, lhsT=xv[:, 5, :], rhs=Hb.ap, start=True, stop=True), reads=[XB, Hb], writes=[psI])
            if "m17" in cfg.dbg: continue
            psCs = [self.ps[1], self.ps[2]]
            for g in range(2):
                S.op("pe", lambda e, psC=psCs[g], xv=xv, g=g: e.matmul(psC.ap[:, 0:128], lhsT=xv[g * 64:(g + 1) * 64, 4, :], rhs=xv[g * 64:(g + 1) * 64, 5, :], start=True, stop=True), reads=[XB], writes=[psCs[g]])
            if "m2" in cfg.dbg: continue
            psY = self.ps[3]
            for h in range(8):
                g = h // 4
                psR = psq.next()
                S.op("pe", lambda e, psR=psR, TAB=TAB, h=h: e.matmul(psR.ap[:, 0:128], lhsT=sel.ap[:, h * 128:(h + 1) * 128], rhs=TAB.ap, start=True, stop=True), reads=[sel, TAB], writes=[psR])
                Dm = rDm.next(); MT = rMT.next()
                S.op("dve", lambda e, Dm=Dm, psR=psR, TT=TT, h=h: e.tensor_scalar(out=Dm.ap, in0=psR.ap[:, 0:128], scalar1=TT.ap[:, col(0, h):col(0, h) + 1], scalar2=0.0, op0=ALU.subtract, op1=ALU.min), reads=[psR, TT], writes=[Dm])
                S.op("act", lambda e, Dm=Dm: e.activation(out=Dm.ap, in_=Dm.ap, func=AF.Exp), reads=[Dm], writes=[Dm])
                S.op("dve", lambda e, Dm=Dm, TT=TT, h=h: e.scalar_tensor_tensor(out=Dm.ap, in0=Dm.ap, scalar=TT.ap[:, col(1, h):col(1, h) + 1], in1=tri.ap, op0=ALU.mult, op1=ALU.mult), reads=[Dm, TT, tri], writes=[Dm])
                S.op("dve", lambda e, MT=MT, Dm=Dm, psC=psCs[g]: e.tensor_tensor(out=MT.ap, in0=Dm.ap, in1=psC.ap[:, 0:128], op=ALU.mult), reads=[Dm, psCs[g]], writes=[MT])
                S.op("pe", lambda e, psY=psY, MT=MT, XT=XT, h=h: e.matmul(psY.ap[:, h * 64:(h + 1) * 64], lhsT=MT.ap, rhs=XT.ap[:, h * 64:(h + 1) * 64], start=True, stop=True), reads=[MT, XT], writes=[psY], acc=h > 0)
            if "m3" in cfg.dbg: continue
            psS = psq.next()
            S.op("pe", lambda e, psS=psS, BT=BT, XW=XW: e.matmul(psS.ap, lhsT=BT.ap, rhs=XW.ap, start=True, stop=True), reads=[BT, XW], writes=[psS])
            for g in range(2):
                hb = H.ap[g * 64:(g + 1) * 64, g * 256:(g + 1) * 256]
                S.op("dve", lambda e, hb=hb, TT=TT, g=g: e.tensor_tensor(out=hb.rearrange("p (h q) -> p h q", h=4), in0=hb.rearrange("p (h q) -> p h q", h=4), in1=TT.ap[g * 64:(g + 1) * 64, col(4, 4 * g):col(4, 4 * g) + 4].unsqueeze(2).to_broadcast([64, 4, 64]), op=ALU.mult), reads=[H, TT, psI], writes=[H])
                S.op("dve", lambda e, hb=hb, psS=psS, g=g: e.tensor_tensor(out=hb, in0=hb, in1=psS.ap[g * 64:(g + 1) * 64, g * 256:(g + 1) * 256], op=ALU.add), reads=[H, psS], writes=[H])
            S.op("act", lambda e: e.copy(out=Hb.ap, in_=H.ap), reads=[H, psI], writes=[Hb])
            if "m4" in cfg.dbg: continue
            Y = rY.next(); Y2 = rY2.next()
            S.op("dve", lambda e, Y=Y, psI=psI, TT=TT: e.tensor_tensor(out=Y.ap.rearrange("p (h q) -> p h q", h=8), in0=psI.ap.rearrange("p (h q) -> p h q", h=8), in1=TT.ap[:, col(3, 0):col(3, 0) + 8].unsqueeze(2).to_broadcast([128, 8, 64]), op=ALU.mult), reads=[psI, TT], writes=[Y])
            S.op("dve", lambda e, Y=Y, psY=psY: e.tensor_tensor(out=Y.ap, in0=Y.ap, in1=psY.ap, op=ALU.add), reads=[Y, psY], writes=[Y])
            if d == 0:
                S.op("pool", lambda e, Y2=Y2, XT=XT: e.tensor_tensor(out=Y2.ap, in0=XT.ap, in1=dsk.ap, op=ALU.mult), reads=[XT, dsk], writes=[Y2])
                S.op("pool", lambda e, Y=Y, Y2=Y2: e.tensor_tensor(out=Y.ap, in0=Y.ap, in1=Y2.ap, op=ALU.add), reads=[Y, Y2], writes=[Y])
                self.dma("sp", self.ytm[g0:g0 + 128, :], Y.ap, reads=[Y])
            else:
                self.dma("sp", Y2.ap, self.ytm[g0:g0 + 128, :], writes=[Y2])
                S.op("pool", lambda e, Y=Y, Y2=Y2: e.tensor_tensor(out=Y.ap, in0=Y.ap, in1=Y2.ap, op=ALU.add), reads=[Y, Y2], writes=[Y])
                Z = rZ.next(); Zs = rZs.next()
                zv = Z.ap.rearrange("p (c t) -> p c t", c=4)
                self.dma("act", zv, self.pfm[C_SSD_Z:C_SSD_Z + 512, g0:g0 + 128].rearrange("(c p) t -> p c t", p=128), writes=[Z])
                psZ = psq.next()
                for j in range(4):
                    S.op("pe", lambda e, psZ=psZ, zv=zv, j=j: e.transpose(psZ.ap[:, j * 128:(j + 1) * 128], zv[:, j, :], self.ident.ap), reads=[Z, self.ident], writes=[psZ], acc=j > 0)
                S.op("act", lambda e, Zs=Zs, psZ=psZ: e.activation(out=Zs.ap, in_=psZ.ap, func=AF.Silu), reads=[psZ], writes=[Zs])
                S.op("dve", lambda e, Y=Y, Zs=Zs: e.tensor_tensor(out=Y.ap, in0=Y.ap, in1=Zs.ap, op=ALU.mult), reads=[Y, Zs], writes=[Y])
                s1 = rS1.next()
                S.op("dve", lambda e, Zs=Zs, Y=Y: e.tensor_tensor(out=Zs.ap, in0=Y.ap, in1=Y.ap, op=ALU.mult), reads=[Y], writes=[Zs])
                S.op("dve", lambda e, s1=s1, Zs=Zs: e.reduce_sum(out=s1.ap[:, 0:1], in_=Zs.ap, axis=AX.X), reads=[Zs], writes=[s1])
                S.op("dve", lambda e, s1=s1: e.tensor_scalar(out=s1.ap[:, 0:1], in0=s1.ap[:, 0:1], scalar1=1.0 / 512, scalar2=1e-5, op0=ALU.mult, op1=ALU.add), reads=[s1], writes=[s1])
                S.op("act", lambda e, s1=s1: e.activation(out=s1.ap[:, 0:1], in_=s1.ap[:, 0:1], func=AF.Sqrt), reads=[s1], writes=[s1])
                S.op("dve", lambda e, s1=s1: e.reciprocal(out=s1.ap[:, 1:2], in_=s1.ap[:, 0:1]), reads=[s1], writes=[s1])
                S.op("dve", lambda e, Y=Y, s1=s1: e.scalar_tensor_tensor(out=Y.ap, in0=Y.ap, scalar=s1.ap[:, 1:2], in1=gn.ap, op0=ALU.mult, op1=ALU.mult), reads=[Y, s1, gn], writes=[Y])
                self.store_tm_as_fm(Y, 2, g0, rYo, psq)


def store_tm_as_fm(self, Y, branch, g0, rYo, psq=None):
    S = self.S
    ps = (psq or self.psr).next()
    for j in range(4):
        S.op("pe", lambda e, ps=ps, j=j: e.transpose(ps.ap[:, j * 128:(j + 1) * 128], Y.ap[:, j * 128:(j + 1) * 128], self.ident.ap), reads=[Y, self.ident], writes=[ps], acc=j > 0)
    Yo = rYo.next()
    S.op("act", lambda e: e.copy(out=Yo.ap, in_=ps.ap), reads=[ps], writes=[Yo])
    self.dma("sp", self.yT[branch, :, g0:g0 + 128].rearrange("(c p) t -> p c t", p=128), Yo.ap.rearrange("p (c t) -> p c t", c=4), reads=[Yo])


def ssd_phase(self, l, V):
    if not hasattr(self, "xbcT"):
        self.xbcT = self.dram("xbcT", [768, self.cfg.NG], BF16)
    conv_silu_phase(self, l, C_SSD_X, 6, V["ssd_cw"][l], V["ssd_cb"][l], self.xbcT)
    if "stop1" in self.cfg.dbg: return
    ssd_tab_phase(self, l, V)
    if "stop2" in self.cfg.dbg: return
    ssd_main_phase(self, l, V)


K.ssd_phase = ssd_phase
K.store_tm_as_fm = store_tm_as_fm


def gemm_fm(self, ranges, src, kc, tok_blocks, sink, wring, xring, wcols, tbs=512, dyn=False):
    S = self.S
    wt = wring.next()
    wv = wt.ap[:, 0:kc * wcols].rearrange("p (k n) -> p k n", k=kc)
    off = 0
    offs = []
    for (W, c0, n) in ranges:
        self.dma("pool", wv[:, :, off:off + n], W[:, c0:c0 + n].rearrange("(k p) n -> p k n", p=128), writes=[wt], reads=[wt] if off else [])
        offs.append(off)
        off += n
    for (g0, nb) in tok_blocks:
        xb = xring.next()
        xv = xb.ap[:, 0:kc * tbs].rearrange("p (k t) -> p k t", k=kc)
        if dyn:
            base = self.dyn_base
            S.op("sp", lambda h, xv=xv, nb=nb, g0=g0, base=base: h.dma_start(out=xv[:, :, 0:nb], in_=src[:, 0:kc, base:base + self.cfg.SEQ][:, :, bass.ds(self.sval, self.cfg.TL)][:, :, g0:g0 + nb]), writes=[xb], dma=True, late=True)
        else:
            self.dma("sp", xv[:, :, 0:nb], src[:, 0:kc, g0:g0 + nb], writes=[xb])
        for ci, (W, c0, n) in enumerate(ranges):
            ps = self.psr.next()
            o = offs[ci]
            for k in range(kc):
                S.op("pe", lambda e: e.matmul(ps.ap[0:n, 0:nb], lhsT=wv[:, k, o:o + n], rhs=xv[:, k, 0:nb], start=(k == 0), stop=(k == kc - 1)),
                     reads=[wt, xb], writes=[ps], acc=k > 0)
            sink(ci, g0, nb, ps)


def merge_phase(self, l, W_in_l, wb, wo):
    cfg, S = self.cfg, self.S
    NG = self.TN
    dyn = self.dyn_base is not None
    self.PB = [self.tbuf(f"PB{n}", [D, NG]) for n in range(4)]
    self.mT = self.tbuf("mT", [128, KC, NG], BF16)
    self.yfm = self.tbuf("yfm", [D, NG])
    tb = blocks(0, NG, 512)
    self.phase()
    wring = Ring(self.AB.allocn(2, 4 * 512, "wt")); xring = Ring(self.AB.allocn(2, 4 * 512, "xb")); oring = Ring(self.AF.allocn(4, 512, "ost"))
    for n in range(4):
        src = self.yT[n].rearrange("(k p) t -> p k t", p=128)
        for c0 in range(0, D, 512):
            rngs = [(wb[n], c0 + j * 128, 128) for j in range(4)]

            def sink(ci, g0, nb, ps, n=n, c0=c0):
                ot = oring.next()
                S.op("act" if ci % 2 else "dve", (lambda e: e.copy(out=ot.ap[:, 0:nb], in_=ps.ap[:, 0:nb])) if ci % 2 else (lambda e: e.tensor_copy(out=ot.ap[:, 0:nb], in_=ps.ap[:, 0:nb])), reads=[ps], writes=[ot])
                self.dma("act", self.PB[n][c0 + ci * 128:c0 + (ci + 1) * 128, g0:g0 + nb], ot.ap[:, 0:nb], reads=[ot])
            gemm_fm(self, rngs, src, 4, tb, sink, wring, xring, 512, dyn=dyn)
    self.phase()
    wring = Ring(self.AB.allocn(2, KC * 512, "wt")); xring = Ring(self.AB.allocn(3, KC * 512, "xb"))
    sgr = Ring(self.AF.allocn(2, 512, "sg")); pbr = Ring(self.AF.allocn(2, 512, "pb")); accr = Ring(self.AF.allocn(2, 512, "acc")); mor = Ring(self.AB.allocn(2, 512, "mo"))
    for m in range(KC):
        rngs = [(W_in_l, C_GATE + n * D + m * 128, 128) for n in range(4)]
        st = {}

        def sink(ci, g0, nb, ps, m=m, st=st):
            sg = sgr.next(); pb = pbr.next()
            self.dma("act", pb.ap[:, 0:nb], self.PB[ci][m * 128:(m + 1) * 128, g0:g0 + nb], writes=[pb])
            S.op("act", lambda e: e.activation(out=sg.ap[:, 0:nb], in_=ps.ap[:, 0:nb], func=AF.Sigmoid), reads=[ps], writes=[sg])
            if ci == 0:
                st["acc"] = accr.next()
                acc = st["acc"]
                S.op("dve", lambda e: e.tensor_tensor(out=acc.ap[:, 0:nb], in0=sg.ap[:, 0:nb], in1=pb.ap[:, 0:nb], op=ALU.mult), reads=[sg, pb], writes=[acc])
            else:
                acc = st["acc"]
                S.op("dve", lambda e: e.tensor_tensor(out=sg.ap[:, 0:nb], in0=sg.ap[:, 0:nb], in1=pb.ap[:, 0:nb], op=ALU.mult), reads=[sg, pb], writes=[sg])
                S.op("pool", lambda e: e.tensor_tensor(out=acc.ap[:, 0:nb], in0=acc.ap[:, 0:nb], in1=sg.ap[:, 0:nb], op=ALU.add), reads=[acc, sg], writes=[acc])
            if ci == 3:
                mo = mor.next()
                S.op("act", lambda e: e.copy(out=mo.ap[:, 0:nb], in_=acc.ap[:, 0:nb]), reads=[acc], writes=[mo])
                self.dma("sp", self.mT[:, m, g0:g0 + nb], mo.ap[:, 0:nb], reads=[mo])
        gemm_fm(self, rngs, self.xmT, KC, tb, sink, wring, xring, 512, dyn=dyn)
    self.gemm_to_yfm(wo, self.mT, KC)


def gemm_to_yfm(self, W, src, kc, accumulate=False):
    cfg, S = self.cfg, self.S
    tb = blocks(0, self.TN, 256)
    self.phase()
    deep = kc <= 22
    wring = Ring(self.AB.allocn(2 if deep else 1, kc * 512, "wt")); xring = Ring(self.AB.allocn(3 if deep else 2, kc * 256, "xb")); oring = Ring(self.AF.allocn(4, 512, "ost"))
    for c0 in range(0, D, 512):
        rngs = [(W, c0 + j * 128, 128) for j in range(4)]

        def sink(ci, g0, nb, ps, c0=c0):
            ot = oring.next()
            if accumulate:
                self.dma("sp", ot.ap[:, 0:nb], self.yfm[c0 + ci * 128:c0 + (ci + 1) * 128, g0:g0 + nb], writes=[ot])
                S.op("dve", lambda e: e.tensor_tensor(out=ot.ap[:, 0:nb], in0=ot.ap[:, 0:nb], in1=ps.ap[:, 0:nb], op=ALU.add), reads=[ps, ot], writes=[ot])
            else:
                S.op("act" if ci % 2 else "dve", (lambda e: e.copy(out=ot.ap[:, 0:nb], in_=ps.ap[:, 0:nb])) if ci % 2 else (lambda e: e.tensor_copy(out=ot.ap[:, 0:nb], in_=ps.ap[:, 0:nb])), reads=[ps], writes=[ot])
            self.dma("act", self.yfm[c0 + ci * 128:c0 + (ci + 1) * 128, g0:g0 + nb], ot.ap[:, 0:nb], reads=[ot])
        gemm_fm(self, rngs, src, kc, tb, sink, wring, xring, 512, tbs=256)


def epi_phase(self, l, x_src, ctx_src, gate_i, ln_i, dst, lng, lnb, out_ext=None, x_dyn=False):
    cfg, S = self.cfg, self.S
    alpha = float((2.0 * 2) ** 0.25)
    self.phase()
    gm = {}
    for kind in range(2):
        t = self.AF.alloc(D, f"gm{kind}")
        self.dma("sp", t.ap, self.modD[l, kind:kind + 1, gate_i * D:(gate_i + 1) * D].to_broadcast([128, D]), writes=[t])
        gm[kind] = t
    G = self.AF.alloc(D, "lng"); self.dma("sp", G.ap, lng[l, ln_i:ln_i + 1, :].to_broadcast([128, D]), writes=[G])
    Bt = self.AF.alloc(D, "lnb"); self.dma("sp", Bt.ap, lnb[l, ln_i:ln_i + 1, :].to_broadcast([128, D]), writes=[Bt])
    xr = Ring(self.AF.allocn(2, D, "x")); yr = Ring(self.AF.allocn(2, D, "y")); rr = Ring(self.AF.allocn(2, D, "r")); sr = Ring(self.AF.allocn(4, 4, "s"))
    tctx = self.t_ctx
    for ti in range(self.TN // 128):
        g0 = ti * 128
        kind = 1 if g0 < tctx else 0
        X = xr.next(); Y = yr.next(); R = rr.next(); s = sr.next()
        if x_dyn:
            S.op("sp", lambda h, X=X, g0=g0: h.dma_start(out=X.ap, in_=x_src[bass.ds(self.sval, self.cfg.TL), :][g0:g0 + 128, :]), writes=[X], dma=True, late=True)
        else:
            src = ctx_src[g0:g0 + 128, :] if kind == 1 else x_src[g0 - tctx:g0 + 128 - tctx, :]
            self.dma("sp", X.ap, src, writes=[X])
        yv = Y.ap.rearrange("p (c t) -> p c t", c=KC)
        self.dma("act", yv, self.yfm[:, g0:g0 + 128].rearrange("(c p) t -> p c t", p=128), writes=[Y])
        for g in range(4):
            ps = self.psr.next()
            for j in range(4):
                S.op("pe", lambda e: e.transpose(ps.ap[:, j * 128:(j + 1) * 128], yv[:, g * 4 + j, :], self.ident.ap), reads=[Y, self.ident], writes=[ps], acc=j > 0)
            S.op("dve", lambda e: e.tensor_tensor(out=R.ap[:, g * 512:(g + 1) * 512], in0=ps.ap, in1=gm[kind].ap[:, g * 512:(g + 1) * 512], op=ALU.mult), reads=[ps, gm[kind]], writes=[R], acc=True)
        S.op("dve", lambda e: e.scalar_tensor_tensor(out=R.ap, in0=X.ap, scalar=alpha, in1=R.ap, op0=ALU.mult, op1=ALU.add), reads=[X, R], writes=[R])
        S.op("dve", lambda e: e.reduce_sum(out=s.ap[:, 0:1], in_=R.ap, axis=AX.X), reads=[R], writes=[s])
        S.op("dve", lambda e: e.tensor_scalar_mul(out=s.ap[:, 0:1], in0=s.ap[:, 0:1], scalar1=1.0 / D), reads=[s], writes=[s])
        S.op("dve", lambda e: e.tensor_scalar_sub(out=R.ap, in0=R.ap, scalar1=s.ap[:, 0:1]), reads=[R, s], writes=[R])
        S.op("pool", lambda e: e.tensor_tensor(out=X.ap, in0=R.ap, in1=R.ap, op=ALU.mult), reads=[R], writes=[X])
        S.op("dve", lambda e: e.reduce_sum(out=s.ap[:, 1:2], in_=X.ap, axis=AX.X), reads=[X], writes=[s])
        S.op("dve", lambda e: e.tensor_scalar(out=s.ap[:, 1:2], in0=s.ap[:, 1:2], scalar1=1.0 / D, scalar2=1e-5, op0=ALU.mult, op1=ALU.add), reads=[s], writes=[s])
        S.op("act", lambda e: e.activation(out=s.ap[:, 2:3], in_=s.ap[:, 1:2], func=AF.Sqrt), reads=[s], writes=[s])
        S.op("dve", lambda e: e.reciprocal(out=s.ap[:, 3:4], in_=s.ap[:, 2:3]), reads=[s], writes=[s])
        S.op("dve", lambda e: e.scalar_tensor_tensor(out=R.ap, in0=R.ap, scalar=s.ap[:, 3:4], in1=G.ap, op0=ALU.mult, op1=ALU.mult), reads=[R, s, G], writes=[R])
        S.op("pool", lambda e: e.tensor_tensor(out=R.ap, in0=R.ap, in1=Bt.ap, op=ALU.add), reads=[R, Bt], writes=[R])
        self.dma("sp", dst[g0:g0 + 128, :], R.ap, reads=[R])
        if out_ext is not None and kind == 0:
            self.dma("act", out_ext[g0 - tctx:g0 - tctx + 128, :], R.ap, reads=[R])


def ffn_dense_phase(self, l, wg, wu, wd, FD, comb_row=None, accumulate=False):
    cfg, S = self.cfg, self.S
    nf = FD // 128
    self.UT = self.tbuf("UT", [128, max(cfg.FD, cfg.FE) // 128, self.TN], BF16)
    self.phase()
    tb = blocks(0, self.TN, 512)
    wring = Ring(self.AB.allocn(2, KC * 512, "wt")); xring = Ring(self.AB.allocn(3, KC * 512, "xb"))
    sgr = Ring(self.AF.allocn(2, 512, "sg")); uor = Ring(self.AB.allocn(2, 512, "uo"))
    cbr = Ring(self.AF.allocn(2, 512, "cb")) if comb_row is not None else None
    for f0 in range(0, nf, 2):
        fcs = [f0, f0 + 1] if f0 + 1 < nf else [f0]
        rngs = []
        for fc in fcs:
            rngs += [(wg, fc * 128, 128), (wu, fc * 128, 128)]
        st = {}

        def sink(ci, g0, nb, ps, fcs=fcs, st=st):
            if ci == 0 and comb_row is not None:
                st["cb"] = cbr.next()
                self.dma("act", st["cb"].ap[:, 0:nb], comb_row[:, g0:g0 + nb].to_broadcast([128, nb]), writes=[st["cb"]])
            if ci % 2 == 0:
                st["sg"] = sgr.next()
                sg = st["sg"]
                S.op("act", lambda e: e.activation(out=sg.ap[:, 0:nb], in_=ps.ap[:, 0:nb], func=AF.Silu), reads=[ps], writes=[sg])
                if comb_row is not None:
                    cb = st["cb"]
                    S.op("pool", lambda e: e.tensor_tensor(out=sg.ap[:, 0:nb], in0=sg.ap[:, 0:nb], in1=cb.ap[:, 0:nb], op=ALU.mult), reads=[sg, cb], writes=[sg])
            else:
                sg = st["sg"]; uo = uor.next()
                S.op("dve", lambda e: e.tensor_tensor(out=uo.ap[:, 0:nb], in0=sg.ap[:, 0:nb], in1=ps.ap[:, 0:nb], op=ALU.mult), reads=[sg, ps], writes=[uo])
                self.dma("act", self.UT[:, fcs[ci // 2], g0:g0 + nb], uo.ap[:, 0:nb], reads=[uo])
        gemm_fm(self, rngs, self.xmT2, KC, tb, sink, wring, xring, 512)
    self.gemm_to_yfm(wd, self.UT, nf, accumulate=accumulate)


K.merge_phase = merge_phase
K.gemm_to_yfm = gemm_to_yfm
K.epi_phase = epi_phase
K.ffn_dense_phase = ffn_dense_phase


def zero_fill_phase(self, targets):
    S = self.S
    self.phase()
    zf = self.AF.alloc(2048, "zf"); S.op("dve", lambda e: e.memset(zf.ap, 0.0), writes=[zf])
    zb = self.AB.alloc(2048, "zb"); S.op("dve", lambda e: e.memset(zb.ap, 0.0), writes=[zb])
    i = 0
    for t in targets:
        z = zb if t.dtype == BF16 else zf
        R, Cn = t.shape
        for r0 in range(0, R, 128):
            for (c0, n) in blocks(0, Cn, 2048):
                self.dma("sp" if i % 2 == 0 else "act", t[r0:r0 + 128, c0:c0 + n], z.ap[:, 0:n], reads=[z])
                i += 1


def moe_phase(self, l, V):
    j = l // 2
    for e in range(8):
        ffn_dense_phase(self, l, V["moe_w_gate"][j, e], V["moe_w_up"][j, e], V["moe_w_down"][j, e], self.cfg.FE, comb_row=self.combT[e:e + 1, :], accumulate=(e > 0))


K.zero_fill_phase = zero_fill_phase
K.moe_phase = moe_phase


def mlstm_tab_phase(self, l, V):
    cfg, S = self.cfg, self.S
    NG = cfg.NG
    NB = 1024
    if not hasattr(self, "tabM"):
        self.tabM = self.dram("tabM", [2, 6, 4, NG])
    self.phase()
    pr = self.AF.alloc(4, "mlp", parts=4)
    self.dma("sp", pr.ap, V["ml_hp"][l], writes=[pr])
    ones = self.AF.alloc(NB, "ones", parts=4)
    S.op("dve", lambda e: e.memset(ones.ap, 1.0), writes=[ones])
    zero1 = self.AF.alloc(1, "z1", parts=4)
    S.op("dve", lambda e: e.memset(zero1.ap, 0.0), writes=[zero1])
    names = ("I", "F", "T", "BC", "G", "GE", "GP", "KS", "WIN", "E3", "CAR")
    rr = {n: Ring(self.AF.allocn(2, NB, n, parts=4)) for n in names}
    cring = Ring(self.AF.allocn(6, 2, "car", parts=4))
    for d in range(2):
        carry = None
        segs = _segments(cfg)
        blks = []
        for (s0, s1) in segs:
            b = blocks(s0, s1, NB)
            blks += b if d == 0 else b[::-1]
        for (g0, nb) in blks:
            I, F, T, BC, G, GE, GP, KS, WIN, E3, CAR = [rr[n].next() for n in names]
            v = lambda X: X.ap[:, 0:nb]
            f = (lambda a: a) if d == 0 else (lambda a: rev_ap(a, nb))
            self.dma("sp", v(I), self.pfm[C_ML_G + d * 8:C_ML_G + d * 8 + 4, g0:g0 + nb], writes=[I])
            self.dma("act", v(F), self.pfm[C_ML_G + d * 8 + 4:C_ML_G + d * 8 + 8, g0:g0 + nb], writes=[F])
            S.op("dve", lambda e: e.tensor_scalar_add(out=v(I), in0=v(I), scalar1=pr.ap[:, d:d + 1]), reads=[I, pr], writes=[I])
            S.op("dve", lambda e: e.tensor_scalar(out=v(F), in0=v(F), scalar1=pr.ap[:, 2 + d:3 + d], scalar2=-1.0, op0=ALU.add, op1=ALU.mult), reads=[F, pr], writes=[F])
            softplus_ops(self, F, T, nb, 4)
            cb_ap = zero1.ap[:, 0:1] if carry is None else carry.ap[:, 0:1]
            cg_ap = zero1.ap[:, 0:1] if carry is None else carry.ap[:, 1:2]
            cdeps = [zero1] if carry is None else [carry]
            S.op("dve", lambda e: e.tensor_tensor_scan(out=f(v(BC)), data0=f(v(ones)), data1=f(v(F)), initial=cb_ap, op0=ALU.mult, op1=ALU.subtract), reads=[F, ones] + cdeps, writes=[BC])
            S.op("dve", lambda e: e.tensor_tensor(out=v(I), in0=v(I), in1=v(BC), op=ALU.subtract), reads=[I, BC], writes=[I])
            S.op("dve", lambda e: e.tensor_tensor_scan(out=f(v(G)), data0=f(v(ones)), data1=f(v(I)), initial=cg_ap, op0=ALU.mult, op1=ALU.max), reads=[I, ones] + cdeps, writes=[G])
            endpos = 127 if d == 0 else 0
            nch = nb // 128
            gv = v(G).rearrange("p (c t) -> p c t", t=128)
            S.op("dve", lambda e: e.tensor_copy(out=v(GE).rearrange("p (c t) -> p c t", t=128), in_=gv[:, :, endpos:endpos + 1].to_broadcast([4, nch, 128])), reads=[G], writes=[GE])
            if d == 0:
                if nch > 1:
                    S.op("dve", lambda e: e.tensor_copy(out=GP.ap[:, 128:nb], in_=GE.ap[:, 0:nb - 128]), reads=[GE], writes=[GP])
                S.op("dve", lambda e: e.tensor_copy(out=GP.ap[:, 0:128], in_=cg_ap.to_broadcast([4, 128])), reads=cdeps, writes=[GP], acc=True)
            else:
                if nch > 1:
                    S.op("dve", lambda e: e.tensor_copy(out=GP.ap[:, 0:nb - 128], in_=GE.ap[:, 128:nb]), reads=[GE], writes=[GP])
                S.op("dve", lambda e: e.tensor_copy(out=GP.ap[:, nb - 128:nb], in_=cg_ap.to_broadcast([4, 128])), reads=cdeps, writes=[GP], acc=True)
            S.op("dve", lambda e: e.tensor_tensor(out=v(KS), in0=v(I), in1=v(GE), op=ALU.subtract), reads=[I, GE], writes=[KS])
            S.op("act", lambda e: e.activation(out=v(KS), in_=v(KS), func=AF.Exp), reads=[KS], writes=[KS])
            S.op("dve", lambda e: e.tensor_tensor(out=v(WIN), in0=v(GP), in1=v(G), op=ALU.subtract), reads=[GP, G], writes=[WIN])
            S.op("act", lambda e: e.activation(out=v(WIN), in_=v(WIN), func=AF.Exp), reads=[WIN], writes=[WIN])
            S.op("dve", lambda e: e.tensor_tensor(out=v(E3), in0=v(BC), in1=v(G), op=ALU.add), reads=[BC, G], writes=[E3])
            S.op("act", lambda e: e.activation(out=v(E3), in_=v(E3), func=AF.Exp, scale=-1.0), reads=[E3], writes=[E3])
            S.op("dve", lambda e: e.tensor_tensor(out=v(CAR), in0=v(GP), in1=v(GE), op=ALU.subtract), reads=[GP, GE], writes=[CAR])
            S.op("act", lambda e: e.activation(out=v(CAR), in_=v(CAR), func=AF.Exp), reads=[CAR], writes=[CAR])
            ncar = cring.next()
            lastpos = nb - 1 if d == 0 else 0
            S.op("dve", lambda e: e.tensor_copy(out=ncar.ap[:, 0:1], in_=BC.ap[:, lastpos:lastpos + 1]), reads=[BC], writes=[ncar])
            S.op("dve", lambda e: e.tensor_copy(out=ncar.ap[:, 1:2], in_=G.ap[:, lastpos:lastpos + 1]), reads=[G], writes=[ncar], acc=True)
            carry = ncar
            for qi, X in enumerate((I, KS, WIN, E3, CAR, G)):
                self.dma("sp" if qi % 2 else "act", self.tabM[d, qi, :, g0:g0 + nb], v(X), reads=[X])


def mlstm_main_phase(self, l, V):
    cfg, S = self.cfg, self.S
    NG = cfg.NG
    if not hasattr(self, "htm"):
        self.htm = self.dram("htm", [NG, 512])
    for d in range(2):
        self.phase()
        AFa, ABa = self.AF, self.AB
        tri = AFa.alloc(128, "tri"); self.dma("sp", tri.ap, V["tri"][d], writes=[tri])
        sel = AFa.alloc(4 * 128, "sel", parts=24); self.dma("sp", sel.ap, V["sel24"].rearrange("r h p -> r (h p)"), writes=[sel])
        Cst = AFa.alloc(4 * 129, "C"); S.op("dve", lambda e: e.memset(Cst.ap, 0.0), writes=[Cst])
        Cb = Cst
        rQK = Ring(AFa.allocn(2, 8 * 128, "qk")); rVF = Ring(AFa.allocn(2, 512, "vf")); rTAB = Ring(AFa.allocn(2, 128, "tabm", parts=24)); rTT = Ring(AFa.allocn(2, 24, "TTm"))
        vas = AFa.allocn(2, 4 * 129, "VA")
        for va in vas:
            S.op("dve", lambda e, va=va: e.memset(va.ap, 1.0), writes=[va])
        rVA = Ring(vas); rKS = Ring(AFa.allocn(2, 512, "KSt"))
        rDm = Ring(AFa.allocn(3, 128, "Dm")); rST = Ring(AFa.allocn(3, 128, "ST")); rN = Ring(AFa.allocn(3, 132, "N")); rs = Ring(AFa.allocn(4, 2, "s"))
        rHS = Ring(AFa.allocn(2, 512, "HS")); rH2 = Ring(AFa.allocn(2, 512, "H2")); rO = Ring(AFa.allocn(2, 512, "Of")); rOs = Ring(AFa.allocn(2, 512, "Os")); rYo = Ring(ABa.allocn(2, 512, "Yo"))
        psq = self.psr
        col = lambda q, h: q * 4 + h
        for c in chunk_order(cfg, d):
            g0 = c * 128
            QK = rQK.next(); VF = rVF.next(); TAB = rTAB.next(); TT = rTT.next(); VA = rVA.next(); KS = rKS.next(); HS = rHS.next()
            qv = QK.ap.rearrange("p (c t) -> p c t", c=8)
            self.dma("sp", qv, self.qkT[:, g0:g0 + 128].rearrange("(c p) t -> p c t", p=128), writes=[QK])
            vv = VF.ap.rearrange("p (c t) -> p c t", c=4)
            self.dma("act", vv, self.pfm[C_ML_V:C_ML_V + 512, g0:g0 + 128].rearrange("(c p) t -> p c t", p=128), writes=[VF])
            self.dma("act", TAB.ap, self.tabM[d, :, :, g0:g0 + 128].rearrange("q h t -> (q h) t"), writes=[TAB])
            ps = psq.next()
            S.op("pe", lambda e: e.transpose(ps.ap[:, 0:24], TAB.ap, self.ident.ap[0:24, 0:24]), reads=[TAB, self.ident], writes=[ps])
            S.op("act", lambda e: e.copy(out=TT.ap, in_=ps.ap[:, 0:24]), reads=[ps], writes=[TT])
            ps = psq.next()
            for j in range(4):
                S.op("pe", lambda e: e.transpose(ps.ap[:, j * 128:(j + 1) * 128], vv[:, j, :], self.ident.ap), reads=[VF, self.ident], writes=[ps], acc=j > 0)
            va3 = VA.ap.rearrange("p (h q) -> p h q", h=4)
            S.op("act", lambda e: e.copy(out=va3[:, :, 0:128], in_=ps.ap.rearrange("p (h q) -> p h q", h=4)), reads=[ps], writes=[VA], acc=True)
            ps = psq.next()
            pb = ps.ap
            for j in range(4):
                S.op("pe", lambda e: e.transpose(pb[:, j * 128:(j + 1) * 128], qv[:, 4 + j, :], self.ident.ap), reads=[QK, self.ident], writes=[ps], acc=j > 0)
            for h in range(4):
                S.op("act", lambda e: e.activation(out=KS.ap[:, h * 128:(h + 1) * 128], in_=pb[:, h * 128:(h + 1) * 128], func=AF.Copy, scale=TT.ap[:, col(1, h):col(1, h) + 1]), reads=[ps, TT], writes=[KS], acc=h > 0)
            for h in range(4):
                psS = psq.next()
                S.op("pe", lambda e: e.matmul(psS.ap[:, 0:128], lhsT=qv[:, 4 + h, :], rhs=qv[:, h, :], start=True, stop=True), reads=[QK], writes=[psS])
                psR = psq.next()
                S.op("pe", lambda e: e.matmul(psR.ap[:, 0:128], lhsT=sel.ap[:, h * 128:(h + 1) * 128], rhs=TAB.ap, start=True, stop=True), reads=[sel, TAB], writes=[psR])
                Dm = rDm.next(); ST = rST.next(); N = rN.next(); s = rs.next()
                S.op("dve", lambda e: e.tensor_scalar(out=Dm.ap, in0=psR.ap[:, 0:128], scalar1=TT.ap[:, col(0, h):col(0, h) + 1], scalar2=0.0, op0=ALU.subtract, op1=ALU.max), reads=[psR, TT], writes=[Dm])
                S.op("act", lambda e: e.activation(out=Dm.ap, in_=Dm.ap, func=AF.Exp, scale=-1.0), reads=[Dm], writes=[Dm])
                S.op("pool", lambda e: e.tensor_tensor(out=Dm.ap, in0=Dm.ap, in1=tri.ap, op=ALU.mult), reads=[Dm, tri], writes=[Dm])
                S.op("dve", lambda e: e.tensor_tensor(out=ST.ap, in0=Dm.ap, in1=psS.ap[:, 0:128], op=ALU.mult), reads=[Dm, psS], writes=[ST])
                psA = psq.next()
                S.op("pe", lambda e: e.matmul(psA.ap[:, 0:129], lhsT=ST.ap, rhs=VA.ap[:, h * 129:(h + 1) * 129], start=True, stop=True), reads=[ST, VA], writes=[psA])
                psB = psq.next()
                S.op("pe", lambda e: e.matmul(psB.ap[:, 0:129], lhsT=qv[:, h, :], rhs=Cb.ap[:, h * 129:(h + 1) * 129], start=True, stop=True), reads=[QK, Cb], writes=[psB])
                S.op("dve", lambda e: e.tensor_scalar_mul(out=N.ap[:, 0:129], in0=psB.ap[:, 0:129], scalar1=TT.ap[:, col(2, h):col(2, h) + 1]), reads=[psB, TT], writes=[N])
                S.op("dve", lambda e: e.tensor_tensor(out=N.ap[:, 0:129], in0=N.ap[:, 0:129], in1=psA.ap[:, 0:129], op=ALU.add), reads=[N, psA], writes=[N])
                S.op("act", lambda e: e.activation(out=s.ap[:, 0:1], in_=N.ap[:, 128:129], func=AF.Abs), reads=[N], writes=[s])
                S.op("dve", lambda e: e.tensor_tensor(out=s.ap[:, 0:1], in0=s.ap[:, 0:1], in1=TT.ap[:, col(3, h):col(3, h) + 1], op=ALU.max), reads=[s, TT], writes=[s])
                S.op("dve", lambda e: e.reciprocal(out=s.ap[:, 1:2], in_=s.ap[:, 0:1]), reads=[s], writes=[s])
                S.op("dve", lambda e: e.tensor_scalar_mul(out=HS.ap[:, h * 128:(h + 1) * 128], in0=N.ap[:, 0:128], scalar1=s.ap[:, 1:2]), reads=[N, s], writes=[HS], acc=h > 0)
                psU = psq.next()
                S.op("pe", lambda e: e.matmul(psU.ap[:, 0:129], lhsT=KS.ap[:, h * 128:(h + 1) * 128], rhs=VA.ap[:, h * 129:(h + 1) * 129], start=True, stop=True), reads=[KS, VA], writes=[psU])
                cs = Cst.ap[:, h * 129:(h + 1) * 129]
                S.op("dve", lambda e: e.scalar_tensor_tensor(out=cs, in0=cs, scalar=TT.ap[:, col(4, h):col(4, h) + 1], in1=psU.ap[:, 0:129], op0=ALU.mult, op1=ALU.add), reads=[Cst, TT, psU, psB], writes=[Cst])
            if d == 0:
                self.dma("sp", self.htm[g0:g0 + 128, :], HS.ap, reads=[HS])
            else:
                H2 = rH2.next(); Of = rO.next(); Os = rOs.next()
                self.dma("sp", H2.ap, self.htm[g0:g0 + 128, :], writes=[H2])
                S.op("pool", lambda e: e.tensor_tensor(out=HS.ap, in0=HS.ap, in1=H2.ap, op=ALU.add), reads=[HS, H2], writes=[HS])
                ov = Of.ap.rearrange("p (c t) -> p c t", c=4)
                self.dma("act", ov, self.pfm[C_ML_O:C_ML_O + 512, g0:g0 + 128].rearrange("(c p) t -> p c t", p=128), writes=[Of])
                psZ = psq.next()
                for j in range(4):
                    S.op("pe", lambda e: e.transpose(psZ.ap[:, j * 128:(j + 1) * 128], ov[:, j, :], self.ident.ap), reads=[Of, self.ident], writes=[psZ], acc=j > 0)
                S.op("act", lambda e: e.activation(out=Os.ap, in_=psZ.ap, func=AF.Sigmoid), reads=[psZ], writes=[Os])
                S.op("dve", lambda e: e.tensor_tensor(out=HS.ap, in0=HS.ap, in1=Os.ap, op=ALU.mult), reads=[HS, Os], writes=[HS])
                self.store_tm_as_fm(HS, 3, g0, rYo)


def mlstm_phase(self, l, V):
    cfg, S = self.cfg, self.S
    if not hasattr(self, "qkT"):
        self.qkT = self.dram("qkT", [1024, cfg.NG])
    st = {}

    def post(c, s0, g0, nb, SG, O, ctxp):
        scale = (128.0 ** -0.5) if c < 4 else 1.0
        if "init" not in ctxp:
            ctxp["init"] = True
            ctxp["P"] = self.AB.alloc(128, "ropeP")
            pf = self.AF.alloc(128, "ropePf")
            self.dma("sp", pf.ap, V["ropeP"], writes=[pf])
            S.op("dve", lambda e: e.tensor_copy(out=ctxp["P"].ap, in_=pf.ap), reads=[pf], writes=[ctxp["P"]])
            ctxp["sgb"] = Ring(self.AB.allocn(2, 1024, "sgb"))
            ctxp["cos"] = Ring(self.AF.allocn(2, 1024, "cos")); ctxp["sin"] = Ring(self.AF.allocn(2, 1024, "sin")); ctxp["t1"] = Ring(self.AF.allocn(2, 1024, "t1"))
        if s0 == 0:
            S.op("act", lambda e: e.activation(out=O.ap[:, 0:nb], in_=SG.ap[:, 0:nb], func=AF.Copy, scale=scale), reads=[SG], writes=[O])
            return
        P = ctxp["P"]; sgb = ctxp["sgb"].next(); cs = ctxp["cos"].next(); sn = ctxp["sin"].next(); t1 = ctxp["t1"].next()
        t0 = g0 - CTX
        self.dma("sp", cs.ap[:, 0:nb], V["rope_cos"][:, t0:t0 + nb], writes=[cs])
        self.dma("sp", sn.ap[:, 0:nb], V["rope_sin"][:, t0:t0 + nb], writes=[sn])
        S.op("act", lambda e: e.copy(out=sgb.ap[:, 0:nb], in_=SG.ap[:, 0:nb]), reads=[SG], writes=[sgb])
        S.op("pool", lambda e: e.tensor_tensor(out=t1.ap[:, 0:nb], in0=SG.ap[:, 0:nb], in1=cs.ap[:, 0:nb], op=ALU.mult), reads=[SG, cs], writes=[t1])
        for (o, n) in blocks(0, nb, 512):
            ps = self.psr.next()
            S.op("pe", lambda e: e.matmul(ps.ap[:, 0:n], lhsT=P.ap, rhs=sgb.ap[:, o:o + n], start=True, stop=True), reads=[P, sgb], writes=[ps])
            S.op("dve", lambda e: e.tensor_tensor(out=sn.ap[:, o:o + n], in0=ps.ap[:, 0:n], in1=sn.ap[:, o:o + n], op=ALU.mult), reads=[ps, sn], writes=[sn], acc=True)
        S.op("dve", lambda e: e.tensor_tensor(out=t1.ap[:, 0:nb], in0=t1.ap[:, 0:nb], in1=sn.ap[:, 0:nb], op=ALU.add), reads=[t1, sn], writes=[t1])
        S.op("act", lambda e: e.activation(out=O.ap[:, 0:nb], in_=t1.ap[:, 0:nb], func=AF.Copy, scale=scale), reads=[t1], writes=[O])
    conv_silu_phase(self, l, C_ML_Q, 8, V["ml_cw"][l], V["ml_cb"][l], self.qkT, post=post, out_f32=True)
    mlstm_tab_phase(self, l, V)
    mlstm_main_phase(self, l, V)


K.mlstm_phase = mlstm_phase


def na_rowinfo(ROWS):
    info = []
    classes = {}
    for r in range(ROWS):
        ws = min(max(r - 4, 0), ROWS - 8)
        as_ = min(2 * (ws // 2), ROWS - 10)
        key = (ws - as_, r - ws)
        if key not in classes:
            classes[key] = len(classes)
        info.append((ws, as_, classes[key]))
    return info, classes


def na_phase(self, l, V):
    cfg, S = self.cfg, self.S
    NG, NT, ROWS = cfg.NG, cfg.NT, cfg.ROWS
    info, classes = na_rowinfo(ROWS)
    ncls = len(classes)
    if not hasattr(self, "naq"):
        self.naq = self.dram("naq", [512, NG], BF16)
        self.nak = self.dram("nak", [512, NG], BF16)
        self.nav = self.dram("nav", [NG, 8 * 65], BF16)
        self.natm = self.dram("natm", [NG, 512])
    self.phase()
    NB = 2048
    rin = Ring(self.AF.allocn(2, NB, "nin")); rout = Ring(self.AB.allocn(2, NB, "nout"))
    for (row0, dst, sc) in ((C_NA_Q, self.naq, 0.125), (C_NA_K, self.nak, 1.0)):
        for c in range(4):
            for (g0, nb) in blocks(0, NG, NB):
                ti = rin.next(); to = rout.next()
                self.dma("sp", ti.ap[:, 0:nb], self.pfm[row0 + c * 128:row0 + (c + 1) * 128, g0:g0 + nb], writes=[ti])
                S.op("act", lambda e: e.activation(out=to.ap[:, 0:nb], in_=ti.ap[:, 0:nb], func=AF.Copy, scale=sc), reads=[ti], writes=[to])
                self.dma("act", dst[c * 128:(c + 1) * 128, g0:g0 + nb], to.ap[:, 0:nb], reads=[to])
    rvf = Ring(self.AF.allocn(2, 512, "vf"))
    vas = self.AB.allocn(2, 8 * 65, "va")
    for va in vas:
        S.op("dve", lambda e, va=va: e.memset(va.ap, 1.0), writes=[va])
    rva = Ring(vas)
    for ti_ in range(NT):
        g0 = ti_ * 128
        VF = rvf.next(); VA = rva.next()
        vv = VF.ap.rearrange("p (c t) -> p c t", c=4)
        self.dma("sp", vv, self.pfm[C_NA_V:C_NA_V + 512, g0:g0 + 128].rearrange("(c p) t -> p c t", p=128), writes=[VF])
        ps = self.psr.next()
        for j in range(4):
            S.op("pe", lambda e: e.transpose(ps.ap[:, j * 128:(j + 1) * 128], vv[:, j, :], self.ident.ap), reads=[VF, self.ident], writes=[ps], acc=j > 0)
        S.op("act", lambda e: e.copy(out=VA.ap.rearrange("p (h q) -> p h q", h=8)[:, :, 0:64], in_=ps.ap.rearrange("p (h q) -> p h q", h=8)), reads=[ps], writes=[VA], acc=True)
        self.dma("act", self.nav[g0:g0 + 128, :], VA.ap, reads=[VA])
    for hc in range(4):
        self.phase()
        QT = self.AB.alloc(NG, "QT"); KT = self.AB.alloc(NG, "KT"); VT = self.AB.alloc(NT * 130, "VT"); BM = self.AB.alloc(ncls * 640, "BM")
        self.dma("sp", QT.ap, self.naq[hc * 128:(hc + 1) * 128, :], writes=[QT])
        self.dma("act", KT.ap, self.nak[hc * 128:(hc + 1) * 128, :], writes=[KT])
        self.dma("sp", VT.ap.rearrange("p (t q) -> p t q", t=NT), self.nav[:, hc * 130:(hc + 1) * 130].rearrange("(t p) q -> p t q", p=128), writes=[VT])
        for hh in range(2):
            self.dma("pool", BM.ap[hh * 64:(hh + 1) * 64, :].rearrange("p (c k) -> p c k", c=ncls), V["na_bm"][l, :, 2 * hc + hh].rearrange("c p k -> p c k"), writes=[BM], reads=[BM] if hh else [])
        vt3 = VT.ap.rearrange("p (t q) -> p t q", t=NT)
        bm3 = BM.ap.rearrange("p (c k) -> p c k", c=ncls)
        rPT = Ring(self.AB.allocn(3, 448, "PT")); rO = Ring(self.AF.allocn(3, 128, "NO", parts=64)); rs = Ring(self.AF.allocn(4, 2, "ns", parts=64))
        qrows = [("c", i) for i in range(CTX // 64)] + [("l", r) for r in range(ROWS)]
        for (kind, r) in qrows:
            OUT = rO.next()
            for hh in range(2):
                pb_ = hh * 64
                if kind == "c":
                    q0 = r * 64
                    tiles = [(None, t * 128) for t in range(CTX // 128)]
                else:
                    q0 = CTX + r * 64
                    ws, as_, cls = info[r]
                    tiles = [(j, CTX + (as_ + 2 * j) * 64) for j in range(5)] + [(None, t * 128) for t in range(CTX // 128)]
                nt_ = len(tiles)
                psS = self.psr.next()
                for ti_, (j, k0) in enumerate(tiles):
                    S.op("pe", lambda e: e.matmul(psS.ap[:, ti_ * 64:(ti_ + 1) * 64], lhsT=KT.ap[pb_:pb_ + 64, k0:k0 + 128], rhs=QT.ap[pb_:pb_ + 64, q0:q0 + 64], start=True, stop=(j is None)), reads=[KT, QT], writes=[psS], acc=ti_ > 0)
                    if j is not None:
                        S.op("pe", lambda e: e.matmul(psS.ap[:, ti_ * 64:(ti_ + 1) * 64], lhsT=bm3[pb_:pb_ + 64, cls, j * 128:(j + 1) * 128], rhs=self.identb.ap[pb_:pb_ + 64, pb_:pb_ + 64], start=False, stop=True), reads=[BM, self.identb], writes=[psS], acc=True)
                PT = rPT.next()
                S.op("act", lambda e: e.activation(out=PT.ap[:, 0:nt_ * 64], in_=psS.ap[:, 0:nt_ * 64], func=AF.Exp), reads=[psS], writes=[PT])
                psO = self.psr.next()
                for ti_, (j, k0) in enumerate(tiles):
                    S.op("pe", lambda e: e.matmul(psO.ap[0:64, 0:65], lhsT=PT.ap[:, ti_ * 64:(ti_ + 1) * 64], rhs=vt3[:, k0 // 128, hh * 65:(hh + 1) * 65], start=(ti_ == 0), stop=(ti_ == nt_ - 1)), reads=[PT, VT], writes=[psO], acc=ti_ > 0)
                s = rs.next()
                S.op("dve", lambda e: e.reciprocal(out=s.ap[:, 0:1], in_=psO.ap[0:64, 64:65]), reads=[psO], writes=[s])
                S.op("dve", lambda e: e.tensor_scalar_mul(out=OUT.ap[:, hh * 64:(hh + 1) * 64], in0=psO.ap[0:64, 0:64], scalar1=s.ap[:, 0:1]), reads=[psO, s], writes=[OUT], acc=hh > 0)
            self.dma("sp", self.natm[q0:q0 + 64, hc * 128:(hc + 1) * 128], OUT.ap, reads=[OUT])
    self.phase()
    rY = Ring(self.AF.allocn(2, 512, "naY")); rYo = Ring(self.AB.allocn(2, 512, "Yo"))
    for ti_ in range(NT):
        g0 = ti_ * 128
        Y = rY.next()
        self.dma("sp", Y.ap, self.natm[g0:g0 + 128, :], writes=[Y])
        self.store_tm_as_fm(Y, 1, g0, rYo)


K.na_phase = na_phase


import numpy as np
def pmaj(v, p=128):
    v = np.asarray(v)
    return np.ascontiguousarray(v.reshape(-1, p).T)
def base_inputs(inp, b):
    m = {}
    m["x"] = np.ascontiguousarray(inp["x"][b])
    m["ctx"] = np.ascontiguousarray(inp["ctx"][b])
    cT = np.stack([pmaj(inp["c"][b]), pmaj(inp["c_ctx"])], axis=-1)
    m["cT"] = np.ascontiguousarray(cT.astype(np.float32))
    m["ident"] = np.eye(128, dtype=np.float32)
    for k in ("w_ada", "b_ada", "w_in"):
        m[k] = np.ascontiguousarray(inp[k])
    return m

def lru_inputs(inp):
    m = {}
    L = inp["lru_conv_w"].shape[0]
    m["lru_cw"] = np.ascontiguousarray(np.stack([np.stack([pmaj(inp["lru_conv_w"][l, k]) for k in range(4)], -1) for l in range(L)]))
    m["lru_cb"] = np.ascontiguousarray(np.stack([pmaj(inp["lru_conv_b"][l]) for l in range(L)]))
    vec = []
    for l in range(L):
        vec.append(np.stack([pmaj(inp[nm][l, d]) for nm in ("lru_ba", "lru_bx", "lru_lambda") for d in range(2)], 1))
    m["lru_vec"] = np.ascontiguousarray(np.stack(vec))
    m["lru_wa"] = np.ascontiguousarray(inp["lru_wa"]); m["lru_wx"] = np.ascontiguousarray(inp["lru_wx"])
    return m

def ssd_inputs(inp):
    m = {}
    L = inp["ssd_conv_w"].shape[0]
    m["ssd_cw"] = np.ascontiguousarray(np.stack([np.stack([pmaj(inp["ssd_conv_w"][l, k]) for k in range(4)], -1) for l in range(L)]))
    m["ssd_cb"] = np.ascontiguousarray(np.stack([pmaj(inp["ssd_conv_b"][l]) for l in range(L)]))
    m["ssd_hp"] = np.ascontiguousarray(np.stack([np.stack([inp["ssd_dt_bias"][l,0], inp["ssd_dt_bias"][l,1], inp["ssd_a_log"][l,0], inp["ssd_a_log"][l,1]], -1) for l in range(L)]))
    m["ssd_dskip"] = np.ascontiguousarray(np.repeat(inp["ssd_d"], 64, axis=-1))
    m["ssd_gn"] = np.ascontiguousarray(inp["ssd_norm_g"])
    j = np.arange(128)[:, None]; i = np.arange(128)[None, :]
    m["tri"] = np.stack([(j <= i), (j >= i)]).astype(np.float32)
    sel = np.zeros((40, 8, 128), np.float32)
    for h in range(8): sel[h, h, :] = 1.0
    m["sel40"] = sel
    return m

def mlstm_inputs(inp, SEQ):
    m = {}
    L = inp["ml_conv_w"].shape[0]
    m["ml_cw"] = np.ascontiguousarray(np.stack([np.stack([pmaj(inp["ml_conv_w"][l, k]) for k in range(4)], -1) for l in range(L)]))
    m["ml_cb"] = np.ascontiguousarray(np.stack([pmaj(inp["ml_conv_b"][l]) for l in range(L)]))
    m["ml_hp"] = np.ascontiguousarray(np.stack([np.stack([inp["ml_i_bias"][l,0], inp["ml_i_bias"][l,1], inp["ml_f_bias"][l,0], inp["ml_f_bias"][l,1]], -1) for l in range(L)]))
    sel = np.zeros((24, 4, 128), np.float32)
    for h in range(4): sel[5 * 4 + h, h, :] = 1.0
    m["sel24"] = sel
    t = np.arange(SEQ, dtype=np.int32)
    pos = np.stack([t // 64, t % 64], axis=-1).astype(np.float32)
    nf = 32
    inv_freq = (np.float32(10000.0) ** (-np.arange(nf, dtype=np.float32) / nf)).astype(np.float32)
    ang = np.broadcast_to(pos[:, :, None, None] * inv_freq, (SEQ, 2, 2, nf)).reshape(SEQ, 128).astype(np.float32)
    m["rope_cos"] = np.ascontiguousarray(np.cos(ang).T.astype(np.float32))
    m["rope_sin"] = np.ascontiguousarray(np.sin(ang).T.astype(np.float32))
    P = np.zeros((128, 128), np.float32)
    for dp in range(128):
        pair = (dp % 64) // 32
        if pair == 0: P[dp + 32, dp] = -1.0
        else: P[dp - 32, dp] = 1.0
    m["ropeP"] = P
    return m

def na_inputs(inp, ROWS):
    info = []; classes = {}
    for r in range(ROWS):
        ws = min(max(r - 4, 0), ROWS - 8); as_ = min(2 * (ws // 2), ROWS - 10)
        key = (ws - as_, r - ws)
        if key not in classes: classes[key] = (len(classes), r, ws, as_)
    ncls = len(classes)
    rpb = inp["na_rpb"]; L = rpb.shape[0]
    wq = np.arange(64)[:, None]; wk = np.arange(64)[None, :]
    cs = np.clip(wq - 8, 0, 48)
    col_ok = (wk >= cs) & (wk < cs + 16)
    dc = np.clip(wk - wq + 15, 0, 30)
    bm = np.full((L, ncls, 8, 64, 640), -30000.0, np.float32)
    for key, (ci, r, ws, as_) in classes.items():
        for j in range(5):
            for par in range(2):
                kr = as_ + 2 * j + par
                if ws <= kr < ws + 8:
                    dr = kr - r + 7
                    vals = rpb[:, :, dr][:, :, dc]
                    blk = np.where(col_ok[None, None], vals, np.float32(-30000.0))
                    bm[:, ci, :, :, j * 128 + par * 64: j * 128 + par * 64 + 64] = blk
    return {"na_bm": np.ascontiguousarray(bm)}

def moe_inputs(inp):
    wr = inp["moe_w_router"]
    m = {}
    m["moe_wr_p"] = np.ascontiguousarray(wr.reshape(wr.shape[0], 16, 128, 8).transpose(0, 2, 1, 3).reshape(wr.shape[0], 128, 128))
    m["moe_br_p"] = np.ascontiguousarray(np.broadcast_to(inp["moe_b_router"][:, None, :], (wr.shape[0], 128, 8)))
    for k in ("moe_w_gate", "moe_w_up", "moe_w_down"):
        m[k] = np.ascontiguousarray(inp[k])
    return m


def kernel(**inputs):
    inp = {k: np.asarray(v) for k, v in inputs.items()}
    B, SEQ, _ = inp["x"].shape
    NS = 4
    cfg = Cfg(SEQ, inp["ffn_w_gate"].shape[-1], inp["moe_w_gate"].shape[-1], layers=(0, 1), dbg=(), NS=NS)
    k = K(cfg)
    nc = k.build()
    maps = []
    shared = {}
    shared.update(lru_inputs(inp)); shared.update(ssd_inputs(inp)); shared.update(mlstm_inputs(inp, SEQ))
    shared.update(na_inputs(inp, SEQ // 64)); shared.update(moe_inputs(inp))
    for kk in ("w_branch", "w_out", "ln_g", "ln_b", "ffn_w_gate", "ffn_w_up", "ffn_w_down"):
        shared[kk] = np.ascontiguousarray(inp[kk])
    for b in range(B):
        m = base_inputs(inp, b)
        m.update(shared)
        mm = {kk: m[kk] for kk in k.ins}
        for s_ in range(NS):
            maps.append(mm)
    res = run_bass_kernel_spmd(nc, maps, core_ids=list(range(B * NS)))
    return np.stack([np.concatenate([res.results[b * NS + s_]["y_out"] for s_ in range(NS)], axis=0) for b in range(B)], axis=0).astype(np.float32)
```
